# Optimizing a Trainium2 kernel written in Bass

```python
import jax, jax.numpy as jnp
from jax import lax
import numpy as np

D_MODEL = 1024
BATCH = 8
SEQ = 8192
DEPTH = 2

D_FF = 2816
FFN_RES_WEIGHT = 0.5
ATT_HEADS = 8
ATT_HEAD_DIM = 64
ATT_WIDTH = ATT_HEADS * ATT_HEAD_DIM
KV_DIM = ATT_HEAD_DIM
IDX_HEADS = 8
IDX_DIM = 32
TOPK_MAX = 256
Q_BLOCK = 128
SC_WIDTH = D_MODEL - ATT_WIDTH
SC_GROUPS = 8
SC_KERNEL = 3
CONF_WIDTH = D_MODEL
CONF_KERNEL = 31
ROPE_THETA = 500000.0
ROT_FRACTION = 4
NORM_EPS = 1e-6

HYB_SPLIT_SIZES = (ATT_WIDTH, KV_DIM, KV_DIM, IDX_HEADS * IDX_DIM, IDX_DIM, IDX_HEADS,
                   SC_WIDTH, SC_WIDTH, SC_WIDTH)
HYB_COLS = ATT_WIDTH + 2 * KV_DIM + IDX_HEADS * IDX_DIM + IDX_DIM + IDX_HEADS + 3 * SC_WIDTH

kernel_name = "hybrid_dsa_shortconv_conformer_macaron"


def rms_norm(x, g):
    xf = x.astype(jnp.float32)
    y = xf * lax.rsqrt(jnp.mean(xf * xf, axis=-1, keepdims=True) + NORM_EPS)
    return (y * g.astype(jnp.float32)).astype(x.dtype)


def layer_norm(x, g, b):
    xf = x.astype(jnp.float32)
    mu = jnp.mean(xf, axis=-1, keepdims=True)
    xc = xf - mu
    var = jnp.mean(xc * xc, axis=-1, keepdims=True)
    y = xc * lax.rsqrt(var + NORM_EPS) * g.astype(jnp.float32) + b.astype(jnp.float32)
    return y.astype(x.dtype)


def half_step_swiglu(h, g, w_gate, w_up, w_down):
    hn = rms_norm(h, g)
    return h + FFN_RES_WEIGHT * ((jax.nn.silu(hn @ w_gate) * (hn @ w_up)) @ w_down)


def rope_tables(seq, rot_dim):
    half = rot_dim // 2
    inv_freq = ROPE_THETA ** (-jnp.arange(half, dtype=jnp.float32) / half)
    ang = jnp.arange(seq, dtype=jnp.float32)[:, None] * inv_freq[None, :]
    return jnp.cos(ang), jnp.sin(ang)


def partial_rope(x, cos, sin):
    half = cos.shape[-1]
    shape = (1, cos.shape[0]) + (1,) * (x.ndim - 3) + (half,)
    c = cos.reshape(shape).astype(x.dtype)
    s = sin.reshape(shape).astype(x.dtype)
    x1 = x[..., :half]
    x2 = x[..., half:2 * half]
    return jnp.concatenate([x1 * c - x2 * s, x2 * c + x1 * s, x[..., 2 * half:]], axis=-1)


def causal_depthwise_conv(x, w):
    k, c = w.shape
    return lax.conv_general_dilated(
        x, w[:, None, :].astype(x.dtype), window_strides=(1,), padding=[(k - 1, 0)],
        dimension_numbers=('NWC', 'WIO', 'NWC'), feature_group_count=c)


def dsa_sparse_attention(q, k, v, q_idx, k_idx, w_idx):
    b, s, n_heads, dh = q.shape
    topk = min(TOPK_MAX, s // 4)
    n_blk = s // Q_BLOCK
    key_pos = jnp.arange(s)
    k_idx_f = k_idx.astype(jnp.float32)

    def to_blocks(a):
        return a.reshape((b, n_blk, Q_BLOCK) + a.shape[2:]).swapaxes(0, 1)

    def block(args):
        start, qb, qib, wib = args
        q_pos = start + jnp.arange(Q_BLOCK)
        dots = jnp.einsum('bqhd,bkd->bqhk', qib.astype(jnp.float32), k_idx_f) * (IDX_DIM ** -0.5)
        score = jnp.einsum('bqh,bqhk->bqk', wib.astype(jnp.float32), jax.nn.relu(dots))
        causal = key_pos[None, :] <= q_pos[:, None]
        score = jnp.where(causal[None], score, -jnp.inf)
        _, sel = lax.top_k(score, topk)
        valid = sel <= q_pos[None, :, None]
        k_sel = jax.vmap(lambda kk, ii: kk[ii])(k, sel)
        v_sel = jax.vmap(lambda vv, ii: vv[ii])(v, sel)
        logits = jnp.einsum('bqhd,bqkd->bqhk', qb, k_sel).astype(jnp.float32) * (dh ** -0.5)
        logits = jnp.where(valid[:, :, None, :], logits, -jnp.inf)
        p = jax.nn.softmax(logits, axis=-1).astype(v.dtype)
        return jnp.einsum('bqhk,bqkd->bqhd', p, v_sel)

    starts = jnp.arange(n_blk) * Q_BLOCK
    out = lax.map(block, (starts, to_blocks(q), to_blocks(q_idx), to_blocks(w_idx)))
    return out.swapaxes(0, 1).reshape(b, s, n_heads, dh)


def hybrid_dsa_shortconv_mixer(hn, w_in, conv_w, w_out, cos_a, sin_a, cos_i, sin_i):
    b, s, _ = hn.shape
    z = hn @ w_in
    offsets = []
    acc = 0
    for size in HYB_SPLIT_SIZES[:-1]:
        acc += size
        offsets.append(acc)
    q, k, v, qi, ki, wi, gate_b, gate_c, u = jnp.split(z, offsets, axis=-1)
    q = partial_rope(q.reshape(b, s, ATT_HEADS, ATT_HEAD_DIM), cos_a, sin_a)
    k = partial_rope(k, cos_a, sin_a)
    qi = partial_rope(qi.reshape(b, s, IDX_HEADS, IDX_DIM), cos_i, sin_i)
    ki = partial_rope(ki, cos_i, sin_i)
    wi = wi * (IDX_HEADS ** -0.5)
    y_attn = dsa_sparse_attention(q, k, v, qi, ki, wi).reshape(b, s, ATT_WIDTH)
    y_conv = gate_b * causal_depthwise_conv(gate_c * u, conv_w)
    return jnp.concatenate([y_attn, y_conv], axis=-1) @ w_out


def conformer_conv_module(hn, w_pw1, b_pw1, conv_w, conv_b, ln_g, ln_b, w_pw2, b_pw2):
    a, gate = jnp.split(hn @ w_pw1 + b_pw1, 2, axis=-1)
    u = a * jax.nn.sigmoid(gate)
    u = causal_depthwise_conv(u, conv_w) + conv_b
    u = jax.nn.silu(layer_norm(u, ln_g, ln_b))
    return u @ w_pw2 + b_pw2


def setup_inputs(seed: int = 0) -> dict:
    key = jax.random.key(seed)
    ks = jax.random.split(key, 20)
    n_even = (DEPTH + 1) // 2
    n_odd = DEPTH // 2
    f32 = jnp.float32

    def nrm(k, shape, scale):
        return jax.random.normal(k, shape, f32) * scale

    return {
        "x": nrm(ks[0], (BATCH, SEQ, D_MODEL), 1.0),
        "ffn_norm": 1.0 + nrm(ks[1], (DEPTH, 2, D_MODEL), 0.02),
        "ffn_w_gate": nrm(ks[2], (DEPTH, 2, D_MODEL, D_FF), D_MODEL ** -0.5),
        "ffn_w_up": nrm(ks[3], (DEPTH, 2, D_MODEL, D_FF), D_MODEL ** -0.5),
        "ffn_w_down": nrm(ks[4], (DEPTH, 2, D_FF, D_MODEL), D_FF ** -0.5),
        "mix_norm": 1.0 + nrm(ks[5], (DEPTH, D_MODEL), 0.02),
        "hyb_w_in": nrm(ks[6], (n_even, D_MODEL, HYB_COLS), D_MODEL ** -0.5),
        "hyb_conv_w": nrm(ks[7], (n_even, SC_KERNEL, SC_WIDTH), SC_KERNEL ** -0.5),
        "hyb_w_out": nrm(ks[8], (n_even, ATT_WIDTH + SC_WIDTH, D_MODEL), (ATT_WIDTH + SC_WIDTH) ** -0.5),
        "conf_w_pw1": nrm(ks[9], (n_odd, D_MODEL, 2 * CONF_WIDTH), D_MODEL ** -0.5),
        "conf_b_pw1": nrm(ks[10], (n_odd, 2 * CONF_WIDTH), 0.02),
        "conf_conv_w": nrm(ks[11], (n_odd, CONF_KERNEL, CONF_WIDTH), CONF_KERNEL ** -0.5),
        "conf_conv_b": nrm(ks[12], (n_odd, CONF_WIDTH), 0.02),
        "conf_ln_g": 1.0 + nrm(ks[13], (n_odd, CONF_WIDTH), 0.02),
        "conf_ln_b": nrm(ks[14], (n_odd, CONF_WIDTH), 0.02),
        "conf_w_pw2": nrm(ks[15], (n_odd, CONF_WIDTH, D_MODEL), CONF_WIDTH ** -0.5),
        "conf_b_pw2": nrm(ks[16], (n_odd, D_MODEL), 0.02),
        "final_norm": 1.0 + nrm(ks[17], (D_MODEL,), 0.02),
    }


def reference(x, ffn_norm, ffn_w_gate, ffn_w_up, ffn_w_down, mix_norm, hyb_w_in, hyb_conv_w,
              hyb_w_out, conf_w_pw1, conf_b_pw1, conf_conv_w, conf_conv_b, conf_ln_g, conf_ln_b,
              conf_w_pw2, conf_b_pw2, final_norm):
    s = x.shape[1]
    cos_a, sin_a = rope_tables(s, ATT_HEAD_DIM // ROT_FRACTION)
    cos_i, sin_i = rope_tables(s, IDX_DIM // ROT_FRACTION)
    h = x
    for layer in range(DEPTH):
        h = half_step_swiglu(h, ffn_norm[layer, 0], ffn_w_gate[layer, 0], ffn_w_up[layer, 0],
                             ffn_w_down[layer, 0])
        hn = rms_norm(h, mix_norm[layer])
        if layer % 2 == 0:
            e = layer // 2
            h = h + hybrid_dsa_shortconv_mixer(hn, hyb_w_in[e], hyb_conv_w[e], hyb_w_out[e],
                                               cos_a, sin_a, cos_i, sin_i)
        else:
            o = layer // 2
            h = h + conformer_conv_module(hn, conf_w_pw1[o], conf_b_pw1[o], conf_conv_w[o],
                                          conf_conv_b[o], conf_ln_g[o], conf_ln_b[o],
                                          conf_w_pw2[o], conf_b_pw2[o])
        h = half_step_swiglu(h, ffn_norm[layer, 1], ffn_w_gate[layer, 1], ffn_w_up[layer, 1],
                             ffn_w_down[layer, 1])
    return rms_norm(h, final_norm)
```

```python
import numpy as np
import concourse.bass as bass
import concourse.mybir as mybir

F32 = mybir.dt.float32
BF16 = mybir.dt.bfloat16
AF = mybir.ActivationFunctionType
ALU = mybir.AluOpType
AX = mybir.AxisListType

DT_SIZE = {F32: 4, BF16: 2}
SEM_LIMIT = 2000
N_DMA_SEMS = 16


class View:
    __slots__ = ("ap", "regs")

    def __init__(self, ap, regs):
        self.ap = ap
        self.regs = regs


class Space:
    def __init__(self):
        self.b = [0, 1 << 60]
        self.w = [None]
        self.r = [[]]

    def _split(self, x):
        import bisect
        i = bisect.bisect_left(self.b, x)
        if self.b[i] == x:
            return i
        self.b.insert(i, x)
        self.w.insert(i, self.w[i - 1])
        self.r.insert(i, list(self.r[i - 1]))
        return i

    def rng(self, lo, hi):
        i = self._split(lo)
        j = self._split(hi)
        return range(i, j)


class Op:
    __slots__ = ("eng", "fn", "deps", "needed", "ticket", "dma", "idx")

    def __init__(self, eng, fn, dma):
        self.eng = eng
        self.fn = fn
        self.deps = []
        self.needed = False
        self.ticket = None
        self.dma = dma


class SBT:
    def __init__(self, K, name, shape, dtype, offset=None):
        self.K = K
        self.shape = list(shape)
        self.dtype = dtype
        es = DT_SIZE[dtype]
        nfree = int(np.prod(shape[1:]))
        if offset is None:
            offset = K.sb_alloc(nfree * es)
        self.off = offset
        self.es = es
        self.h = K.nc.alloc_sbuf_tensor_at(name, list(shape), dtype, offset=offset)
        st = []
        acc = 1
        for d in reversed(shape[1:]):
            st.append(acc)
            acc *= d
        self.strides = list(reversed(st))

    def __getitem__(self, idx):
        if not isinstance(idx, tuple):
            idx = (idx,)
        idx = tuple(idx) + (slice(None),) * (len(self.shape) - len(idx))
        lo = 0
        hi = 0
        for k, (ix, d) in enumerate(zip(idx[1:], self.shape[1:])):
            s = self.strides[k]
            if isinstance(ix, int):
                a, b = ix, ix + 1
            else:
                a, b, step = ix.indices(d)
                assert step == 1
            lo += a * s
            hi += (b - 1) * s
        hi += 1
        ap = self.h[idx]
        return View(ap, [("sb", self.off + lo * self.es, self.off + hi * self.es)])


class PST:
    def __init__(self, K, name, bank):
        self.K = K
        self.bank = bank
        self.h = K.nc.alloc_psum_tensor(name, [128, 512], F32)

    def __getitem__(self, idx):
        if not isinstance(idx, tuple):
            idx = (idx,)
        idx = tuple(idx) + (slice(None),) * (2 - len(idx))
        return View(self.h[idx], [("ps", self.bank * 2048, self.bank * 2048 + 2048)])


class Kern:
    def __init__(self, nc):
        self.nc = nc
        self.ops = []
        self.spaces = {}
        self.sb_top = 16512
        self.eng = {"pe": nc.tensor, "act": nc.scalar, "dve": nc.vector,
                    "pool": nc.gpsimd, "sp": nc.sync}

    def sb_alloc(self, nbytes):
        off = (self.sb_top + 63) // 64 * 64
        self.sb_top = off + nbytes
        return off

    def sb(self, name, shape, dtype, offset=None):
        return SBT(self, name, shape, dtype, offset)

    def dview(self, ap, space, lo, hi):
        return View(ap, [(space, lo, hi)])

    def _sp(self, name):
        if name not in self.spaces:
            self.spaces[name] = Space()
        return self.spaces[name]

    def op(self, eng, fn, reads=(), writes=(), dma=False):
        o = Op(eng, fn, dma)
        o.idx = len(self.ops)
        deps = {}

        def add(d, raw):
            if d is None:
                return
            if not d.dma and not o.dma and d.eng == eng:
                if eng == "pe" or not raw:
                    return
            deps[d.idx] = d

        for v in reads:
            for (s, lo, hi) in v.regs:
                sp = self._sp(s)
                for i in sp.rng(lo, hi):
                    add(sp.w[i], True)
        for v in writes:
            for (s, lo, hi) in v.regs:
                sp = self._sp(s)
                for i in sp.rng(lo, hi):
                    add(sp.w[i], False)
                    for r in sp.r[i]:
                        add(r, False)
        for v in reads:
            for (s, lo, hi) in v.regs:
                sp = self._sp(s)
                for i in sp.rng(lo, hi):
                    sp.r[i].append(o)
        for v in writes:
            for (s, lo, hi) in v.regs:
                sp = self._sp(s)
                for i in sp.rng(lo, hi):
                    sp.w[i] = o
                    sp.r[i] = []
        o.deps = list(deps.values())
        for d in o.deps:
            d.needed = True
        self.ops.append(o)
        return o

    def mm(self, out, lhsT, rhs, start=True, stop=True):
        nc = self.nc
        return self.op("pe", lambda: nc.tensor.matmul(out.ap, lhsT.ap, rhs.ap, start=start, stop=stop),
                       reads=[lhsT, rhs], writes=[out])

    def act(self, out, in_, func, bias=None, scale=None, accum=None, eng="act"):
        nc = self.nc
        kw = {}
        rd = [in_]
        wr = [out]
        if bias is not None:
            if isinstance(bias, View):
                kw["bias"] = bias.ap
                rd.append(bias)
            else:
                kw["bias"] = bias
        if scale is not None:
            if isinstance(scale, View):
                kw["scale"] = scale.ap
                rd.append(scale)
            else:
                kw["scale"] = scale
        if accum is not None:
            kw["accum_out"] = accum.ap
            wr.append(accum)
        return self.op("act", lambda: nc.scalar.activation(out.ap, in_.ap, func, **kw), reads=rd, writes=wr)

    def _v(self, eng):
        return self.nc.vector if eng == "dve" else self.nc.gpsimd

    def tt(self, out, in0, in1, op, eng="dve"):
        e = self._v(eng)
        return self.op(eng, lambda: e.tensor_tensor(out.ap, in0.ap, in1.ap, op), reads=[in0, in1], writes=[out])

    def ts(self, out, in0, s1, s2, op0, op1=None, accum=None, eng="dve"):
        e = self._v(eng)
        rd = [in0]
        wr = [out]
        a1 = s1
        a2 = s2
        if isinstance(s1, View):
            rd.append(s1)
            a1 = s1.ap
        if isinstance(s2, View):
            rd.append(s2)
            a2 = s2.ap
        kw = {}
        if op1 is not None:
            kw["op1"] = op1
        if accum is not None:
            kw["accum_out"] = accum.ap
            wr.append(accum)
        return self.op(eng, lambda: e.tensor_scalar(out.ap, in0.ap, a1, a2, op0, **kw), reads=rd, writes=wr)

    def stt(self, out, in0, scalar, in1, op0, op1, eng="dve"):
        e = self._v(eng)
        rd = [in0, in1]
        a = scalar
        if isinstance(scalar, View):
            rd.append(scalar)
            a = scalar.ap
        return self.op(eng, lambda: e.scalar_tensor_tensor(out.ap, in0.ap, a, in1.ap, op0, op1),
                       reads=rd, writes=[out])

    def copy(self, out, in_, eng="dve"):
        if eng == "act":
            nc = self.nc
            return self.op("act", lambda: nc.scalar.copy(out.ap, in_.ap), reads=[in_], writes=[out])
        e = self._v(eng)
        return self.op(eng, lambda: e.tensor_copy(out.ap, in_.ap), reads=[in_], writes=[out])

    def memset(self, out, val, eng="dve"):
        e = self._v(eng)
        return self.op(eng, lambda: e.memset(out.ap, val), writes=[out])

    def dma(self, q, out, in_):
        e = self.eng[q]
        return self.op(q, lambda: e.dma_start(out=out.ap, in_=in_.ap), reads=[in_], writes=[out], dma=True)

    def emit(self, final_waits=()):
        nc = self.nc
        import contextlib
        self._stack = contextlib.ExitStack()
        sems = []

        def new_sem(nm):
            s = self._stack.enter_context(nc.semaphore(nm))
            sems.append(s)
            return s

        cur = {}
        nsem = [0]
        dsem = [[new_sem(f"dq{i}"), 0] for i in range(N_DMA_SEMS)]
        dnext = [0]
        waited = {}

        def wait(engname, sem, val):
            key = (engname, id(sem))
            if waited.get(key, 0) >= val:
                return
            waited[key] = val
            self.eng[engname].wait_ge(sem, val)

        nw = 0
        for o in self.ops:
            for d in o.deps:
                sem, val = d.ticket
                wait(o.eng, sem, val)
            if o.dma:
                k = dnext[0] % N_DMA_SEMS
                dnext[0] += 1
                sem, cnt = dsem[k]
                if cnt:
                    wait(o.eng, sem, cnt)
                ins = o.fn()
                ins.then_inc(sem, 16)
                dsem[k][1] = cnt + 16
                o.ticket = (sem, cnt + 16)
            else:
                ins = o.fn()
                if o.needed:
                    c = cur.get(o.eng)
                    if c is None or c[1] >= SEM_LIMIT:
                        nsem[0] += 1
                        c = [new_sem(f"s_{o.eng}{nsem[0]}"), 0]
                        cur[o.eng] = c
                    c[1] += 1
                    ins.then_inc(c[0], 1)
                    o.ticket = (c[0], c[1])
        for d in final_waits:
            sem, val = d.ticket
            wait("sp", sem, val)
        self.n_sems = len(sems)
        return len(self.ops)

from concourse.bass_utils import run_bass_kernel_spmd

D = 1024
DFF = 2816
NF = DFF // 128
NC8 = D // 128
NT = 512
EPS = 1e-6


class Ctx:
    pass


def build(S, phases, n_vec_cols=None):
    if n_vec_cols is None:
        n_vec_cols = NVEC
    nc = bass.Bass("TRN2", target_bir_lowering=False)
    K = Kern(nc)
    C = Ctx()
    C.nc, C.K, C.S = nc, K, S
    C.ntiles = S // NT

    def din(name, shape, dt=F32):
        return nc.dram_tensor(name, list(shape), dt, kind="ExternalInput").ap()

    C.xT = din("xT", [D, S])
    C.vecs = din("vecs", [128, n_vec_cols])
    C.wg = din("ffn_w_gate", [2, 2, D, DFF])
    C.wu = din("ffn_w_up", [2, 2, D, DFF])
    C.wd = din("ffn_w_down", [2, 2, DFF, D])
    C.w_pw1 = din("conf_w_pw1", [D, 2 * D])
    C.w_pw2 = din("conf_w_pw2", [D, D])
    C.w_in = din("hyb_w_in", [D, 2472])
    C.w_hyb_a = din("w_hyb_a", [D, HCOLS_A])
    C.w_out = din("hyb_w_out", [D, D])
    C.tab = din("rope_tab", [4, 128, S])
    C.cst = din("hyb_cst", [128, 896])
    C.outT = nc.dram_tensor("outT", [D, S], F32, kind="ExternalOutput").ap()
    C.hA = nc.dram_tensor("hA", [D, S], F32, kind="Internal").ap()

    C.VEC = K.sb("VEC", [128, n_vec_cols], F32)
    C.ONES = K.sb("ONES", [128, 128], F32)
    C.X = [K.sb(f"X{i}", [128, NC8, NT], F32) for i in range(2)]
    C.TMP = [K.sb(f"TMP{i}", [128, NT], F32) for i in range(2)]
    C.ACC = K.sb("ACC", [128, NT], F32)
    C.RSTD = K.sb("RSTD", [128, NT], F32)
    C.HN = K.sb("HN", [128, NC8, NT], BF16)
    C.arena = K.sb_top
    C.PS = [PST(K, f"PS{i}", i) for i in range(8)]

    K.dma("sp", C.VEC[:, :], K.dview(C.vecs, "d_vecs", 0, 1))
    K.memset(C.ONES[:, :], 1.0)

    ffn_alloc(C)
    conf_alloc(C)
    hyb_alloc(C)
    last = None
    for ph in phases:
        if ph[0] == "ffn":
            last = ffn_phase(C, getattr(C, ph[1]), ph[2], getattr(C, ph[3]), *ph[4:])
        elif ph[0] == "hyb":
            last = hyb_phase(C, getattr(C, ph[1]), ph[2], getattr(C, ph[3]), ph[4])
        elif ph[0] == "conf":
            last = conf_phase(C, getattr(C, ph[1]), ph[2], getattr(C, ph[3]), ph[4])
    n = K.emit(final_waits=last)
    print("ops", n, "sems", K.n_sems, "sbuf top", K.sb_top)
    return nc

def ffn_alloc(C):
    K = C.K
    K.sb_top = C.arena
    C.WG = K.sb("WG", [128, NC8, DFF], BF16)
    C.WU = K.sb("WU", [128, NC8, DFF], BF16)
    C.WD = K.sb("WD", [128, NF, D], BF16)
    C.G = K.sb("G", [128, NF, NT], BF16)
    C.SILU = [K.sb(f"SILU{i}", [128, NT], BF16) for i in range(2)]
    C.OUTT = K.sb("OUTT", [128, NC8, NT], F32, offset=C.G.off)


def dram_tile(C, ap, name, i):
    t0 = i * NT
    return C.K.dview(ap[:, t0:t0 + NT].rearrange("(c p) t -> p c t", p=128), name, t0, t0 + NT)


def rmsnorm(C, xt, vcol, out):
    K = C.K
    K.tt(C.ACC[:, :], xt[:, 0, :], xt[:, 0, :], ALU.mult)
    for c in range(1, NC8):
        tmp = C.TMP[c % 2]
        K.tt(tmp[:, :], xt[:, c, :], xt[:, c, :], ALU.mult, eng="pool")
        K.tt(C.ACC[:, :], C.ACC[:, :], tmp[:, :], ALU.add)
    ps = C.PS[6][:, :]
    K.mm(ps, C.ONES[:, :], C.ACC[:, :])
    K.ts(C.RSTD[:, :], ps, 1.0 / D, EPS, ALU.mult, ALU.add)
    nc = C.nc
    rs = C.RSTD[:, :]
    K.op("dve", lambda: nc.vector.reciprocal(rs.ap, rs.ap), reads=[rs], writes=[rs])
    K.act(rs, rs, AF.Sqrt)
    for c in range(NC8):
        K.stt(out[:, c, :], xt[:, c, :], C.VEC[:, vcol + c:vcol + c + 1], C.RSTD[:, :], ALU.mult, ALU.mult)


def ffn_phase(C, src, src_name, dst, dst_name, l, j, vcol, final_vcol=None):
    K, nc = C.K, C.nc
    wg = C.wg[l, j].rearrange("(c p) f -> p c f", p=128)
    wu = C.wu[l, j].rearrange("(c p) f -> p c f", p=128)
    wd = C.wd[l, j].rearrange("(c p) m -> p c m", p=128)
    for c in range(NC8):
        K.dma("pool", C.WG[:, c, :], K.dview(wg[:, c, :], "d_w", 0, 1))
        K.dma("pool", C.WU[:, c, :], K.dview(wu[:, c, :], "d_w", 0, 1))
    for c in range(NF):
        K.dma("pool", C.WD[:, c, :], K.dview(wd[:, c, :], "d_w", 0, 1))
    stores = []
    nt = C.ntiles
    K.dma("sp", C.X[0][:, :, :], dram_tile(C, src, src_name, 0))
    rmsnorm(C, C.X[0], vcol, C.HN)
    for i in range(nt):
        xt = C.X[i % 2]
        if i + 1 < nt:
            K.dma("sp", C.X[(i + 1) % 2][:, :, :], dram_tile(C, src, src_name, i + 1))
        for f in range(NF):
            pg = C.PS[f % 2][:, :]
            pu = C.PS[2 + f % 2][:, :]
            for c in range(NC8):
                K.mm(pg, C.WG[:, c, f * 128:(f + 1) * 128], C.HN[:, c, :], start=(c == 0), stop=(c == NC8 - 1))
            for c in range(NC8):
                K.mm(pu, C.WU[:, c, f * 128:(f + 1) * 128], C.HN[:, c, :], start=(c == 0), stop=(c == NC8 - 1))
            sl = C.SILU[f % 2]
            K.act(sl[:, :], pg, AF.Silu)
            K.tt(C.G[:, f, :], sl[:, :], pu, ALU.mult)
        if i + 1 < nt:
            rmsnorm(C, C.X[(i + 1) % 2], vcol, C.HN)
        for m in range(NC8):
            po = C.PS[4 + m % 2][:, :]
            for f in range(NF):
                K.mm(po, C.WD[:, f, m * 128:(m + 1) * 128], C.G[:, f, :], start=(f == 0), stop=(f == NF - 1))
            K.stt(xt[:, m, :], po, 0.5, xt[:, m, :], ALU.mult, ALU.add)
        if final_vcol is None:
            stores.append(K.dma("act", dram_tile(C, dst, dst_name, i), xt[:, :, :]))
        else:
            rmsnorm(C, xt, final_vcol, C.OUTT)
            stores.append(K.dma("act", dram_tile(C, dst, dst_name, i), C.OUTT[:, :, :]))
    return stores


VEC_SPEC = [("ffn_norm", 4 * 8), ("mix_norm", 2 * 8), ("final_norm", 8), ("conf_b_pw1", 16), ("conf_conv_b", 8),
            ("conf_ln_g", 8), ("conf_ln_b", 8), ("conf_b_pw2", 8), ("conf_conv_w", 8 * 31), ("hyb_conv_w", 4 * 3)]
VOFF = {}
_o = 0
for _n, _c in VEC_SPEC:
    VOFF[_n] = _o
    _o += _c
NVEC = _o


def pack_vecs(inp):
    v = np.zeros((128, NVEC), np.float32)

    def put(name, arr):
        a = np.asarray(arr, np.float32).reshape(-1)
        n = a.size // 128
        v[:, VOFF[name]:VOFF[name] + n] = a.reshape(n, 128).T

    put("ffn_norm", inp["ffn_norm"])
    put("mix_norm", inp["mix_norm"])
    put("final_norm", inp["final_norm"])
    put("conf_b_pw1", inp["conf_b_pw1"])
    put("conf_conv_b", inp["conf_conv_b"])
    put("conf_ln_g", inp["conf_ln_g"])
    put("conf_ln_b", inp["conf_ln_b"])
    put("conf_b_pw2", inp["conf_b_pw2"])
    cw = np.asarray(inp["conf_conv_w"], np.float32).reshape(31, 8, 128)
    v[:, VOFF["conf_conv_w"]:VOFF["conf_conv_w"] + 248] = cw.transpose(2, 1, 0).reshape(128, 248)
    hw = np.asarray(inp["hyb_conv_w"], np.float32).reshape(3, 4, 128)
    v[:, VOFF["hyb_conv_w"]:VOFF["hyb_conv_w"] + 12] = hw.transpose(2, 1, 0).reshape(128, 12)
    return v


CK = 31
HALO = CK - 1
CONV_POOL_CHUNKS = 0


def conf_alloc(C):
    K = C.K
    K.sb_top = C.arena
    C.W1 = K.sb("W1", [128, NC8, 2 * D], BF16)
    C.W2 = K.sb("W2", [128, NC8, D], BF16)
    C.U = [K.sb(f"U{i}", [128, NC8, HALO + NT], F32) for i in range(2)]
    C.Y = K.sb("Y", [128, NC8, NT], F32)
    C.V = K.sb("V", [128, NC8, NT], BF16)
    C.SG = [K.sb(f"SG{i}", [128, NT], F32) for i in range(2)]
    C.A1 = K.sb("A1", [128, NT], F32)
    C.A2 = K.sb("A2", [128, NT], F32)
    C.MEAN = K.sb("MEAN", [128, NT], F32)


def conf_phase(C, src, src_name, dst, dst_name):
    K, nc = C.K, C.nc
    w1 = C.w_pw1.rearrange("(c p) f -> p c f", p=128)
    w2 = C.w_pw2.rearrange("(c p) f -> p c f", p=128)
    for c in range(NC8):
        K.dma("pool", C.W1[:, c, :], K.dview(w1[:, c, :], "d_w", 0, 1))
    for c in range(NC8):
        K.dma("pool", C.W2[:, c, :], K.dview(w2[:, c, :], "d_w", 0, 1))
    vb1 = VOFF["conf_b_pw1"]
    vcb = VOFF["conf_conv_b"]
    vg, vb = VOFF["conf_ln_g"], VOFF["conf_ln_b"]
    vb2 = VOFF["conf_b_pw2"]
    vcw = VOFF["conf_conv_w"]
    vnorm = VOFF["mix_norm"] + 8
    stores = []
    nt = C.ntiles
    K.dma("sp", C.X[0][:, :, :], dram_tile(C, src, src_name, 0))
    for i in range(nt):
        xt = C.X[i % 2]
        if i + 1 < nt:
            K.dma("sp", C.X[(i + 1) % 2][:, :, :], dram_tile(C, src, src_name, i + 1))
        rmsnorm(C, xt, vnorm, C.HN)
        U = C.U[i % 2]
        Up = C.U[(i - 1) % 2]
        if i == 0:
            K.memset(U[:, :, 0:HALO], 0.0)
        else:
            K.copy(U[:, :, 0:HALO], Up[:, :, NT:NT + HALO], eng="pool")
        for c in range(NC8):
            pa = C.PS[c % 2][:, :]
            pg = C.PS[2 + c % 2][:, :]
            for k in range(NC8):
                K.mm(pa, C.W1[:, k, c * 128:(c + 1) * 128], C.HN[:, k, :], start=(k == 0), stop=(k == NC8 - 1))
            for k in range(NC8):
                K.mm(pg, C.W1[:, k, D + c * 128:D + (c + 1) * 128], C.HN[:, k, :], start=(k == 0), stop=(k == NC8 - 1))
            sg = C.SG[c % 2]
            K.act(sg[:, :], pg, AF.Sigmoid, bias=C.VEC[:, vb1 + 8 + c:vb1 + 9 + c])
            K.stt(U[:, c, HALO:HALO + NT], pa, C.VEC[:, vb1 + c:vb1 + c + 1], sg[:, :], ALU.add, ALU.mult)
        for c in range(NC8):
            eng = "pool" if c >= NC8 - CONV_POOL_CHUNKS else "dve"
            K.ts(C.Y[:, c, :], U[:, c, 0:NT], C.VEC[:, vcw + c * CK:vcw + c * CK + 1], C.VEC[:, vcb + c:vcb + c + 1],
                 ALU.mult, ALU.add, eng=eng)
            for j in range(1, CK):
                K.stt(C.Y[:, c, :], U[:, c, j:j + NT], C.VEC[:, vcw + c * CK + j:vcw + c * CK + j + 1], C.Y[:, c, :],
                      ALU.mult, ALU.add, eng=eng)
        K.copy(C.A1[:, :], C.Y[:, 0, :])
        K.tt(C.A2[:, :], C.Y[:, 0, :], C.Y[:, 0, :], ALU.mult)
        for c in range(1, NC8):
            tmp = C.TMP[c % 2]
            K.tt(tmp[:, :], C.Y[:, c, :], C.Y[:, c, :], ALU.mult, eng="pool")
            K.tt(C.A1[:, :], C.A1[:, :], C.Y[:, c, :], ALU.add)
            K.tt(C.A2[:, :], C.A2[:, :], tmp[:, :], ALU.add)
        p1 = C.PS[6][:, :]
        p2 = C.PS[7][:, :]
        K.mm(p1, C.ONES[:, :], C.A1[:, :])
        K.mm(p2, C.ONES[:, :], C.A2[:, :])
        K.ts(C.MEAN[:, :], p1, 1.0 / D, None, ALU.mult)
        K.tt(C.A1[:, :], C.MEAN[:, :], C.MEAN[:, :], ALU.mult)
        K.stt(C.A2[:, :], p2, 1.0 / D, C.A1[:, :], ALU.mult, ALU.subtract)
        K.ts(C.A2[:, :], C.A2[:, :], EPS, None, ALU.add)
        a2 = C.A2[:, :]
        K.op("dve", lambda: nc.vector.reciprocal(a2.ap, a2.ap), reads=[a2], writes=[a2])
        K.act(C.RSTD[:, :], a2, AF.Sqrt)
        for c in range(NC8):
            K.tt(C.Y[:, c, :], C.Y[:, c, :], C.MEAN[:, :], ALU.subtract)
            K.tt(C.Y[:, c, :], C.Y[:, c, :], C.RSTD[:, :], ALU.mult)
            K.act(C.V[:, c, :], C.Y[:, c, :], AF.Silu, bias=C.VEC[:, vb + c:vb + c + 1],
                  scale=C.VEC[:, vg + c:vg + c + 1])
        for m in range(NC8):
            po = C.PS[4 + m % 2][:, :]
            for k in range(NC8):
                K.mm(po, C.W2[:, k, m * 128:(m + 1) * 128], C.V[:, k, :], start=(k == 0), stop=(k == NC8 - 1))
            K.stt(xt[:, m, :], po, C.VEC[:, vb2 + m:vb2 + m + 1], xt[:, m, :], ALU.add, ALU.add)
        stores.append(K.dma("act", dram_tile(C, dst, dst_name, i), xt[:, :, :]))
    return stores


NH = 256
HCOLS_A = 1224
CQ, CK2, CVW, CQI, CKI = 0, 512, 640, 712, 1096
GU0 = 936
N_ITER = 18
TOPK = 256
NEG = -30000.0


def hyb_consts(S):
    theta = np.float32(500000.0)
    t = np.arange(S, dtype=np.float32)

    def tabs(half, period, nrep):
        inv = (theta ** (-(np.arange(half, dtype=np.float32) / np.float32(half)))).astype(np.float32)
        ang = (t[:, None] * inv[None, :]).astype(np.float32)
        c, s = np.cos(ang).astype(np.float32).T, np.sin(ang).astype(np.float32).T
        CC = np.ones((128, S), np.float32)
        SS = np.zeros((128, S), np.float32)
        for base in range(0, 128, period):
            CC[base:base + half] = c
            CC[base + half:base + 2 * half] = c
            SS[base:base + half] = -s
            SS[base + half:base + 2 * half] = s
        return CC, SS

    CCa, SSa = tabs(8, 64, 2)
    CCi, SSi = tabs(4, 32, 4)
    tab = np.stack([CCa, SSa, CCi, SSi], 0)

    def perm(half, period):
        P = np.zeros((128, 128), np.float32)
        for m in range(128):
            r = m % period
            if r < half:
                P[m + half, m] = 1.0
            elif r < 2 * half:
                P[m - half, m] = 1.0
        return P

    cst = np.zeros((128, 128 + 128 + 512 + 128), np.float32)
    cst[:, 0:128] = perm(8, 64)
    cst[:, 128:256] = perm(4, 32)
    cst[:, 256:768] = np.tile(np.eye(128, dtype=np.float32), (1, 4))
    ii = np.arange(128)
    cst[:, 768:896] = np.where(ii[None, :] <= ii[:, None], 0.0, -1e30).astype(np.float32)
    return tab, cst


def pack_hyb_w(w_in):
    w = np.asarray(w_in, np.float32)
    q = w[:, 0:512]
    k = w[:, 512:576]
    v = w[:, 576:640]
    qi = w[:, 640:896]
    ki = w[:, 896:928]
    wi = w[:, 928:936]
    qig = []
    for g in range(3):
        hs = [3 * g, 3 * g + 1, 3 * g + 2]
        cols = [qi[:, 32 * h:32 * h + 32] if h < 8 else qi[:, 0:32] for h in hs] + [qi[:, 0:32]]
        qig.append(np.concatenate(cols, 1))
    out = np.concatenate([q, k, k, v, wi] + qig + [ki, ki, ki, ki], 1)
    assert out.shape[1] == HCOLS_A
    return np.ascontiguousarray(out)


def hyb_alloc(C):
    K = C.K
    S = C.S
    o = C.X[0].off
    C.XH = [K.sb(f"XH{i}", [128, NC8, NH], F32, offset=o + i * 8192) for i in range(2)]
    C.TAB = K.sb("TAB", [128, 4, NH], F32, offset=o + 16384)
    C.YAT = K.sb("YAT", [128, 8, NH], BF16, offset=o + 20480)
    C.QT = [K.sb(f"QT{i}", [128, 4, NH], BF16, offset=o + 24576 + i * 2048) for i in range(2)]
    C.YC = [K.sb(f"YC{i}", [128, 4, NH], BF16, offset=o + 28672 + i * 2048) for i in range(2)]
    o = C.TMP[0].off
    C.TMPH = [K.sb(f"TMPH{i}", [128, NH], F32, offset=o + i * 1024) for i in range(2)]
    C.ACCH = K.sb("ACCH", [128, NH], F32, offset=o + 2048)
    C.RSTDH = K.sb("RSTDH", [128, NH], F32, offset=o + 3072)
    C.HNH = K.sb("HNH", [128, NC8, NH], BF16, offset=o + 4096)
    C.RR = [K.sb(f"RR{i}", [128, 512], BF16, offset=o + 8192 + i * 1024) for i in range(4)]
    C.PT = [K.sb(f"PT{i}", [128, 512], BF16, offset=o + 12288 + i * 1024) for i in range(2)]
    C.T1 = K.sb("T1", [128, NH], F32, offset=o + 14336)
    assert o + 16384 <= C.arena
    K.sb_top = C.arena
    C.WINR = K.sb("WINR", [128, NC8, HCOLS_A], BF16)
    C.WOA = K.sb("WOA", [128, 8, D], BF16)
    C.WOC = K.sb("WOC", [128, 4, D], BF16)
    C.KT2 = K.sb("KT2", [128, S], BF16)
    C.KI3 = K.sb("KI3", [128, S], BF16)
    C.VA = K.sb("VA", [128, S // 128, 65], BF16)
    C.SS_ = K.sb("SS_", [128, 8192], F32)
    so = C.SS_.off
    C.WGU = K.sb("WGU", [128, NC8, 1536], BF16, offset=so)
    C.PBUF = K.sb("PBUF", [128, 4, NH + 2], F32, offset=so + 24576)
    C.GCS = K.sb("GCS", [128, NH], F32, offset=so + 24576 + 4160)
    C.CV = K.sb("CV", [128, NH], F32, offset=so + 24576 + 4160 + 1024)
    C.QF = K.sb("QF", [128, NH], F32, offset=so + 24576 + 4160 + 2048)
    assert 24576 + 4160 + 3072 <= 32768
    C.NEGM = K.sb("NEGM", [128, 8192], BF16)
    C.QIT = K.sb("QIT", [128, 3, NH], BF16)
    C.DG = K.sb("DG", [128, 8, 128], BF16)
    C.RDEN = K.sb("RDEN", [128, 1024], F32)
    C.RB = [K.sb(f"RB{i}", [128, 512], F32) for i in range(2)]
    C.HALO = K.sb("HALO", [128, 4, 2], F32)
    C.WI = K.sb("WI", [128, 2, 8], F32)
    C.WA = K.sb("WA", [128, 2, 8], F32)
    C.SGN = K.sb("SGN", [128, 2, 8], F32)
    C.ST = K.sb("ST", [128, 8], F32)
    C.CST = K.sb("CST", [128, 896], F32)
    C.IDB = K.sb("IDB", [128, 512], BF16)
    print("hyb sbuf top", K.sb_top)
    assert K.sb_top <= 229344


def rmsnorm_h(C, xt, vcol):
    K = C.K
    nc = C.nc
    K.tt(C.ACCH[:, :], xt[:, 0, :], xt[:, 0, :], ALU.mult)
    for c in range(1, NC8):
        tmp = C.TMPH[c % 2]
        K.tt(tmp[:, :], xt[:, c, :], xt[:, c, :], ALU.mult, eng="pool")
        K.tt(C.ACCH[:, :], C.ACCH[:, :], tmp[:, :], ALU.add)
    ps = C.PS[6][:, 0:NH]
    K.mm(ps, C.ONES[:, :], C.ACCH[:, :])
    rs = C.RSTDH[:, :]
    K.ts(rs, ps, 1.0 / D, EPS, ALU.mult, ALU.add)
    K.op("dve", lambda: nc.vector.reciprocal(rs.ap, rs.ap), reads=[rs], writes=[rs])
    K.act(rs, rs, AF.Sqrt)
    for c in range(NC8):
        K.stt(C.HNH[:, c, :], xt[:, c, :], C.VEC[:, vcol + c:vcol + c + 1], rs, ALU.mult, ALU.mult)


def hyb_phase(C, src, src_name, dst, dst_name):
    K, nc = C.K, C.nc
    S = C.S
    nht = S // NH
    nblk = S // 128
    wa = C.w_hyb_a.rearrange("(c p) f -> p c f", p=128)
    for c in range(NC8):
        K.dma("pool", C.WINR[:, c, :], K.dview(wa[:, c, :], "d_w", 0, 1))
    woa = C.w_out[0:512, :].rearrange("(h p) m -> p h m", p=64)
    woc = C.w_out[512:1024, :].rearrange("(c p) m -> p c m", p=128)
    for h in range(8):
        K.dma("pool", C.WOA[0:64, h, :], K.dview(woa[:, h, :], "d_w", 0, 1))
    for c in range(4):
        K.dma("pool", C.WOC[:, c, :], K.dview(woc[:, c, :], "d_w", 0, 1))
    K.dma("sp", C.CST[:, :], K.dview(C.cst, "d_cst", 0, 1))
    K.copy(C.IDB[:, :], C.CST[:, 256:768])
    K.memset(C.VA[:, :, 64:65], 1.0)
    K.memset(C.HALO[:, :, :], 0.0)
    PMA = C.CST[:, 0:128]
    PMI = C.CST[:, 128:256]
    CAUS = C.CST[:, 768:896]
    wgu = C.w_in[:, GU0:GU0 + 1536].rearrange("(c p) f -> p c f", p=128)
    vnorm = VOFF["mix_norm"]
    vcw = VOFF["hyb_conv_w"]
    stores = []

    def rope(ps, ci, si, PM, out):
        K.copy(C.QF[:, :], ps, eng="act")
        ps2 = C.PS[4 + rope.n % 2][:, 0:NH]
        rope.n += 1
        K.mm(ps2, PM, C.QF[:, :])
        K.tt(C.T1[:, :], C.QF[:, :], C.TAB[:, ci, :], ALU.mult)
        K.tt(C.QF[:, :], ps2, C.TAB[:, si, :], ALU.mult)
        K.tt(out, C.T1[:, :], C.QF[:, :], ALU.add)
    rope.n = 0

    def proj(i):
        t0 = i * NH
        xt = C.XH[i % 2]
        K.dma("sp", xt[:, :, :], C.K.dview(src[:, t0:t0 + NH].rearrange("(c p) t -> p c t", p=128), src_name, t0, t0 + NH))
        K.dma("sp", C.TAB[:, :, :], K.dview(C.tab[:, :, t0:t0 + NH].rearrange("k p t -> p k t"), "d_tab", 0, 1))
        for c in range(NC8):
            K.dma("pool", C.WGU[:, c, :], K.dview(wgu[:, c, :], "d_w", 0, 1))
        rmsnorm_h(C, xt, vnorm)
        pi = [0]

        def nps(rows=128, cols=NH):
            p = C.PS[pi[0] % 4][0:rows, 0:cols]
            pi[0] += 1
            return p

        def fm(ps, W, col0, ncol):
            for k in range(NC8):
                K.mm(ps, W[:, k, col0:col0 + ncol], C.HNH[:, k, :], start=(k == 0), stop=(k == NC8 - 1))

        QT = C.QT[i % 2]
        for c in range(4):
            ps = nps()
            fm(ps, C.WINR, CQ + c * 128, 128)
            rope(ps, 0, 1, PMA, QT[:, c, :])
        ps = nps()
        fm(ps, C.WINR, CK2, 128)
        rope(ps, 0, 1, PMA, C.KT2[:, t0:t0 + NH])
        for g in range(3):
            ps = nps()
            fm(ps, C.WINR, CQI + g * 128, 128)
            rope(ps, 2, 3, PMI, C.QIT[:, g, :])
        ps = nps()
        fm(ps, C.WINR, CKI, 128)
        rope(ps, 2, 3, PMI, C.KI3[:, t0:t0 + NH])
        for q in range(2):
            ps = nps(128, 72)
            for k in range(NC8):
                K.mm(ps, C.HNH[:, k, q * 128:(q + 1) * 128], C.WINR[:, k, CVW:CVW + 72], start=(k == 0), stop=(k == NC8 - 1))
            ch = (t0 // 128) + q
            K.copy(C.VA[:, ch, 0:64], C.PS[(pi[0] - 1) % 4][:, 0:64])
            K.ts(C.WI[:, q, :], C.PS[(pi[0] - 1) % 4][:, 64:72], 1.0 / 16.0, None, ALU.mult)
            K.ts(C.SGN[:, q, :], C.WI[:, q, :], 0.0, 2.0, ALU.is_ge, ALU.mult)
            K.ts(C.SGN[:, q, :], C.SGN[:, q, :], -1.0, None, ALU.add)
            K.tt(C.WA[:, q, :], C.WI[:, q, :], C.SGN[:, q, :], ALU.mult)
        YC = C.YC[i % 2]
        for c in range(4):
            pgc = nps()
            fm(pgc, C.WGU, 512 + c * 128, 128)
            K.copy(C.GCS[:, :], pgc, eng="act")
            pu = nps()
            fm(pu, C.WGU, 1024 + c * 128, 128)
            K.copy(C.PBUF[:, c, 0:2], C.HALO[:, c, :])
            K.tt(C.PBUF[:, c, 2:2 + NH], pu, C.GCS[:, :], ALU.mult)
            K.copy(C.HALO[:, c, :], C.PBUF[:, c, NH:NH + 2])
            K.ts(C.CV[:, :], C.PBUF[:, c, 2:2 + NH], C.VEC[:, vcw + c * 3 + 2:vcw + c * 3 + 3], None, ALU.mult)
            K.stt(C.CV[:, :], C.PBUF[:, c, 1:1 + NH], C.VEC[:, vcw + c * 3 + 1:vcw + c * 3 + 2], C.CV[:, :], ALU.mult, ALU.add)
            K.stt(C.CV[:, :], C.PBUF[:, c, 0:NH], C.VEC[:, vcw + c * 3:vcw + c * 3 + 1], C.CV[:, :], ALU.mult, ALU.add)
            pgb = nps()
            fm(pgb, C.WGU, c * 128, 128)
            K.tt(YC[:, c, :], pgb, C.CV[:, :], ALU.mult)

    def score(b):
        L = 128 * (b + 1)
        qq = b % 2
        for h in range(8):
            K.ts(C.DG[:, h, :], C.IDB[:, 0:128], C.SGN[:, qq, h:h + 1], None, ALU.mult)
        nkt = (L + 511) // 512
        for kt in range(nkt):
            k0 = 512 * kt
            w = min(512, L - k0)
            pss = C.PS[4 + kt % 2][:, 0:w]
            pend = []
            for h in range(8):
                g, r = divmod(h, 3)
                ps = C.PS[h % 4][:, 0:w]
                K.mm(ps, C.QIT[32 * r:32 * r + 32, g, qq * 128:(qq + 1) * 128], C.KI3[32 * r:32 * r + 32, k0:k0 + w])
                K.act(C.RR[h % 4][:, 0:w], ps, AF.Relu, scale=C.WA[:, qq, h:h + 1])
                pend.append(h)
                if len(pend) > 2:
                    hh = pend.pop(0)
                    K.mm(pss, C.DG[:, hh, :], C.RR[hh % 4][:, 0:w], start=(hh == 0), stop=(hh == 7))
            for hh in pend:
                K.mm(pss, C.DG[:, hh, :], C.RR[hh % 4][:, 0:w], start=(hh == 0), stop=(hh == 7))
            K.copy(C.SS_[:, k0:k0 + w], pss)
        K.tt(C.SS_[:, L - 128:L], C.SS_[:, L - 128:L], CAUS, ALU.add)

    def thresh(b):
        L = 128 * (b + 1)
        LO = C.ST[:, 0:1]
        MID = C.ST[:, 1:2]
        CNT = C.ST[:, 2:3]
        M = C.ST[:, 3:4]
        if b < 2:
            K.memset(LO, -1e29)
        else:
            B = 16.0
            step = B
            K.memset(MID, 0.0)
            junk = View(C.T1.h[:, 0:1].broadcast_to([128, L]), C.T1[:, 0:1].regs)
            for it in range(N_ITER):
                K.ts(junk, C.SS_[:, 0:L], MID, 0.0, ALU.is_ge, ALU.add, accum=CNT)
                K.ts(M, CNT, float(TOPK), step, ALU.is_ge, ALU.mult)
                nstep = step / 2.0
                if it < N_ITER - 1:
                    K.stt(MID, M, -nstep, MID, ALU.add, ALU.add)
                else:
                    K.stt(LO, M, -step, MID, ALU.add, ALU.add)
                step = nstep

    def negm_piece(b, p):
        L = 128 * (b + 1)
        k0 = 512 * p
        w = min(512, L - k0)
        K.ts(C.NEGM[:, k0:k0 + w], C.SS_[:, k0:k0 + w], C.ST[:, 0:1], NEG, ALU.is_lt, ALU.mult)

    def npieces(b):
        return (128 * (b + 1) + 511) // 512

    import os
    dbg = os.environ.get("HYB_DBG", "psta3")

    def attn(b, nb=None):
        qq = b % 2
        QT = C.QT[(b // 2) % 2]
        OT = [C.PS[6], C.PS[7]]
        pdone = 0
        for j in range(b + 1):
            psl = [C.PS[(2 * j) % 4], C.PS[(2 * j + 1) % 4]]
            for hl in range(4):
                for par in range(2):
                    h = 2 * hl + par
                    base = 64 * par
                    K.mm(psl[par][:, hl * 128:(hl + 1) * 128], C.KT2[base:base + 64, 128 * j:128 * j + 128],
                         QT[base:base + 64, hl, qq * 128:(qq + 1) * 128], start=(hl == 0), stop=False)
            for par in range(2):
                K.mm(psl[par][:, :], C.NEGM[:, 128 * j:128 * j + 128], C.IDB[:, :], start=False, stop=True)
            for par in range(2):
                pt = C.PT[par]
                K.act(pt[:, :], psl[par][:, :], AF.Exp, scale=0.125)
                if "2" in dbg or "3" in dbg:
                    K.mm(OT[par][0:65, :], C.VA[:, j, 0:65], pt[:, :], start=(j == 0), stop=(j == b))
            if nb is not None and (j + 1) % 4 == 0:
                negm_piece(nb, pdone)
                pdone += 1
        if nb is not None:
            while pdone < npieces(nb):
                negm_piece(nb, pdone)
                pdone += 1
        if "3" not in dbg:
            return
        for par in range(2):
            rd = C.RDEN[64:65, par * 512:(par + 1) * 512]
            ot = OT[par]
            K.op("dve", (lambda rd=rd, ot=ot: nc.vector.reciprocal(rd.ap, ot[64:65, :].ap)), reads=[ot[64:65, :]], writes=[rd])
            pb = C.PS[4 + par][0:64, :]
            K.mm(pb, C.ONES[64:65, 0:64], rd)
            rb = C.RB[par]
            K.copy(rb[0:64, :], pb, eng="act")
            yv = C.YAT[0:64, 4 * par:4 * par + 4, qq * 128:(qq + 1) * 128]
            otv = View(ot.h[0:64, :].rearrange("p (h t) -> p h t", h=4), ot[0:64, :].regs)
            rbv = View(rb.h[0:64, :].rearrange("p (h t) -> p h t", h=4), rb[0:64, :].regs)
            K.tt(yv, otv, rbv, ALU.mult)

    def outproj(i):
        t0 = i * NH
        xt = C.XH[i % 2]
        YC = C.YC[i % 2]
        for m in range(NC8):
            po = C.PS[m % 4][:, 0:NH]
            for h in range(8):
                K.mm(po, C.WOA[0:64, h, m * 128:(m + 1) * 128], C.YAT[0:64, 4 * (h % 2) + h // 2, :], start=(h == 0), stop=False)
            for c in range(4):
                K.mm(po, C.WOC[:, c, m * 128:(m + 1) * 128], YC[:, c, :], start=False, stop=(c == 3))
            K.tt(xt[:, m, :], xt[:, m, :], po, ALU.add)
        stores.append(K.dma("act", K.dview(dst[:, t0:t0 + NH].rearrange("(c p) t -> p c t", p=128), dst_name, t0, t0 + NH), xt[:, :, :]))

    for i in range(nht):
        proj(i)
        for b in (2 * i, 2 * i + 1):
            score(b)
            thresh(b)
            if b == 0:
                for p in range(npieces(0)):
                    negm_piece(0, p)
            else:
                attn(b - 1, b)
                if (b - 1) % 2 == 1:
                    outproj((b - 1) // 2)
    attn(nblk - 1, None)
    outproj(nht - 1)
    return stores


PHASES = [
    ("ffn", "xT", "d_x", "hA", "d_h", 0, 0, VOFF["ffn_norm"] + 0),
    ("hyb", "hA", "d_h", "hA", "d_h"),
    ("ffn", "hA", "d_h", "hA", "d_h", 0, 1, VOFF["ffn_norm"] + 8),
    ("ffn", "hA", "d_h", "hA", "d_h", 1, 0, VOFF["ffn_norm"] + 16),
    ("conf", "hA", "d_h", "hA", "d_h"),
    ("ffn", "hA", "d_h", "outT", "d_out", 1, 1, VOFF["ffn_norm"] + 24, VOFF["final_norm"]),
]

_CACHE = {}


def host_inputs(inputs, S):
    f = lambda a: np.ascontiguousarray(np.asarray(a, np.float32))
    tab, cst = hyb_consts(S)
    shared = {
        "vecs": pack_vecs(inputs),
        "ffn_w_gate": f(inputs["ffn_w_gate"]), "ffn_w_up": f(inputs["ffn_w_up"]), "ffn_w_down": f(inputs["ffn_w_down"]),
        "conf_w_pw1": f(np.asarray(inputs["conf_w_pw1"])[0]), "conf_w_pw2": f(np.asarray(inputs["conf_w_pw2"])[0]),
        "hyb_w_in": f(np.asarray(inputs["hyb_w_in"])[0]), "w_hyb_a": pack_hyb_w(np.asarray(inputs["hyb_w_in"])[0]),
        "hyb_w_out": f(np.asarray(inputs["hyb_w_out"])[0]), "rope_tab": tab, "hyb_cst": cst,
    }
    return shared


def kernel(**inputs):
    x = np.asarray(inputs["x"], np.float32)
    B, S, _ = x.shape
    if S not in _CACHE:
        _CACHE[S] = build(S, PHASES)
    nc = _CACHE[S]
    shared = host_inputs(inputs, S)
    in_maps = []
    for b in range(B):
        m = dict(shared)
        m["xT"] = np.ascontiguousarray(x[b].T)
        in_maps.append(m)
    res = run_bass_kernel_spmd(nc, in_maps, core_ids=list(range(B)))
    out = np.stack([np.ascontiguousarray(r["outT"].T) for r in res.results], 0)
    return out.astype(np.float32)
```

```python
import numpy as np
import concourse.bass as bass
import concourse.mybir as mybir

F32 = mybir.dt.float32
BF16 = mybir.dt.bfloat16
AF = mybir.ActivationFunctionType
ALU = mybir.AluOpType
AX = mybir.AxisListType

DT_SIZE = {F32: 4, BF16: 2}
SEM_LIMIT = 2000
N_DMA_SEMS = 16


class View:
    __slots__ = ("ap", "regs")

    def __init__(self, ap, regs):
        self.ap = ap
        self.regs = regs


class Space:
    def __init__(self):
        self.b = [0, 1 << 60]
        self.w = [None]
        self.r = [[]]

    def _split(self, x):
        import bisect
        i = bisect.bisect_left(self.b, x)
        if self.b[i] == x:
            return i
        self.b.insert(i, x)
        self.w.insert(i, self.w[i - 1])
        self.r.insert(i, list(self.r[i - 1]))
        return i

    def rng(self, lo, hi):
        i = self._split(lo)
        j = self._split(hi)
        return range(i, j)


class Op:
    __slots__ = ("eng", "fn", "deps", "needed", "ticket", "dma", "idx")

    def __init__(self, eng, fn, dma):
        self.eng = eng
        self.fn = fn
        self.deps = []
        self.needed = False
        self.ticket = None
        self.dma = dma


class SBT:
    def __init__(self, K, name, shape, dtype, offset=None):
        self.K = K
        self.shape = list(shape)
        self.dtype = dtype
        es = DT_SIZE[dtype]
        nfree = int(np.prod(shape[1:]))
        if offset is None:
            offset = K.sb_alloc(nfree * es)
        self.off = offset
        self.es = es
        self.h = K.nc.alloc_sbuf_tensor_at(name, list(shape), dtype, offset=offset)
        st = []
        acc = 1
        for d in reversed(shape[1:]):
            st.append(acc)
            acc *= d
        self.strides = list(reversed(st))

    def __getitem__(self, idx):
        if not isinstance(idx, tuple):
            idx = (idx,)
        idx = tuple(idx) + (slice(None),) * (len(self.shape) - len(idx))
        lo = 0
        hi = 0
        for k, (ix, d) in enumerate(zip(idx[1:], self.shape[1:])):
            s = self.strides[k]
            if isinstance(ix, int):
                a, b = ix, ix + 1
            else:
                a, b, step = ix.indices(d)
                assert step == 1
            lo += a * s
            hi += (b - 1) * s
        hi += 1
        ap = self.h[idx]
        return View(ap, [("sb", self.off + lo * self.es, self.off + hi * self.es)])


class PST:
    def __init__(self, K, name, bank):
        self.K = K
        self.bank = bank
        self.h = K.nc.alloc_psum_tensor(name, [128, 512], F32)

    def __getitem__(self, idx):
        if not isinstance(idx, tuple):
            idx = (idx,)
        idx = tuple(idx) + (slice(None),) * (2 - len(idx))
        return View(self.h[idx], [("ps", self.bank * 2048, self.bank * 2048 + 2048)])


class Kern:
    def __init__(self, nc):
        self.nc = nc
        self.ops = []
        self.spaces = {}
        self.sb_top = 16512
        self.eng = {"pe": nc.tensor, "act": nc.scalar, "dve": nc.vector,
                    "pool": nc.gpsimd, "sp": nc.sync}

    def sb_alloc(self, nbytes):
        off = (self.sb_top + 63) // 64 * 64
        self.sb_top = off + nbytes
        return off

    def sb(self, name, shape, dtype, offset=None):
        return SBT(self, name, shape, dtype, offset)

    def dview(self, ap, space, lo, hi):
        return View(ap, [(space, lo, hi)])

    def _sp(self, name):
        if name not in self.spaces:
            self.spaces[name] = Space()
        return self.spaces[name]

    def op(self, eng, fn, reads=(), writes=(), dma=False):
        o = Op(eng, fn, dma)
        o.idx = len(self.ops)
        deps = {}

        def add(d, raw):
            if d is None:
                return
            if not d.dma and not o.dma and d.eng == eng:
                if eng == "pe" or not raw:
                    return
            deps[d.idx] = d

        for v in reads:
            for (s, lo, hi) in v.regs:
                sp = self._sp(s)
                for i in sp.rng(lo, hi):
                    add(sp.w[i], True)
        for v in writes:
            for (s, lo, hi) in v.regs:
                sp = self._sp(s)
                for i in sp.rng(lo, hi):
                    add(sp.w[i], False)
                    for r in sp.r[i]:
                        add(r, False)
        for v in reads:
            for (s, lo, hi) in v.regs:
                sp = self._sp(s)
                for i in sp.rng(lo, hi):
                    sp.r[i].append(o)
        for v in writes:
            for (s, lo, hi) in v.regs:
                sp = self._sp(s)
                for i in sp.rng(lo, hi):
                    sp.w[i] = o
                    sp.r[i] = []
        o.deps = list(deps.values())
        for d in o.deps:
            d.needed = True
        self.ops.append(o)
        return o

    def mm(self, out, lhsT, rhs, start=True, stop=True):
        nc = self.nc
        return self.op("pe", lambda: nc.tensor.matmul(out.ap, lhsT.ap, rhs.ap, start=start, stop=stop),
                       reads=[lhsT, rhs], writes=[out])

    def act(self, out, in_, func, bias=None, scale=None, accum=None, eng="act"):
        nc = self.nc
        kw = {}
        rd = [in_]
        wr = [out]
        if bias is not None:
            if isinstance(bias, View):
                kw["bias"] = bias.ap
                rd.append(bias)
            else:
                kw["bias"] = bias
        if scale is not None:
            if isinstance(scale, View):
                kw["scale"] = scale.ap
                rd.append(scale)
            else:
                kw["scale"] = scale
        if accum is not None:
            kw["accum_out"] = accum.ap
            wr.append(accum)
        return self.op("act", lambda: nc.scalar.activation(out.ap, in_.ap, func, **kw), reads=rd, writes=wr)

    def _v(self, eng):
        return self.nc.vector if eng == "dve" else self.nc.gpsimd

    def tt(self, out, in0, in1, op, eng="dve"):
        e = self._v(eng)
        return self.op(eng, lambda: e.tensor_tensor(out.ap, in0.ap, in1.ap, op), reads=[in0, in1], writes=[out])

    def ts(self, out, in0, s1, s2, op0, op1=None, accum=None, eng="dve"):
        e = self._v(eng)
        rd = [in0]
        wr = [out]
        a1 = s1
        a2 = s2
        if isinstance(s1, View):
            rd.append(s1)
            a1 = s1.ap
        if isinstance(s2, View):
            rd.append(s2)
            a2 = s2.ap
        kw = {}
        if op1 is not None:
            kw["op1"] = op1
        if accum is not None:
            kw["accum_out"] = accum.ap
            wr.append(accum)
        return self.op(eng, lambda: e.tensor_scalar(out.ap, in0.ap, a1, a2, op0, **kw), reads=rd, writes=wr)

    def stt(self, out, in0, scalar, in1, op0, op1, eng="dve"):
        e = self._v(eng)
        rd = [in0, in1]
        a = scalar
        if isinstance(scalar, View):
            rd.append(scalar)
            a = scalar.ap
        return self.op(eng, lambda: e.scalar_tensor_tensor(out.ap, in0.ap, a, in1.ap, op0, op1),
                       reads=rd, writes=[out])

    def copy(self, out, in_, eng="dve"):
        if eng == "act":
            nc = self.nc
            return self.op("act", lambda: nc.scalar.copy(out.ap, in_.ap), reads=[in_], writes=[out])
        e = self._v(eng)
        return self.op(eng, lambda: e.tensor_copy(out.ap, in_.ap), reads=[in_], writes=[out])

    def memset(self, out, val, eng="dve"):
        e = self._v(eng)
        return self.op(eng, lambda: e.memset(out.ap, val), writes=[out])

    def dma(self, q, out, in_):
        e = self.eng[q]
        return self.op(q, lambda: e.dma_start(out=out.ap, in_=in_.ap), reads=[in_], writes=[out], dma=True)

    def emit(self, final_waits=()):
        nc = self.nc
        import contextlib
        self._stack = contextlib.ExitStack()
        sems = []

        def new_sem(nm):
            s = self._stack.enter_context(nc.semaphore(nm))
            sems.append(s)
            return s

        cur = {}
        nsem = [0]
        dsem = [[new_sem(f"dq{i}"), 0] for i in range(N_DMA_SEMS)]
        dnext = [0]
        waited = {}

        def wait(engname, sem, val):
            key = (engname, id(sem))
            if waited.get(key, 0) >= val:
                return
            waited[key] = val
            self.eng[engname].wait_ge(sem, val)

        nw = 0
        for o in self.ops:
            for d in o.deps:
                sem, val = d.ticket
                wait(o.eng, sem, val)
            if o.dma:
                k = dnext[0] % N_DMA_SEMS
                dnext[0] += 1
                sem, cnt = dsem[k]
                if cnt:
                    wait(o.eng, sem, cnt)
                ins = o.fn()
                ins.then_inc(sem, 16)
                dsem[k][1] = cnt + 16
                o.ticket = (sem, cnt + 16)
            else:
                ins = o.fn()
                if o.needed:
                    c = cur.get(o.eng)
                    if c is None or c[1] >= SEM_LIMIT:
                        nsem[0] += 1
                        c = [new_sem(f"s_{o.eng}{nsem[0]}"), 0]
                        cur[o.eng] = c
                    c[1] += 1
                    ins.then_inc(c[0], 1)
                    o.ticket = (c[0], c[1])
        for d in final_waits:
            sem, val = d.ticket
            wait("sp", sem, val)
        self.n_sems = len(sems)
        return len(self.ops)

from concourse.bass_utils import run_bass_kernel_spmd

D = 1024
DFF = 2816
NF = DFF // 128
NC8 = D // 128
NT = 512
EPS = 1e-6


class Ctx:
    pass


def build(S, phases, n_vec_cols=None):
    if n_vec_cols is None:
        n_vec_cols = NVEC
    nc = bass.Bass("TRN2", target_bir_lowering=False)
    K = Kern(nc)
    C = Ctx()
    C.nc, C.K, C.S = nc, K, S
    C.ntiles = S // NT

    def din(name, shape, dt=F32):
        return nc.dram_tensor(name, list(shape), dt, kind="ExternalInput").ap()

    C.xT = din("xT", [D, S])
    C.vecs = din("vecs", [128, n_vec_cols])
    C.wg = din("ffn_w_gate", [2, 2, D, DFF])
    C.wu = din("ffn_w_up", [2, 2, D, DFF])
    C.wd = din("ffn_w_down", [2, 2, DFF, D])
    C.w_pw1 = din("conf_w_pw1", [D, 2 * D])
    C.w_pw2 = din("conf_w_pw2", [D, D])
    C.w_in = din("hyb_w_in", [D, 2472])
    C.w_hyb_a = din("w_hyb_a", [D, HCOLS_A])
    C.w_out = din("hyb_w_out", [D, D])
    C.tab = din("rope_tab", [4, 128, S])
    C.cst = din("hyb_cst", [128, 896])
    C.outT = nc.dram_tensor("outT", [D, S], F32, kind="ExternalOutput").ap()
    C.hA = nc.dram_tensor("hA", [D, S], F32, kind="Internal").ap()

    C.VEC = K.sb("VEC", [128, n_vec_cols], F32)
    C.ONES = K.sb("ONES", [128, 128], F32)
    C.X = [K.sb(f"X{i}", [128, NC8, NT], F32) for i in range(2)]
    C.TMP = [K.sb(f"TMP{i}", [128, NT], F32) for i in range(2)]
    C.ACC = K.sb("ACC", [128, NT], F32)
    C.RSTD = K.sb("RSTD", [128, NT], F32)
    C.HN = K.sb("HN", [128, NC8, NT], BF16)
    C.arena = K.sb_top
    C.PS = [PST(K, f"PS{i}", i) for i in range(8)]

    K.dma("sp", C.VEC[:, :], K.dview(C.vecs, "d_vecs", 0, 1))
    K.memset(C.ONES[:, :], 1.0)

    ffn_alloc(C)
    conf_alloc(C)
    hyb_alloc(C)
    last = None
    for ph in phases:
        if ph[0] == "ffn":
            last = ffn_phase(C, getattr(C, ph[1]), ph[2], getattr(C, ph[3]), *ph[4:])
        elif ph[0] == "hyb":
            last = hyb_phase(C, getattr(C, ph[1]), ph[2], getattr(C, ph[3]), ph[4])
        elif ph[0] == "conf":
            last = conf_phase(C, getattr(C, ph[1]), ph[2], getattr(C, ph[3]), ph[4])
    n = K.emit(final_waits=last)
    print("ops", n, "sems", K.n_sems, "sbuf top", K.sb_top)
    return nc

def ffn_alloc(C):
    K = C.K
    K.sb_top = C.arena
    C.WG = K.sb("WG", [128, NC8, DFF], BF16)
    C.WU = K.sb("WU", [128, NC8, DFF], BF16)
    C.WD = K.sb("WD", [128, NF, D], BF16)
    C.G = K.sb("G", [128, NF, NT], BF16)
    C.SILU = [K.sb(f"SILU{i}", [128, NT], BF16) for i in range(2)]
    C.OUTT = K.sb("OUTT", [128, NC8, NT], F32, offset=C.G.off)


def dram_tile(C, ap, name, i):
    t0 = i * NT
    return C.K.dview(ap[:, t0:t0 + NT].rearrange("(c p) t -> p c t", p=128), name, t0, t0 + NT)


def rmsnorm(C, xt, vcol, out):
    K = C.K
    K.tt(C.ACC[:, :], xt[:, 0, :], xt[:, 0, :], ALU.mult)
    for c in range(1, NC8):
        tmp = C.TMP[c % 2]
        K.tt(tmp[:, :], xt[:, c, :], xt[:, c, :], ALU.mult, eng="pool")
        K.tt(C.ACC[:, :], C.ACC[:, :], tmp[:, :], ALU.add)
    ps = C.PS[6][:, :]
    K.mm(ps, C.ONES[:, :], C.ACC[:, :])
    K.ts(C.RSTD[:, :], ps, 1.0 / D, EPS, ALU.mult, ALU.add)
    nc = C.nc
    rs = C.RSTD[:, :]
    K.op("dve", lambda: nc.vector.reciprocal(rs.ap, rs.ap), reads=[rs], writes=[rs])
    K.act(rs, rs, AF.Sqrt)
    for c in range(NC8):
        K.stt(out[:, c, :], xt[:, c, :], C.VEC[:, vcol + c:vcol + c + 1], C.RSTD[:, :], ALU.mult, ALU.mult)


def ffn_phase(C, src, src_name, dst, dst_name, l, j, vcol, final_vcol=None):
    K, nc = C.K, C.nc
    wg = C.wg[l, j].rearrange("(c p) f -> p c f", p=128)
    wu = C.wu[l, j].rearrange("(c p) f -> p c f", p=128)
    wd = C.wd[l, j].rearrange("(c p) m -> p c m", p=128)
    for c in range(NC8):
        K.dma("pool", C.WG[:, c, :], K.dview(wg[:, c, :], "d_w", 0, 1))
        K.dma("pool", C.WU[:, c, :], K.dview(wu[:, c, :], "d_w", 0, 1))
    for c in range(NF):
        K.dma("pool", C.WD[:, c, :], K.dview(wd[:, c, :], "d_w", 0, 1))
    stores = []
    nt = C.ntiles
    K.dma("sp", C.X[0][:, :, :], dram_tile(C, src, src_name, 0))
    rmsnorm(C, C.X[0], vcol, C.HN)
    for i in range(nt):
        xt = C.X[i % 2]
        if i + 1 < nt:
            K.dma("sp", C.X[(i + 1) % 2][:, :, :], dram_tile(C, src, src_name, i + 1))
        for f in range(NF):
            pg = C.PS[f % 2][:, :]
            pu = C.PS[2 + f % 2][:, :]
            for c in range(NC8):
                K.mm(pg, C.WG[:, c, f * 128:(f + 1) * 128], C.HN[:, c, :], start=(c == 0), stop=(c == NC8 - 1))
            for c in range(NC8):
                K.mm(pu, C.WU[:, c, f * 128:(f + 1) * 128], C.HN[:, c, :], start=(c == 0), stop=(c == NC8 - 1))
            sl = C.SILU[f % 2]
            K.act(sl[:, :], pg, AF.Silu)
            K.tt(C.G[:, f, :], sl[:, :], pu, ALU.mult)
        if i + 1 < nt:
            rmsnorm(C, C.X[(i + 1) % 2], vcol, C.HN)
        for m in range(NC8):
            po = C.PS[4 + m % 2][:, :]
            for f in range(NF):
                K.mm(po, C.WD[:, f, m * 128:(m + 1) * 128], C.G[:, f, :], start=(f == 0), stop=(f == NF - 1))
            K.stt(xt[:, m, :], po, 0.5, xt[:, m, :], ALU.mult, ALU.add)
        if final_vcol is None:
            stores.append(K.dma("act", dram_tile(C, dst, dst_name, i), xt[:, :, :]))
        else:
            rmsnorm(C, xt, final_vcol, C.OUTT)
            stores.append(K.dma("act", dram_tile(C, dst, dst_name, i), C.OUTT[:, :, :]))
    return stores


VEC_SPEC = [("ffn_norm", 4 * 8), ("mix_norm", 2 * 8), ("final_norm", 8), ("conf_b_pw1", 16), ("conf_conv_b", 8),
            ("conf_ln_g", 8), ("conf_ln_b", 8), ("conf_b_pw2", 8), ("conf_conv_w", 8 * 31), ("hyb_conv_w", 4 * 3)]
VOFF = {}
_o = 0
for _n, _c in VEC_SPEC:
    VOFF[_n] = _o
    _o += _c
NVEC = _o


def pack_vecs(inp):
    v = np.zeros((128, NVEC), np.float32)

    def put(name, arr):
        a = np.asarray(arr, np.float32).reshape(-1)
        n = a.size // 128
        v[:, VOFF[name]:VOFF[name] + n] = a.reshape(n, 128).T

    put("ffn_norm", inp["ffn_norm"])
    put("mix_norm", inp["mix_norm"])
    put("final_norm", inp["final_norm"])
    put("conf_b_pw1", inp["conf_b_pw1"])
    put("conf_conv_b", inp["conf_conv_b"])
    put("conf_ln_g", inp["conf_ln_g"])
    put("conf_ln_b", inp["conf_ln_b"])
    put("conf_b_pw2", inp["conf_b_pw2"])
    cw = np.asarray(inp["conf_conv_w"], np.float32).reshape(31, 8, 128)
    v[:, VOFF["conf_conv_w"]:VOFF["conf_conv_w"] + 248] = cw.transpose(2, 1, 0).reshape(128, 248)
    hw = np.asarray(inp["hyb_conv_w"], np.float32).reshape(3, 4, 128)
    v[:, VOFF["hyb_conv_w"]:VOFF["hyb_conv_w"] + 12] = hw.transpose(2, 1, 0).reshape(128, 12)
    return v


CK = 31
HALO = CK - 1
CONV_POOL_CHUNKS = 0


def conf_alloc(C):
    K = C.K
    K.sb_top = C.arena
    C.W1 = K.sb("W1", [128, NC8, 2 * D], BF16)
    C.W2 = K.sb("W2", [128, NC8, D], BF16)
    C.U = K.sb("U", [128, NC8, HALO + NT], BF16)
    C.Y = K.sb("Y", [128, NC8, NT], F32)
    C.V = K.sb("V", [128, NC8, NT], BF16)
    C.SG = [K.sb(f"SG{i}", [128, NT], F32) for i in range(2)]
    C.A1 = K.sb("A1", [128, NT], F32)
    C.A2 = K.sb("A2", [128, NT], F32)
    C.MEAN = K.sb("MEAN", [128, NT], F32)
    C.DIAG = K.sb("DIAG", [128, NC8, CK, 128], BF16)
    C.IDF = K.sb("IDF", [128, 128], F32)
    C.IDC = K.sb("IDC", [128, 128], BF16)
    print("conf sbuf top", K.sb_top)
    assert K.sb_top <= 229344


def conf_phase(C, src, src_name, dst, dst_name):
    K, nc = C.K, C.nc
    w1 = C.w_pw1.rearrange("(c p) f -> p c f", p=128)
    w2 = C.w_pw2.rearrange("(c p) f -> p c f", p=128)
    for c in range(NC8):
        K.dma("pool", C.W1[:, c, :], K.dview(w1[:, c, :], "d_w", 0, 1))
    for c in range(NC8):
        K.dma("pool", C.W2[:, c, :], K.dview(w2[:, c, :], "d_w", 0, 1))
    vb1 = VOFF["conf_b_pw1"]
    vcb = VOFF["conf_conv_b"]
    vg, vb = VOFF["conf_ln_g"], VOFF["conf_ln_b"]
    vb2 = VOFF["conf_b_pw2"]
    vcw = VOFF["conf_conv_w"]
    vnorm = VOFF["mix_norm"] + 8
    stores = []
    nt = C.ntiles
    K.dma("sp", C.IDF[:, :], K.dview(C.cst[:, 256:384], "d_cst", 0, 1))
    K.copy(C.IDC[:, :], C.IDF[:, :])
    for c in range(NC8):
        for j in range(CK):
            K.ts(C.DIAG[:, c, j, :], C.IDC[:, :], C.VEC[:, vcw + c * CK + j:vcw + c * CK + j + 1], None, ALU.mult)
    K.dma("sp", C.X[0][:, :, :], dram_tile(C, src, src_name, 0))
    U = C.U
    for i in range(nt):
        xt = C.X[i % 2]
        if i + 1 < nt:
            K.dma("sp", C.X[(i + 1) % 2][:, :, :], dram_tile(C, src, src_name, i + 1))
        rmsnorm(C, xt, vnorm, C.HN)
        if i == 0:
            K.memset(U[:, :, 0:HALO], 0.0)
        else:
            for c in range(NC8):
                K.copy(U[:, c, 0:HALO], U[:, c, NT:NT + HALO], eng="pool")
        for c in range(NC8):
            pa = C.PS[c % 2][:, :]
            pg = C.PS[2 + c % 2][:, :]
            for k in range(NC8):
                K.mm(pa, C.W1[:, k, c * 128:(c + 1) * 128], C.HN[:, k, :], start=(k == 0), stop=(k == NC8 - 1))
            for k in range(NC8):
                K.mm(pg, C.W1[:, k, D + c * 128:D + (c + 1) * 128], C.HN[:, k, :], start=(k == 0), stop=(k == NC8 - 1))
            sg = C.SG[c % 2]
            K.act(sg[:, :], pg, AF.Sigmoid, bias=C.VEC[:, vb1 + 8 + c:vb1 + 9 + c])
            K.stt(U[:, c, HALO:HALO + NT], pa, C.VEC[:, vb1 + c:vb1 + c + 1], sg[:, :], ALU.add, ALU.mult)
        for c in range(NC8):
            pc = C.PS[4 + c % 2][:, :]
            for j in range(CK):
                K.mm(pc, C.DIAG[:, c, j, :], U[:, c, j:j + NT], start=(j == 0), stop=(j == CK - 1))
            K.ts(C.Y[:, c, :], pc, C.VEC[:, vcb + c:vcb + c + 1], None, ALU.add)
        K.copy(C.A1[:, :], C.Y[:, 0, :])
        K.tt(C.A2[:, :], C.Y[:, 0, :], C.Y[:, 0, :], ALU.mult)
        for c in range(1, NC8):
            tmp = C.TMP[c % 2]
            K.tt(tmp[:, :], C.Y[:, c, :], C.Y[:, c, :], ALU.mult, eng="pool")
            K.tt(C.A1[:, :], C.A1[:, :], C.Y[:, c, :], ALU.add)
            K.tt(C.A2[:, :], C.A2[:, :], tmp[:, :], ALU.add)
        p1 = C.PS[6][:, :]
        p2 = C.PS[7][:, :]
        K.mm(p1, C.ONES[:, :], C.A1[:, :])
        K.mm(p2, C.ONES[:, :], C.A2[:, :])
        K.ts(C.MEAN[:, :], p1, 1.0 / D, None, ALU.mult)
        K.tt(C.A1[:, :], C.MEAN[:, :], C.MEAN[:, :], ALU.mult)
        K.stt(C.A2[:, :], p2, 1.0 / D, C.A1[:, :], ALU.mult, ALU.subtract)
        K.ts(C.A2[:, :], C.A2[:, :], EPS, None, ALU.add)
        a2 = C.A2[:, :]
        K.op("dve", lambda: nc.vector.reciprocal(a2.ap, a2.ap), reads=[a2], writes=[a2])
        K.act(C.RSTD[:, :], a2, AF.Sqrt)
        for c in range(NC8):
            K.tt(C.Y[:, c, :], C.Y[:, c, :], C.MEAN[:, :], ALU.subtract)
            K.tt(C.Y[:, c, :], C.Y[:, c, :], C.RSTD[:, :], ALU.mult)
            K.act(C.V[:, c, :], C.Y[:, c, :], AF.Silu, bias=C.VEC[:, vb + c:vb + c + 1],
                  scale=C.VEC[:, vg + c:vg + c + 1])
        for m in range(NC8):
            po = C.PS[4 + m % 2][:, :]
            for k in range(NC8):
                K.mm(po, C.W2[:, k, m * 128:(m + 1) * 128], C.V[:, k, :], start=(k == 0), stop=(k == NC8 - 1))
            K.stt(xt[:, m, :], po, C.VEC[:, vb2 + m:vb2 + m + 1], xt[:, m, :], ALU.add, ALU.add)
        stores.append(K.dma("act", dram_tile(C, dst, dst_name, i), xt[:, :, :]))
    return stores


NH = 256
HCOLS_A = 1224
CQ, CK2, CVW, CQI, CKI = 0, 512, 640, 712, 1096
GU0 = 936
N_ITER = 18
TOPK = 256
NEG = -30000.0


def hyb_consts(S):
    theta = np.float32(500000.0)
    t = np.arange(S, dtype=np.float32)

    def tabs(half, period, nrep):
        inv = (theta ** (-(np.arange(half, dtype=np.float32) / np.float32(half)))).astype(np.float32)
        ang = (t[:, None] * inv[None, :]).astype(np.float32)
        c, s = np.cos(ang).astype(np.float32).T, np.sin(ang).astype(np.float32).T
        CC = np.ones((128, S), np.float32)
        SS = np.zeros((128, S), np.float32)
        for base in range(0, 128, period):
            CC[base:base + half] = c
            CC[base + half:base + 2 * half] = c
            SS[base:base + half] = -s
            SS[base + half:base + 2 * half] = s
        return CC, SS

    CCa, SSa = tabs(8, 64, 2)
    CCi, SSi = tabs(4, 32, 4)
    tab = np.stack([CCa, SSa, CCi, SSi], 0)

    def perm(half, period):
        P = np.zeros((128, 128), np.float32)
        for m in range(128):
            r = m % period
            if r < half:
                P[m + half, m] = 1.0
            elif r < 2 * half:
                P[m - half, m] = 1.0
        return P

    cst = np.zeros((128, 128 + 128 + 512 + 128), np.float32)
    cst[:, 0:128] = perm(8, 64)
    cst[:, 128:256] = perm(4, 32)
    cst[:, 256:768] = np.tile(np.eye(128, dtype=np.float32), (1, 4))
    ii = np.arange(128)
    cst[:, 768:896] = np.where(ii[None, :] <= ii[:, None], 0.0, -1e30).astype(np.float32)
    return tab, cst


def pack_hyb_w(w_in):
    w = np.asarray(w_in, np.float32)
    q = w[:, 0:512]
    k = w[:, 512:576]
    v = w[:, 576:640]
    qi = w[:, 640:896]
    ki = w[:, 896:928]
    wi = w[:, 928:936]
    qig = []
    for g in range(3):
        hs = [3 * g, 3 * g + 1, 3 * g + 2]
        cols = [qi[:, 32 * h:32 * h + 32] if h < 8 else qi[:, 0:32] for h in hs] + [qi[:, 0:32]]
        qig.append(np.concatenate(cols, 1))
    out = np.concatenate([q, k, k, v, wi] + qig + [ki, ki, ki, ki], 1)
    assert out.shape[1] == HCOLS_A
    return np.ascontiguousarray(out)


def hyb_alloc(C):
    K = C.K
    S = C.S
    o = C.X[0].off
    C.XH = [K.sb(f"XH{i}", [128, NC8, NH], F32, offset=o + i * 8192) for i in range(2)]
    C.TAB = K.sb("TAB", [128, 4, NH], F32, offset=o + 16384)
    C.YAT = K.sb("YAT", [128, 8, NH], BF16, offset=o + 20480)
    C.QT = [K.sb(f"QT{i}", [128, 4, NH], BF16, offset=o + 24576 + i * 2048) for i in range(2)]
    C.YC = [K.sb(f"YC{i}", [128, 4, NH], BF16, offset=o + 28672 + i * 2048) for i in range(2)]
    o = C.TMP[0].off
    C.TMPH = [K.sb(f"TMPH{i}", [128, NH], F32, offset=o + i * 1024) for i in range(2)]
    C.ACCH = K.sb("ACCH", [128, NH], F32, offset=o + 2048)
    C.RSTDH = K.sb("RSTDH", [128, NH], F32, offset=o + 3072)
    C.HNH = K.sb("HNH", [128, NC8, NH], BF16, offset=o + 4096)
    C.RR = [K.sb(f"RR{i}", [128, 512], BF16, offset=o + 8192 + i * 1024) for i in range(4)]
    C.PT = [K.sb(f"PT{i}", [128, 512], BF16, offset=o + 12288 + i * 1024) for i in range(2)]
    C.T1 = K.sb("T1", [128, NH], F32, offset=o + 14336)
    assert o + 16384 <= C.arena
    K.sb_top = C.arena
    C.WINR = K.sb("WINR", [128, NC8, HCOLS_A], BF16)
    C.WOA = K.sb("WOA", [128, 8, D], BF16)
    C.WOC = K.sb("WOC", [128, 4, D], BF16)
    C.KT2 = K.sb("KT2", [128, S], BF16)
    C.KI3 = K.sb("KI3", [128, S], BF16)
    C.VA = K.sb("VA", [128, S // 128, 65], BF16)
    C.SS_ = K.sb("SS_", [128, 8192], F32)
    so = C.SS_.off
    C.WGU = K.sb("WGU", [128, NC8, 1536], BF16, offset=so)
    C.PBUF = K.sb("PBUF", [128, 4, NH + 2], F32, offset=so + 24576)
    C.GCS = K.sb("GCS", [128, NH], F32, offset=so + 24576 + 4160)
    C.CV = K.sb("CV", [128, NH], F32, offset=so + 24576 + 4160 + 1024)
    C.QF = K.sb("QF", [128, NH], F32, offset=so + 24576 + 4160 + 2048)
    assert 24576 + 4160 + 3072 <= 32768
    C.NEGM = K.sb("NEGM", [128, 8192], BF16)
    C.QIT = K.sb("QIT", [128, 3, NH], BF16)
    C.DG = K.sb("DG", [128, 8, 128], BF16)
    C.RDEN = K.sb("RDEN", [128, 1024], F32)
    C.RB = [K.sb(f"RB{i}", [128, 512], F32) for i in range(2)]
    C.HALO = K.sb("HALO", [128, 4, 2], F32)
    C.WI = K.sb("WI", [128, 2, 8], F32)
    C.WA = K.sb("WA", [128, 2, 8], F32)
    C.SGN = K.sb("SGN", [128, 2, 8], F32)
    C.ST = K.sb("ST", [128, 8], F32)
    C.CST = K.sb("CST", [128, 896], F32)
    C.IDB = K.sb("IDB", [128, 512], BF16)
    print("hyb sbuf top", K.sb_top)
    assert K.sb_top <= 229344


def rmsnorm_h(C, xt, vcol):
    K = C.K
    nc = C.nc
    K.tt(C.ACCH[:, :], xt[:, 0, :], xt[:, 0, :], ALU.mult)
    for c in range(1, NC8):
        tmp = C.TMPH[c % 2]
        K.tt(tmp[:, :], xt[:, c, :], xt[:, c, :], ALU.mult, eng="pool")
        K.tt(C.ACCH[:, :], C.ACCH[:, :], tmp[:, :], ALU.add)
    ps = C.PS[6][:, 0:NH]
    K.mm(ps, C.ONES[:, :], C.ACCH[:, :])
    rs = C.RSTDH[:, :]
    K.ts(rs, ps, 1.0 / D, EPS, ALU.mult, ALU.add)
    K.op("dve", lambda: nc.vector.reciprocal(rs.ap, rs.ap), reads=[rs], writes=[rs])
    K.act(rs, rs, AF.Sqrt)
    for c in range(NC8):
        K.stt(C.HNH[:, c, :], xt[:, c, :], C.VEC[:, vcol + c:vcol + c + 1], rs, ALU.mult, ALU.mult)


def hyb_phase(C, src, src_name, dst, dst_name):
    K, nc = C.K, C.nc
    S = C.S
    nht = S // NH
    nblk = S // 128
    wa = C.w_hyb_a.rearrange("(c p) f -> p c f", p=128)
    for c in range(NC8):
        K.dma("pool", C.WINR[:, c, :], K.dview(wa[:, c, :], "d_w", 0, 1))
    woa = C.w_out[0:512, :].rearrange("(h p) m -> p h m", p=64)
    woc = C.w_out[512:1024, :].rearrange("(c p) m -> p c m", p=128)
    for h in range(8):
        K.dma("pool", C.WOA[0:64, h, :], K.dview(woa[:, h, :], "d_w", 0, 1))
    for c in range(4):
        K.dma("pool", C.WOC[:, c, :], K.dview(woc[:, c, :], "d_w", 0, 1))
    K.dma("sp", C.CST[:, :], K.dview(C.cst, "d_cst", 0, 1))
    K.copy(C.IDB[:, :], C.CST[:, 256:768])
    K.memset(C.VA[:, :, 64:65], 1.0)
    K.memset(C.HALO[:, :, :], 0.0)
    PMA = C.CST[:, 0:128]
    PMI = C.CST[:, 128:256]
    CAUS = C.CST[:, 768:896]
    wgu = C.w_in[:, GU0:GU0 + 1536].rearrange("(c p) f -> p c f", p=128)
    vnorm = VOFF["mix_norm"]
    vcw = VOFF["hyb_conv_w"]
    stores = []

    def rope(ps, ci, si, PM, out):
        K.copy(C.QF[:, :], ps, eng="act")
        ps2 = C.PS[4 + rope.n % 2][:, 0:NH]
        rope.n += 1
        K.mm(ps2, PM, C.QF[:, :])
        K.tt(C.T1[:, :], C.QF[:, :], C.TAB[:, ci, :], ALU.mult)
        K.tt(C.QF[:, :], ps2, C.TAB[:, si, :], ALU.mult)
        K.tt(out, C.T1[:, :], C.QF[:, :], ALU.add)
    rope.n = 0

    def proj(i):
        t0 = i * NH
        xt = C.XH[i % 2]
        K.dma("sp", xt[:, :, :], C.K.dview(src[:, t0:t0 + NH].rearrange("(c p) t -> p c t", p=128), src_name, t0, t0 + NH))
        K.dma("sp", C.TAB[:, :, :], K.dview(C.tab[:, :, t0:t0 + NH].rearrange("k p t -> p k t"), "d_tab", 0, 1))
        for c in range(NC8):
            K.dma("pool", C.WGU[:, c, :], K.dview(wgu[:, c, :], "d_w", 0, 1))
        rmsnorm_h(C, xt, vnorm)
        pi = [0]

        def nps(rows=128, cols=NH):
            p = C.PS[pi[0] % 4][0:rows, 0:cols]
            pi[0] += 1
            return p

        def fm(ps, W, col0, ncol):
            for k in range(NC8):
                K.mm(ps, W[:, k, col0:col0 + ncol], C.HNH[:, k, :], start=(k == 0), stop=(k == NC8 - 1))

        QT = C.QT[i % 2]
        for c in range(4):
            ps = nps()
            fm(ps, C.WINR, CQ + c * 128, 128)
            rope(ps, 0, 1, PMA, QT[:, c, :])
        ps = nps()
        fm(ps, C.WINR, CK2, 128)
        rope(ps, 0, 1, PMA, C.KT2[:, t0:t0 + NH])
        for g in range(3):
            ps = nps()
            fm(ps, C.WINR, CQI + g * 128, 128)
            rope(ps, 2, 3, PMI, C.QIT[:, g, :])
        ps = nps()
        fm(ps, C.WINR, CKI, 128)
        rope(ps, 2, 3, PMI, C.KI3[:, t0:t0 + NH])
        for q in range(2):
            ps = nps(128, 72)
            for k in range(NC8):
                K.mm(ps, C.HNH[:, k, q * 128:(q + 1) * 128], C.WINR[:, k, CVW:CVW + 72], start=(k == 0), stop=(k == NC8 - 1))
            ch = (t0 // 128) + q
            K.copy(C.VA[:, ch, 0:64], C.PS[(pi[0] - 1) % 4][:, 0:64])
            K.ts(C.WI[:, q, :], C.PS[(pi[0] - 1) % 4][:, 64:72], 1.0 / 16.0, None, ALU.mult)
            K.ts(C.SGN[:, q, :], C.WI[:, q, :], 0.0, 2.0, ALU.is_ge, ALU.mult)
            K.ts(C.SGN[:, q, :], C.SGN[:, q, :], -1.0, None, ALU.add)
            K.tt(C.WA[:, q, :], C.WI[:, q, :], C.SGN[:, q, :], ALU.mult)
        YC = C.YC[i % 2]
        for c in range(4):
            pgc = nps()
            fm(pgc, C.WGU, 512 + c * 128, 128)
            K.copy(C.GCS[:, :], pgc, eng="act")
            pu = nps()
            fm(pu, C.WGU, 1024 + c * 128, 128)
            K.copy(C.PBUF[:, c, 0:2], C.HALO[:, c, :])
            K.tt(C.PBUF[:, c, 2:2 + NH], pu, C.GCS[:, :], ALU.mult)
            K.copy(C.HALO[:, c, :], C.PBUF[:, c, NH:NH + 2])
            K.ts(C.CV[:, :], C.PBUF[:, c, 2:2 + NH], C.VEC[:, vcw + c * 3 + 2:vcw + c * 3 + 3], None, ALU.mult)
            K.stt(C.CV[:, :], C.PBUF[:, c, 1:1 + NH], C.VEC[:, vcw + c * 3 + 1:vcw + c * 3 + 2], C.CV[:, :], ALU.mult, ALU.add)
            K.stt(C.CV[:, :], C.PBUF[:, c, 0:NH], C.VEC[:, vcw + c * 3:vcw + c * 3 + 1], C.CV[:, :], ALU.mult, ALU.add)
            pgb = nps()
            fm(pgb, C.WGU, c * 128, 128)
            K.tt(YC[:, c, :], pgb, C.CV[:, :], ALU.mult)

    def score(b):
        L = 128 * (b + 1)
        qq = b % 2
        for h in range(8):
            K.ts(C.DG[:, h, :], C.IDB[:, 0:128], C.SGN[:, qq, h:h + 1], None, ALU.mult)
        nkt = (L + 511) // 512
        for kt in range(nkt):
            k0 = 512 * kt
            w = min(512, L - k0)
            pss = C.PS[4 + kt % 2][:, 0:w]
            pend = []
            for h in range(8):
                g, r = divmod(h, 3)
                ps = C.PS[h % 4][:, 0:w]
                K.mm(ps, C.QIT[32 * r:32 * r + 32, g, qq * 128:(qq + 1) * 128], C.KI3[32 * r:32 * r + 32, k0:k0 + w])
                K.act(C.RR[h % 4][:, 0:w], ps, AF.Relu, scale=C.WA[:, qq, h:h + 1])
                pend.append(h)
                if len(pend) > 2:
                    hh = pend.pop(0)
                    K.mm(pss, C.DG[:, hh, :], C.RR[hh % 4][:, 0:w], start=(hh == 0), stop=(hh == 7))
            for hh in pend:
                K.mm(pss, C.DG[:, hh, :], C.RR[hh % 4][:, 0:w], start=(hh == 0), stop=(hh == 7))
            K.copy(C.SS_[:, k0:k0 + w], pss)
        K.tt(C.SS_[:, L - 128:L], C.SS_[:, L - 128:L], CAUS, ALU.add)

    def thresh(b):
        L = 128 * (b + 1)
        LO = C.ST[:, 0:1]
        MID = C.ST[:, 1:2]
        CNT = C.ST[:, 2:3]
        M = C.ST[:, 3:4]
        if b < 2:
            K.memset(LO, -1e29)
        else:
            B = 16.0
            step = B
            K.memset(MID, 0.0)
            junk = View(C.T1.h[:, 0:1].broadcast_to([128, L]), C.T1[:, 0:1].regs)
            for it in range(N_ITER):
                K.ts(junk, C.SS_[:, 0:L], MID, 0.0, ALU.is_ge, ALU.add, accum=CNT)
                K.ts(M, CNT, float(TOPK), step, ALU.is_ge, ALU.mult)
                nstep = step / 2.0
                if it < N_ITER - 1:
                    K.stt(MID, M, -nstep, MID, ALU.add, ALU.add)
                else:
                    K.stt(LO, M, -step, MID, ALU.add, ALU.add)
                step = nstep

    def negm_piece(b, p):
        L = 128 * (b + 1)
        k0 = 512 * p
        w = min(512, L - k0)
        K.ts(C.NEGM[:, k0:k0 + w], C.SS_[:, k0:k0 + w], C.ST[:, 0:1], NEG, ALU.is_lt, ALU.mult)

    def npieces(b):
        return (128 * (b + 1) + 511) // 512

    import os
    dbg = os.environ.get("HYB_DBG", "psta3")

    def attn(b, nb=None):
        qq = b % 2
        QT = C.QT[(b // 2) % 2]
        OT = [C.PS[6], C.PS[7]]
        pdone = 0
        for j in range(b + 1):
            psl = [C.PS[(2 * j) % 4], C.PS[(2 * j + 1) % 4]]
            for hl in range(4):
                for par in range(2):
                    h = 2 * hl + par
                    base = 64 * par
                    K.mm(psl[par][:, hl * 128:(hl + 1) * 128], C.KT2[base:base + 64, 128 * j:128 * j + 128],
                         QT[base:base + 64, hl, qq * 128:(qq + 1) * 128], start=(hl == 0), stop=False)
            for par in range(2):
                K.mm(psl[par][:, :], C.NEGM[:, 128 * j:128 * j + 128], C.IDB[:, :], start=False, stop=True)
            for par in range(2):
                pt = C.PT[par]
                K.act(pt[:, :], psl[par][:, :], AF.Exp, scale=0.125)
                if "2" in dbg or "3" in dbg:
                    K.mm(OT[par][0:65, :], C.VA[:, j, 0:65], pt[:, :], start=(j == 0), stop=(j == b))
            if nb is not None and (j + 1) % 4 == 0:
                negm_piece(nb, pdone)
                pdone += 1
        if nb is not None:
            while pdone < npieces(nb):
                negm_piece(nb, pdone)
                pdone += 1
        if "3" not in dbg:
            return
        for par in range(2):
            rd = C.RDEN[64:65, par * 512:(par + 1) * 512]
            ot = OT[par]
            K.op("dve", (lambda rd=rd, ot=ot: nc.vector.reciprocal(rd.ap, ot[64:65, :].ap)), reads=[ot[64:65, :]], writes=[rd])
            pb = C.PS[4 + par][0:64, :]
            K.mm(pb, C.ONES[64:65, 0:64], rd)
            rb = C.RB[par]
            K.copy(rb[0:64, :], pb, eng="act")
            yv = C.YAT[0:64, 4 * par:4 * par + 4, qq * 128:(qq + 1) * 128]
            otv = View(ot.h[0:64, :].rearrange("p (h t) -> p h t", h=4), ot[0:64, :].regs)
            rbv = View(rb.h[0:64, :].rearrange("p (h t) -> p h t", h=4), rb[0:64, :].regs)
            K.tt(yv, otv, rbv, ALU.mult)

    def outproj(i):
        t0 = i * NH
        xt = C.XH[i % 2]
        YC = C.YC[i % 2]
        for m in range(NC8):
            po = C.PS[m % 4][:, 0:NH]
            for h in range(8):
                K.mm(po, C.WOA[0:64, h, m * 128:(m + 1) * 128], C.YAT[0:64, 4 * (h % 2) + h // 2, :], start=(h == 0), stop=False)
            for c in range(4):
                K.mm(po, C.WOC[:, c, m * 128:(m + 1) * 128], YC[:, c, :], start=False, stop=(c == 3))
            K.tt(xt[:, m, :], xt[:, m, :], po, ALU.add)
        stores.append(K.dma("act", K.dview(dst[:, t0:t0 + NH].rearrange("(c p) t -> p c t", p=128), dst_name, t0, t0 + NH), xt[:, :, :]))

    for i in range(nht):
        proj(i)
        for b in (2 * i, 2 * i + 1):
            score(b)
            thresh(b)
            if b == 0:
                for p in range(npieces(0)):
                    negm_piece(0, p)
            else:
                attn(b - 1, b)
                if (b - 1) % 2 == 1:
                    outproj((b - 1) // 2)
    attn(nblk - 1, None)
    outproj(nht - 1)
    return stores


PHASES = [
    ("ffn", "xT", "d_x", "hA", "d_h", 0, 0, VOFF["ffn_norm"] + 0),
    ("hyb", "hA", "d_h", "hA", "d_h"),
    ("ffn", "hA", "d_h", "hA", "d_h", 0, 1, VOFF["ffn_norm"] + 8),
    ("ffn", "hA", "d_h", "hA", "d_h", 1, 0, VOFF["ffn_norm"] + 16),
    ("conf", "hA", "d_h", "hA", "d_h"),
    ("ffn", "hA", "d_h", "outT", "d_out", 1, 1, VOFF["ffn_norm"] + 24, VOFF["final_norm"]),
]

_CACHE = {}


def host_inputs(inputs, S):
    f = lambda a: np.ascontiguousarray(np.asarray(a, np.float32))
    tab, cst = hyb_consts(S)
    shared = {
        "vecs": pack_vecs(inputs),
        "ffn_w_gate": f(inputs["ffn_w_gate"]), "ffn_w_up": f(inputs["ffn_w_up"]), "ffn_w_down": f(inputs["ffn_w_down"]),
        "conf_w_pw1": f(np.asarray(inputs["conf_w_pw1"])[0]), "conf_w_pw2": f(np.asarray(inputs["conf_w_pw2"])[0]),
        "hyb_w_in": f(np.asarray(inputs["hyb_w_in"])[0]), "w_hyb_a": pack_hyb_w(np.asarray(inputs["hyb_w_in"])[0]),
        "hyb_w_out": f(np.asarray(inputs["hyb_w_out"])[0]), "rope_tab": tab, "hyb_cst": cst,
    }
    return shared


def kernel(**inputs):
    x = np.asarray(inputs["x"], np.float32)
    B, S, _ = x.shape
    if S not in _CACHE:
        _CACHE[S] = build(S, PHASES)
    nc = _CACHE[S]
    shared = host_inputs(inputs, S)
    in_maps = []
    for b in range(B):
        m = dict(shared)
        m["xT"] = np.ascontiguousarray(x[b].T)
        in_maps.append(m)
    res = run_bass_kernel_spmd(nc, in_maps, core_ids=list(range(B)))
    out = np.stack([np.ascontiguousarray(r["outT"].T) for r in res.results], 0)
    return out.astype(np.float32)
```

```python
import numpy as np
import concourse.bass as bass
import concourse.mybir as mybir

F32 = mybir.dt.float32
BF16 = mybir.dt.bfloat16
AF = mybir.ActivationFunctionType
ALU = mybir.AluOpType
AX = mybir.AxisListType

DT_SIZE = {F32: 4, BF16: 2}
SEM_LIMIT = 2000
N_DMA_SEMS = 16


class View:
    __slots__ = ("ap", "regs")

    def __init__(self, ap, regs):
        self.ap = ap
        self.regs = regs


class Space:
    def __init__(self):
        self.b = [0, 1 << 60]
        self.w = [None]
        self.r = [[]]

    def _split(self, x):
        import bisect
        i = bisect.bisect_left(self.b, x)
        if self.b[i] == x:
            return i
        self.b.insert(i, x)
        self.w.insert(i, self.w[i - 1])
        self.r.insert(i, list(self.r[i - 1]))
        return i

    def rng(self, lo, hi):
        i = self._split(lo)
        j = self._split(hi)
        return range(i, j)


class Op:
    __slots__ = ("eng", "fn", "deps", "needed", "ticket", "dma", "idx")

    def __init__(self, eng, fn, dma):
        self.eng = eng
        self.fn = fn
        self.deps = []
        self.needed = False
        self.ticket = None
        self.dma = dma


class SBT:
    def __init__(self, K, name, shape, dtype, offset=None):
        self.K = K
        self.shape = list(shape)
        self.dtype = dtype
        es = DT_SIZE[dtype]
        nfree = int(np.prod(shape[1:]))
        if offset is None:
            offset = K.sb_alloc(nfree * es)
        self.off = offset
        self.es = es
        self.h = K.nc.alloc_sbuf_tensor_at(name, list(shape), dtype, offset=offset)
        st = []
        acc = 1
        for d in reversed(shape[1:]):
            st.append(acc)
            acc *= d
        self.strides = list(reversed(st))

    def __getitem__(self, idx):
        if not isinstance(idx, tuple):
            idx = (idx,)
        idx = tuple(idx) + (slice(None),) * (len(self.shape) - len(idx))
        lo = 0
        hi = 0
        for k, (ix, d) in enumerate(zip(idx[1:], self.shape[1:])):
            s = self.strides[k]
            if isinstance(ix, int):
                a, b = ix, ix + 1
            else:
                a, b, step = ix.indices(d)
                assert step == 1
            lo += a * s
            hi += (b - 1) * s
        hi += 1
        ap = self.h[idx]
        return View(ap, [("sb", self.off + lo * self.es, self.off + hi * self.es)])


class PST:
    def __init__(self, K, name, bank, handle=None, coff=0):
        self.K = K
        self.bank = bank
        self.coff = coff
        self.h = handle if handle is not None else K.nc.alloc_psum_tensor(name, [128, 512], F32)

    def __getitem__(self, idx):
        if not isinstance(idx, tuple):
            idx = (idx,)
        idx = tuple(idx) + (slice(None),) * (2 - len(idx))
        a, b, _ = idx[1].indices(512)
        return View(self.h[idx[0], self.coff + a:self.coff + b], [("ps", self.bank * 2048, self.bank * 2048 + 2048)])


class Kern:
    def __init__(self, nc):
        self.nc = nc
        self.ops = []
        self.spaces = {}
        self.sb_top = 16512
        self.eng = {"pe": nc.tensor, "act": nc.scalar, "dve": nc.vector,
                    "pool": nc.gpsimd, "sp": nc.sync}

    def sb_alloc(self, nbytes):
        off = (self.sb_top + 63) // 64 * 64
        self.sb_top = off + nbytes
        return off

    def sb(self, name, shape, dtype, offset=None):
        return SBT(self, name, shape, dtype, offset)

    def dview(self, ap, space, lo, hi):
        return View(ap, [(space, lo, hi)])

    def _sp(self, name):
        if name not in self.spaces:
            self.spaces[name] = Space()
        return self.spaces[name]

    def op(self, eng, fn, reads=(), writes=(), dma=False):
        o = Op(eng, fn, dma)
        o.idx = len(self.ops)
        deps = {}

        def add(d, raw):
            if d is None:
                return
            if not d.dma and not o.dma and d.eng == eng:
                if eng == "pe" or not raw:
                    return
            deps[d.idx] = d

        for v in reads:
            for (s, lo, hi) in v.regs:
                sp = self._sp(s)
                for i in sp.rng(lo, hi):
                    add(sp.w[i], True)
        for v in writes:
            for (s, lo, hi) in v.regs:
                sp = self._sp(s)
                for i in sp.rng(lo, hi):
                    add(sp.w[i], False)
                    for r in sp.r[i]:
                        add(r, False)
        for v in reads:
            for (s, lo, hi) in v.regs:
                sp = self._sp(s)
                for i in sp.rng(lo, hi):
                    sp.r[i].append(o)
        for v in writes:
            for (s, lo, hi) in v.regs:
                sp = self._sp(s)
                for i in sp.rng(lo, hi):
                    sp.w[i] = o
                    sp.r[i] = []
        o.deps = list(deps.values())
        for d in o.deps:
            d.needed = True
        self.ops.append(o)
        return o

    def mm(self, out, lhsT, rhs, start=True, stop=True):
        nc = self.nc
        return self.op("pe", lambda: nc.tensor.matmul(out.ap, lhsT.ap, rhs.ap, start=start, stop=stop),
                       reads=[lhsT, rhs], writes=[out])

    def act(self, out, in_, func, bias=None, scale=None, accum=None, eng="act"):
        nc = self.nc
        kw = {}
        rd = [in_]
        wr = [out]
        if bias is not None:
            if isinstance(bias, View):
                kw["bias"] = bias.ap
                rd.append(bias)
            else:
                kw["bias"] = bias
        if scale is not None:
            if isinstance(scale, View):
                kw["scale"] = scale.ap
                rd.append(scale)
            else:
                kw["scale"] = scale
        if accum is not None:
            kw["accum_out"] = accum.ap
            wr.append(accum)
        return self.op("act", lambda: nc.scalar.activation(out.ap, in_.ap, func, **kw), reads=rd, writes=wr)

    def _v(self, eng):
        return self.nc.vector if eng == "dve" else self.nc.gpsimd

    def tt(self, out, in0, in1, op, eng="dve"):
        e = self._v(eng)
        return self.op(eng, lambda: e.tensor_tensor(out.ap, in0.ap, in1.ap, op), reads=[in0, in1], writes=[out])

    def ts(self, out, in0, s1, s2, op0, op1=None, accum=None, eng="dve"):
        e = self._v(eng)
        rd = [in0]
        wr = [out]
        a1 = s1
        a2 = s2
        if isinstance(s1, View):
            rd.append(s1)
            a1 = s1.ap
        if isinstance(s2, View):
            rd.append(s2)
            a2 = s2.ap
        kw = {}
        if op1 is not None:
            kw["op1"] = op1
        if accum is not None:
            kw["accum_out"] = accum.ap
            wr.append(accum)
        return self.op(eng, lambda: e.tensor_scalar(out.ap, in0.ap, a1, a2, op0, **kw), reads=rd, writes=wr)

    def stt(self, out, in0, scalar, in1, op0, op1, eng="dve"):
        e = self._v(eng)
        rd = [in0, in1]
        a = scalar
        if isinstance(scalar, View):
            rd.append(scalar)
            a = scalar.ap
        return self.op(eng, lambda: e.scalar_tensor_tensor(out.ap, in0.ap, a, in1.ap, op0, op1),
                       reads=rd, writes=[out])

    def copy(self, out, in_, eng="dve"):
        if eng == "act":
            nc = self.nc
            return self.op("act", lambda: nc.scalar.copy(out.ap, in_.ap), reads=[in_], writes=[out])
        e = self._v(eng)
        return self.op(eng, lambda: e.tensor_copy(out.ap, in_.ap), reads=[in_], writes=[out])

    def memset(self, out, val, eng="dve"):
        e = self._v(eng)
        return self.op(eng, lambda: e.memset(out.ap, val), writes=[out])

    def dma(self, q, out, in_):
        e = self.eng[q]
        return self.op(q, lambda: e.dma_start(out=out.ap, in_=in_.ap), reads=[in_], writes=[out], dma=True)

    def emit(self, final_waits=()):
        nc = self.nc
        import contextlib
        self._stack = contextlib.ExitStack()
        sems = []

        def new_sem(nm):
            s = self._stack.enter_context(nc.semaphore(nm))
            sems.append(s)
            return s

        cur = {}
        nsem = [0]
        dsem = [[new_sem(f"dq{i}"), 0] for i in range(N_DMA_SEMS)]
        dnext = [0]
        waited = {}

        def wait(engname, sem, val):
            key = (engname, id(sem))
            if waited.get(key, 0) >= val:
                return
            waited[key] = val
            self.eng[engname].wait_ge(sem, val)

        nw = 0
        for o in self.ops:
            for d in o.deps:
                sem, val = d.ticket
                wait(o.eng, sem, val)
            if o.dma:
                k = dnext[0] % N_DMA_SEMS
                dnext[0] += 1
                sem, cnt = dsem[k]
                if cnt:
                    wait(o.eng, sem, cnt)
                ins = o.fn()
                ins.then_inc(sem, 16)
                dsem[k][1] = cnt + 16
                o.ticket = (sem, cnt + 16)
            else:
                ins = o.fn()
                if o.needed:
                    c = cur.get(o.eng)
                    if c is None or c[1] >= SEM_LIMIT:
                        nsem[0] += 1
                        c = [new_sem(f"s_{o.eng}{nsem[0]}"), 0]
                        cur[o.eng] = c
                    c[1] += 1
                    ins.then_inc(c[0], 1)
                    o.ticket = (c[0], c[1])
        for d in final_waits:
            sem, val = d.ticket
            wait("sp", sem, val)
        self.n_sems = len(sems)
        return len(self.ops)

from concourse.bass_utils import run_bass_kernel_spmd

D = 1024
DFF = 2816
NF = DFF // 128
NC8 = D // 128
NT = 512
EPS = 1e-6


class Ctx:
    pass


def build(S, phases, n_vec_cols=None):
    if n_vec_cols is None:
        n_vec_cols = NVEC
    nc = bass.Bass("TRN2", target_bir_lowering=False)
    K = Kern(nc)
    C = Ctx()
    C.nc, C.K, C.S = nc, K, S
    C.ntiles = S // NT

    def din(name, shape, dt=F32):
        return nc.dram_tensor(name, list(shape), dt, kind="ExternalInput").ap()

    C.xT = din("xT", [D, S])
    C.vecs = din("vecs", [128, n_vec_cols])
    C.wg = din("ffn_w_gate", [2, 2, D, DFF])
    C.wu = din("ffn_w_up", [2, 2, D, DFF])
    C.wd = din("ffn_w_down", [2, 2, DFF, D])
    C.w_pw1 = din("conf_w_pw1", [D, 2 * D])
    C.w_pw2 = din("conf_w_pw2", [D, D])
    C.w_in = din("hyb_w_in", [D, 2472])
    C.w_hyb_a = din("w_hyb_a", [D, HCOLS_A])
    C.w_out = din("hyb_w_out", [D, D])
    C.tab = din("rope_tab", [4, 128, S])
    C.cst = din("hyb_cst", [128, 896])
    C.outT = nc.dram_tensor("outT", [D, S], F32, kind="ExternalOutput").ap()
    C.hA = nc.dram_tensor("hA", [D, S], F32, kind="Internal").ap()

    C.VEC = K.sb("VEC", [128, n_vec_cols], F32)
    C.ONES = K.sb("ONES", [128, 128], F32)
    C.X = [K.sb(f"X{i}", [128, NC8, NT], F32) for i in range(2)]
    C.TMP = [K.sb(f"TMP{i}", [128, NT], F32) for i in range(2)]
    C.ACC = K.sb("ACC", [128, NT], F32)
    C.RSTD = K.sb("RSTD", [128, NT], F32)
    C.HN = K.sb("HN", [128, NC8, NT], BF16)
    C.arena = K.sb_top
    C.PSP = [nc.alloc_psum_tensor(f"PSP{i}", [128, 1024], F32) for i in range(4)]
    C.PS = [PST(K, f"PS{i}", i, C.PSP[i // 2], (i % 2) * 512) for i in range(8)]

    K.dma("sp", C.VEC[:, :], K.dview(C.vecs, "d_vecs", 0, 1))
    K.memset(C.ONES[:, :], 1.0)

    ffn_alloc(C)
    conf_alloc(C)
    hyb_alloc(C)
    last = None
    for ph in phases:
        if ph[0] == "ffn":
            last = ffn_phase(C, getattr(C, ph[1]), ph[2], getattr(C, ph[3]), *ph[4:])
        elif ph[0] == "hyb":
            last = hyb_phase(C, getattr(C, ph[1]), ph[2], getattr(C, ph[3]), ph[4])
        elif ph[0] == "conf":
            last = conf_phase(C, getattr(C, ph[1]), ph[2], getattr(C, ph[3]), ph[4])
    n = K.emit(final_waits=last)
    print("ops", n, "sems", K.n_sems, "sbuf top", K.sb_top)
    return nc

def ffn_alloc(C):
    K = C.K
    K.sb_top = C.arena
    C.WG = K.sb("WG", [128, NC8, DFF], BF16)
    C.WU = K.sb("WU", [128, NC8, DFF], BF16)
    C.WD = K.sb("WD", [128, NF, D], BF16)
    C.G = K.sb("G", [128, NF, NT], BF16)
    C.SILU = [K.sb(f"SILU{i}", [128, NT], BF16) for i in range(2)]
    C.OUTT = K.sb("OUTT", [128, NC8, NT], F32, offset=C.G.off)


def dram_tile(C, ap, name, i):
    t0 = i * NT
    return C.K.dview(ap[:, t0:t0 + NT].rearrange("(c p) t -> p c t", p=128), name, t0, t0 + NT)


def rmsnorm(C, xt, vcol, out):
    K = C.K
    K.tt(C.ACC[:, :], xt[:, 0, :], xt[:, 0, :], ALU.mult)
    for c in range(1, NC8):
        tmp = C.TMP[c % 2]
        K.tt(tmp[:, :], xt[:, c, :], xt[:, c, :], ALU.mult, eng="pool")
        K.tt(C.ACC[:, :], C.ACC[:, :], tmp[:, :], ALU.add)
    ps = C.PS[6][:, :]
    K.mm(ps, C.ONES[:, :], C.ACC[:, :])
    K.ts(C.RSTD[:, :], ps, 1.0 / D, EPS, ALU.mult, ALU.add)
    nc = C.nc
    rs = C.RSTD[:, :]
    K.op("dve", lambda: nc.vector.reciprocal(rs.ap, rs.ap), reads=[rs], writes=[rs])
    K.act(rs, rs, AF.Sqrt)
    for c in range(NC8):
        K.stt(out[:, c, :], xt[:, c, :], C.VEC[:, vcol + c:vcol + c + 1], C.RSTD[:, :], ALU.mult, ALU.mult)


def ffn_phase(C, src, src_name, dst, dst_name, l, j, vcol, final_vcol=None):
    K, nc = C.K, C.nc
    wg = C.wg[l, j].rearrange("(c p) f -> p c f", p=128)
    wu = C.wu[l, j].rearrange("(c p) f -> p c f", p=128)
    wd = C.wd[l, j].rearrange("(c p) m -> p c m", p=128)
    for c in range(NC8):
        K.dma("pool", C.WG[:, c, :], K.dview(wg[:, c, :], "d_w", 0, 1))
        K.dma("pool", C.WU[:, c, :], K.dview(wu[:, c, :], "d_w", 0, 1))
    for c in range(NF):
        K.dma("pool", C.WD[:, c, :], K.dview(wd[:, c, :], "d_w", 0, 1))
    stores = []
    nt = C.ntiles
    K.dma("sp", C.X[0][:, :, :], dram_tile(C, src, src_name, 0))
    rmsnorm(C, C.X[0], vcol, C.HN)
    for i in range(nt):
        xt = C.X[i % 2]
        if i + 1 < nt:
            K.dma("sp", C.X[(i + 1) % 2][:, :, :], dram_tile(C, src, src_name, i + 1))
        for f in range(NF):
            pg = C.PS[f % 2][:, :]
            pu = C.PS[2 + f % 2][:, :]
            for c in range(NC8):
                K.mm(pg, C.WG[:, c, f * 128:(f + 1) * 128], C.HN[:, c, :], start=(c == 0), stop=(c == NC8 - 1))
            for c in range(NC8):
                K.mm(pu, C.WU[:, c, f * 128:(f + 1) * 128], C.HN[:, c, :], start=(c == 0), stop=(c == NC8 - 1))
            sl = C.SILU[f % 2]
            K.act(sl[:, :], pg, AF.Silu)
            K.tt(C.G[:, f, :], sl[:, :], pu, ALU.mult)
        if i + 1 < nt:
            rmsnorm(C, C.X[(i + 1) % 2], vcol, C.HN)
        for m in range(NC8):
            po = C.PS[4 + m % 2][:, :]
            for f in range(NF):
                K.mm(po, C.WD[:, f, m * 128:(m + 1) * 128], C.G[:, f, :], start=(f == 0), stop=(f == NF - 1))
            K.stt(xt[:, m, :], po, 0.5, xt[:, m, :], ALU.mult, ALU.add)
        if final_vcol is None:
            stores.append(K.dma("act", dram_tile(C, dst, dst_name, i), xt[:, :, :]))
        else:
            rmsnorm(C, xt, final_vcol, C.OUTT)
            stores.append(K.dma("act", dram_tile(C, dst, dst_name, i), C.OUTT[:, :, :]))
    return stores


VEC_SPEC = [("ffn_norm", 4 * 8), ("mix_norm", 2 * 8), ("final_norm", 8), ("conf_b_pw1", 16), ("conf_conv_b", 8),
            ("conf_ln_g", 8), ("conf_ln_b", 8), ("conf_b_pw2", 8), ("conf_conv_w", 8 * 31), ("hyb_conv_w", 4 * 3)]
VOFF = {}
_o = 0
for _n, _c in VEC_SPEC:
    VOFF[_n] = _o
    _o += _c
NVEC = _o


def pack_vecs(inp):
    v = np.zeros((128, NVEC), np.float32)

    def put(name, arr):
        a = np.asarray(arr, np.float32).reshape(-1)
        n = a.size // 128
        v[:, VOFF[name]:VOFF[name] + n] = a.reshape(n, 128).T

    put("ffn_norm", inp["ffn_norm"])
    put("mix_norm", inp["mix_norm"])
    put("final_norm", inp["final_norm"])
    put("conf_b_pw1", inp["conf_b_pw1"])
    put("conf_conv_b", inp["conf_conv_b"])
    put("conf_ln_g", inp["conf_ln_g"])
    put("conf_ln_b", inp["conf_ln_b"])
    put("conf_b_pw2", inp["conf_b_pw2"])
    cw = np.asarray(inp["conf_conv_w"], np.float32).reshape(31, 8, 128)
    v[:, VOFF["conf_conv_w"]:VOFF["conf_conv_w"] + 248] = cw.transpose(2, 1, 0).reshape(128, 248)
    hw = np.asarray(inp["hyb_conv_w"], np.float32).reshape(3, 4, 128)
    v[:, VOFF["hyb_conv_w"]:VOFF["hyb_conv_w"] + 12] = hw.transpose(2, 1, 0).reshape(128, 12)
    return v


CK = 31
HALO = CK - 1
CONV_POOL_CHUNKS = 0


def conf_alloc(C):
    K = C.K
    K.sb_top = C.arena
    C.W1 = K.sb("W1", [128, NC8, 2 * D], BF16)
    C.W2 = K.sb("W2", [128, NC8, D], BF16)
    C.U = K.sb("U", [128, NC8, HALO + NT], BF16)
    C.Y = K.sb("Y", [128, NC8, NT], F32)
    C.V = K.sb("V", [128, NC8, NT], BF16)
    C.SG = [K.sb(f"SG{i}", [128, NT], F32) for i in range(2)]
    C.A1 = K.sb("A1", [128, NT], F32)
    C.A2 = K.sb("A2", [128, NT], F32)
    C.MEAN = K.sb("MEAN", [128, NT], F32)
    C.DIAG = K.sb("DIAG", [128, NC8, CK, 128], BF16)
    C.IDF = K.sb("IDF", [128, 128], F32)
    C.IDC = K.sb("IDC", [128, 128], BF16)
    print("conf sbuf top", K.sb_top)
    assert K.sb_top <= 229344


def conf_phase(C, src, src_name, dst, dst_name):
    K, nc = C.K, C.nc
    w1 = C.w_pw1.rearrange("(c p) f -> p c f", p=128)
    w2 = C.w_pw2.rearrange("(c p) f -> p c f", p=128)
    for c in range(NC8):
        K.dma("pool", C.W1[:, c, :], K.dview(w1[:, c, :], "d_w", 0, 1))
    for c in range(NC8):
        K.dma("pool", C.W2[:, c, :], K.dview(w2[:, c, :], "d_w", 0, 1))
    vb1 = VOFF["conf_b_pw1"]
    vcb = VOFF["conf_conv_b"]
    vg, vb = VOFF["conf_ln_g"], VOFF["conf_ln_b"]
    vb2 = VOFF["conf_b_pw2"]
    vcw = VOFF["conf_conv_w"]
    vnorm = VOFF["mix_norm"] + 8
    stores = []
    nt = C.ntiles
    K.dma("sp", C.IDF[:, :], K.dview(C.cst[:, 256:384], "d_cst", 0, 1))
    K.copy(C.IDC[:, :], C.IDF[:, :])
    for c in range(NC8):
        for j in range(CK):
            K.ts(C.DIAG[:, c, j, :], C.IDC[:, :], C.VEC[:, vcw + c * CK + j:vcw + c * CK + j + 1], None, ALU.mult)
    K.dma("sp", C.X[0][:, :, :], dram_tile(C, src, src_name, 0))
    U = C.U
    for i in range(nt):
        xt = C.X[i % 2]
        if i + 1 < nt:
            K.dma("sp", C.X[(i + 1) % 2][:, :, :], dram_tile(C, src, src_name, i + 1))
        rmsnorm(C, xt, vnorm, C.HN)
        if i == 0:
            K.memset(U[:, :, 0:HALO], 0.0)
        else:
            for c in range(NC8):
                K.copy(U[:, c, 0:HALO], U[:, c, NT:NT + HALO], eng="pool")
        for c in range(NC8):
            pa = C.PS[c % 2][:, :]
            pg = C.PS[2 + c % 2][:, :]
            for k in range(NC8):
                K.mm(pa, C.W1[:, k, c * 128:(c + 1) * 128], C.HN[:, k, :], start=(k == 0), stop=(k == NC8 - 1))
            for k in range(NC8):
                K.mm(pg, C.W1[:, k, D + c * 128:D + (c + 1) * 128], C.HN[:, k, :], start=(k == 0), stop=(k == NC8 - 1))
            sg = C.SG[c % 2]
            K.act(sg[:, :], pg, AF.Sigmoid, bias=C.VEC[:, vb1 + 8 + c:vb1 + 9 + c])
            K.stt(U[:, c, HALO:HALO + NT], pa, C.VEC[:, vb1 + c:vb1 + c + 1], sg[:, :], ALU.add, ALU.mult)
        for c in range(NC8):
            pc = C.PS[4 + c % 2][:, :]
            for j in range(CK):
                K.mm(pc, C.DIAG[:, c, j, :], U[:, c, j:j + NT], start=(j == 0), stop=(j == CK - 1))
            K.ts(C.Y[:, c, :], pc, C.VEC[:, vcb + c:vcb + c + 1], None, ALU.add)
        K.copy(C.A1[:, :], C.Y[:, 0, :])
        K.tt(C.A2[:, :], C.Y[:, 0, :], C.Y[:, 0, :], ALU.mult)
        for c in range(1, NC8):
            tmp = C.TMP[c % 2]
            K.tt(tmp[:, :], C.Y[:, c, :], C.Y[:, c, :], ALU.mult, eng="pool")
            K.tt(C.A1[:, :], C.A1[:, :], C.Y[:, c, :], ALU.add)
            K.tt(C.A2[:, :], C.A2[:, :], tmp[:, :], ALU.add)
        p1 = C.PS[6][:, :]
        p2 = C.PS[7][:, :]
        K.mm(p1, C.ONES[:, :], C.A1[:, :])
        K.mm(p2, C.ONES[:, :], C.A2[:, :])
        K.ts(C.MEAN[:, :], p1, 1.0 / D, None, ALU.mult)
        K.tt(C.A1[:, :], C.MEAN[:, :], C.MEAN[:, :], ALU.mult)
        K.stt(C.A2[:, :], p2, 1.0 / D, C.A1[:, :], ALU.mult, ALU.subtract)
        K.ts(C.A2[:, :], C.A2[:, :], EPS, None, ALU.add)
        a2 = C.A2[:, :]
        K.op("dve", lambda: nc.vector.reciprocal(a2.ap, a2.ap), reads=[a2], writes=[a2])
        K.act(C.RSTD[:, :], a2, AF.Sqrt)
        for c in range(NC8):
            K.tt(C.Y[:, c, :], C.Y[:, c, :], C.MEAN[:, :], ALU.subtract)
            K.tt(C.Y[:, c, :], C.Y[:, c, :], C.RSTD[:, :], ALU.mult)
            K.act(C.V[:, c, :], C.Y[:, c, :], AF.Silu, bias=C.VEC[:, vb + c:vb + c + 1],
                  scale=C.VEC[:, vg + c:vg + c + 1])
        for m in range(NC8):
            po = C.PS[4 + m % 2][:, :]
            for k in range(NC8):
                K.mm(po, C.W2[:, k, m * 128:(m + 1) * 128], C.V[:, k, :], start=(k == 0), stop=(k == NC8 - 1))
            K.stt(xt[:, m, :], po, C.VEC[:, vb2 + m:vb2 + m + 1], xt[:, m, :], ALU.add, ALU.add)
        stores.append(K.dma("act", dram_tile(C, dst, dst_name, i), xt[:, :, :]))
    return stores


NH = 256
HCOLS_A = 1224
CQ, CK2, CVW, CQI, CKI = 0, 512, 640, 712, 1096
GU0 = 936
N_ITER = 17
THR_B = 8.0
ACT_FRAC = 0.4
DVE_RELU_HEADS = (1, 4, 6)
TOPK = 256
NEG = -30000.0


def hyb_consts(S):
    theta = np.float32(500000.0)
    t = np.arange(S, dtype=np.float32)

    def tabs(half, period, nrep):
        inv = (theta ** (-(np.arange(half, dtype=np.float32) / np.float32(half)))).astype(np.float32)
        ang = (t[:, None] * inv[None, :]).astype(np.float32)
        c, s = np.cos(ang).astype(np.float32).T, np.sin(ang).astype(np.float32).T
        CC = np.ones((128, S), np.float32)
        SS = np.zeros((128, S), np.float32)
        for base in range(0, 128, period):
            CC[base:base + half] = c
            CC[base + half:base + 2 * half] = c
            SS[base:base + half] = -s
            SS[base + half:base + 2 * half] = s
        return CC, SS

    CCa, SSa = tabs(8, 64, 2)
    CCi, SSi = tabs(4, 32, 4)
    tab = np.stack([CCa, SSa, CCi, SSi], 0)

    def perm(half, period):
        P = np.zeros((128, 128), np.float32)
        for m in range(128):
            r = m % period
            if r < half:
                P[m + half, m] = 1.0
            elif r < 2 * half:
                P[m - half, m] = 1.0
        return P

    cst = np.zeros((128, 128 + 128 + 512 + 128), np.float32)
    cst[:, 0:128] = perm(8, 64)
    cst[:, 128:256] = perm(4, 32)
    cst[:, 256:768] = np.tile(np.eye(128, dtype=np.float32), (1, 4))
    ii = np.arange(128)
    cst[:, 768:896] = np.where(ii[None, :] <= ii[:, None], 0.0, -1e30).astype(np.float32)
    return tab, cst


def pack_hyb_w(w_in):
    w = np.asarray(w_in, np.float32)
    q = w[:, 0:512]
    k = w[:, 512:576]
    v = w[:, 576:640]
    qi = w[:, 640:896]
    ki = w[:, 896:928]
    wi = w[:, 928:936]
    qig = []
    for g in range(3):
        hs = [3 * g, 3 * g + 1, 3 * g + 2]
        cols = [qi[:, 32 * h:32 * h + 32] if h < 8 else qi[:, 0:32] for h in hs] + [qi[:, 0:32]]
        qig.append(np.concatenate(cols, 1))
    out = np.concatenate([q, k, k, v, wi] + qig + [ki, ki, ki, ki], 1)
    assert out.shape[1] == HCOLS_A
    return np.ascontiguousarray(out)


def hyb_alloc(C):
    K = C.K
    S = C.S
    o = C.X[0].off
    C.XH = [K.sb(f"XH{i}", [128, NC8, NH], F32, offset=o + i * 8192) for i in range(2)]
    C.TAB = K.sb("TAB", [128, 4, NH], F32, offset=o + 16384)
    C.YAT = K.sb("YAT", [128, 8, NH], BF16, offset=o + 20480)
    C.QT = [K.sb(f"QT{i}", [128, 4, NH], BF16, offset=o + 24576 + i * 2048) for i in range(2)]
    C.YC = [K.sb(f"YC{i}", [128, 4, NH], BF16, offset=o + 28672 + i * 2048) for i in range(2)]
    o = C.TMP[0].off
    C.TMPH = [K.sb(f"TMPH{i}", [128, NH], F32, offset=o + i * 1024) for i in range(2)]
    C.ACCH = K.sb("ACCH", [128, NH], F32, offset=o + 2048)
    C.RSTDH = K.sb("RSTDH", [128, NH], F32, offset=o + 3072)
    C.HNH = K.sb("HNH", [128, NC8, NH], BF16, offset=o + 4096)
    C.RR = [K.sb(f"RR{i}", [128, 512], BF16, offset=o + 8192 + i * 1024) for i in range(4)]
    C.PTF = [K.sb("PTF0", [128, 1024], BF16, offset=o + 12288), None]
    C.T1 = K.sb("T1", [128, NH], F32, offset=o + 14336)
    assert o + 16384 <= C.arena
    K.sb_top = C.arena
    C.WINR = K.sb("WINR", [128, NC8, HCOLS_A], BF16)
    C.WOA = K.sb("WOA", [128, 8, D], BF16)
    C.WOC = K.sb("WOC", [128, 4, D], BF16)
    C.KT2 = K.sb("KT2", [128, S], BF16)
    C.KI3 = K.sb("KI3", [128, S], BF16)
    C.VA = K.sb("VA", [128, S // 128, 65], BF16)
    C.SS_ = K.sb("SS_", [128, 8192], F32)
    so = C.SS_.off
    C.WGU = K.sb("WGU", [128, NC8, 1536], BF16, offset=so)
    C.PBUF = K.sb("PBUF", [128, 4, NH + 2], F32, offset=so + 24576)
    C.GCS = K.sb("GCS", [128, NH], F32, offset=so + 24576 + 4160)
    C.CV = K.sb("CV", [128, NH], F32, offset=so + 24576 + 4160 + 1024)
    C.QF = K.sb("QF", [128, NH], F32, offset=so + 24576 + 4160 + 2048)
    assert 24576 + 4160 + 3072 <= 32768
    C.NEGM = K.sb("NEGM", [128, 8192], BF16)
    C.QIT = K.sb("QIT", [128, 3, NH], BF16)
    C.DG = K.sb("DG", [128, 8, 128], BF16)
    C.RDEN = K.sb("RDEN", [128, 1024], F32)
    C.RB = [K.sb(f"RB{i}", [128, 512], F32) for i in range(2)]
    C.HALO = K.sb("HALO", [128, 4, 2], F32)
    C.WI = K.sb("WI", [128, 2, 8], F32)
    C.WA = K.sb("WA", [128, 2, 8], F32)
    C.SGN = K.sb("SGN", [128, 2, 8], F32)
    C.ST = K.sb("ST", [128, 8], F32)
    C.JA = K.sb("JA", [128, 3328], BF16)
    C.PTF[1] = K.sb("PTF1", [128, 1024], BF16)
    C.CST = K.sb("CST", [128, 896], F32)
    C.IDB = K.sb("IDB", [128, 512], BF16)
    print("hyb sbuf top", K.sb_top)
    assert K.sb_top <= 229344


def rmsnorm_h(C, xt, vcol):
    K = C.K
    nc = C.nc
    K.tt(C.ACCH[:, :], xt[:, 0, :], xt[:, 0, :], ALU.mult)
    for c in range(1, NC8):
        tmp = C.TMPH[c % 2]
        K.tt(tmp[:, :], xt[:, c, :], xt[:, c, :], ALU.mult, eng="pool")
        K.tt(C.ACCH[:, :], C.ACCH[:, :], tmp[:, :], ALU.add)
    ps = C.PS[6][:, 0:NH]
    K.mm(ps, C.ONES[:, :], C.ACCH[:, :])
    rs = C.RSTDH[:, :]
    K.ts(rs, ps, 1.0 / D, EPS, ALU.mult, ALU.add)
    K.op("dve", lambda: nc.vector.reciprocal(rs.ap, rs.ap), reads=[rs], writes=[rs])
    K.act(rs, rs, AF.Sqrt)
    for c in range(NC8):
        K.stt(C.HNH[:, c, :], xt[:, c, :], C.VEC[:, vcol + c:vcol + c + 1], rs, ALU.mult, ALU.mult)


def hyb_phase(C, src, src_name, dst, dst_name):
    K, nc = C.K, C.nc
    S = C.S
    nht = S // NH
    nblk = S // 128
    wa = C.w_hyb_a.rearrange("(c p) f -> p c f", p=128)
    for c in range(NC8):
        K.dma("pool", C.WINR[:, c, :], K.dview(wa[:, c, :], "d_w", 0, 1))
    woa = C.w_out[0:512, :].rearrange("(h p) m -> p h m", p=64)
    woc = C.w_out[512:1024, :].rearrange("(c p) m -> p c m", p=128)
    for h in range(8):
        K.dma("pool", C.WOA[0:64, h, :], K.dview(woa[:, h, :], "d_w", 0, 1))
    for c in range(4):
        K.dma("pool", C.WOC[:, c, :], K.dview(woc[:, c, :], "d_w", 0, 1))
    K.dma("sp", C.CST[:, :], K.dview(C.cst, "d_cst", 0, 1))
    K.copy(C.IDB[:, :], C.CST[:, 256:768])
    K.memset(C.VA[:, :, 64:65], 1.0)
    K.memset(C.HALO[:, :, :], 0.0)
    PMA = C.CST[:, 0:128]
    PMI = C.CST[:, 128:256]
    CAUS = C.CST[:, 768:896]
    wgu = C.w_in[:, GU0:GU0 + 1536].rearrange("(c p) f -> p c f", p=128)
    vnorm = VOFF["mix_norm"]
    vcw = VOFF["hyb_conv_w"]
    stores = []

    def rope(ps, ci, si, PM, out):
        K.copy(C.QF[:, :], ps, eng="act")
        ps2 = C.PS[4 + rope.n % 2][:, 0:NH]
        rope.n += 1
        K.mm(ps2, PM, C.QF[:, :])
        K.tt(C.T1[:, :], C.QF[:, :], C.TAB[:, ci, :], ALU.mult)
        K.tt(C.QF[:, :], ps2, C.TAB[:, si, :], ALU.mult)
        K.tt(out, C.T1[:, :], C.QF[:, :], ALU.add)
    rope.n = 0

    def proj(i):
        t0 = i * NH
        xt = C.XH[i % 2]
        K.dma("sp", xt[:, :, :], C.K.dview(src[:, t0:t0 + NH].rearrange("(c p) t -> p c t", p=128), src_name, t0, t0 + NH))
        K.dma("sp", C.TAB[:, :, :], K.dview(C.tab[:, :, t0:t0 + NH].rearrange("k p t -> p k t"), "d_tab", 0, 1))
        for c in range(NC8):
            K.dma("pool", C.WGU[:, c, :], K.dview(wgu[:, c, :], "d_w", 0, 1))
        rmsnorm_h(C, xt, vnorm)
        pi = [0]

        def nps(rows=128, cols=NH):
            p = C.PS[pi[0] % 4][0:rows, 0:cols]
            pi[0] += 1
            return p

        def fm(ps, W, col0, ncol):
            for k in range(NC8):
                K.mm(ps, W[:, k, col0:col0 + ncol], C.HNH[:, k, :], start=(k == 0), stop=(k == NC8 - 1))

        QT = C.QT[i % 2]
        for c in range(4):
            ps = nps()
            fm(ps, C.WINR, CQ + c * 128, 128)
            rope(ps, 0, 1, PMA, QT[:, c, :])
        ps = nps()
        fm(ps, C.WINR, CK2, 128)
        rope(ps, 0, 1, PMA, C.KT2[:, t0:t0 + NH])
        for g in range(3):
            ps = nps()
            fm(ps, C.WINR, CQI + g * 128, 128)
            rope(ps, 2, 3, PMI, C.QIT[:, g, :])
        ps = nps()
        fm(ps, C.WINR, CKI, 128)
        rope(ps, 2, 3, PMI, C.KI3[:, t0:t0 + NH])
        for q in range(2):
            ps = nps(128, 72)
            for k in range(NC8):
                K.mm(ps, C.HNH[:, k, q * 128:(q + 1) * 128], C.WINR[:, k, CVW:CVW + 72], start=(k == 0), stop=(k == NC8 - 1))
            ch = (t0 // 128) + q
            K.copy(C.VA[:, ch, 0:64], C.PS[(pi[0] - 1) % 4][:, 0:64])
            K.ts(C.WI[:, q, :], C.PS[(pi[0] - 1) % 4][:, 64:72], 1.0 / 16.0, None, ALU.mult)
            K.ts(C.SGN[:, q, :], C.WI[:, q, :], 0.0, 2.0, ALU.is_ge, ALU.mult)
            K.ts(C.SGN[:, q, :], C.SGN[:, q, :], -1.0, None, ALU.add)
            K.tt(C.WA[:, q, :], C.WI[:, q, :], C.SGN[:, q, :], ALU.mult)
        YC = C.YC[i % 2]
        for c in range(4):
            pgc = nps()
            fm(pgc, C.WGU, 512 + c * 128, 128)
            K.copy(C.GCS[:, :], pgc, eng="act")
            pu = nps()
            fm(pu, C.WGU, 1024 + c * 128, 128)
            K.copy(C.PBUF[:, c, 0:2], C.HALO[:, c, :])
            K.tt(C.PBUF[:, c, 2:2 + NH], pu, C.GCS[:, :], ALU.mult)
            K.copy(C.HALO[:, c, :], C.PBUF[:, c, NH:NH + 2])
            K.ts(C.CV[:, :], C.PBUF[:, c, 2:2 + NH], C.VEC[:, vcw + c * 3 + 2:vcw + c * 3 + 3], None, ALU.mult)
            K.stt(C.CV[:, :], C.PBUF[:, c, 1:1 + NH], C.VEC[:, vcw + c * 3 + 1:vcw + c * 3 + 2], C.CV[:, :], ALU.mult, ALU.add)
            K.stt(C.CV[:, :], C.PBUF[:, c, 0:NH], C.VEC[:, vcw + c * 3:vcw + c * 3 + 1], C.CV[:, :], ALU.mult, ALU.add)
            pgb = nps()
            fm(pgb, C.WGU, c * 128, 128)
            K.tt(YC[:, c, :], pgb, C.CV[:, :], ALU.mult)

    def score(b):
        L = 128 * (b + 1)
        qq = b % 2
        for h in range(8):
            K.ts(C.DG[:, h, :], C.IDB[:, 0:128], C.SGN[:, qq, h:h + 1], None, ALU.mult)
        nkt = (L + 511) // 512
        for kt in range(nkt):
            k0 = 512 * kt
            w = min(512, L - k0)
            pss = C.PS[4 + kt % 2][:, 0:w]
            pend = []
            for h in range(8):
                g, r = divmod(h, 3)
                ps = C.PS[h % 4][:, 0:w]
                K.mm(ps, C.QIT[32 * r:32 * r + 32, g, qq * 128:(qq + 1) * 128], C.KI3[32 * r:32 * r + 32, k0:k0 + w])
                if h in DVE_RELU_HEADS:
                    K.ts(C.RR[h % 4][:, 0:w], ps, C.WA[:, qq, h:h + 1], 0.0, ALU.mult, ALU.max)
                else:
                    K.act(C.RR[h % 4][:, 0:w], ps, AF.Relu, scale=C.WA[:, qq, h:h + 1])
                pend.append(h)
                if len(pend) > 2:
                    hh = pend.pop(0)
                    K.mm(pss, C.DG[:, hh, :], C.RR[hh % 4][:, 0:w], start=(hh == 0), stop=(hh == 7))
            for hh in pend:
                K.mm(pss, C.DG[:, hh, :], C.RR[hh % 4][:, 0:w], start=(hh == 0), stop=(hh == 7))
            K.copy(C.SS_[:, k0:k0 + w], pss)
        K.tt(C.SS_[:, L - 128:L], C.SS_[:, L - 128:L], CAUS, ALU.add)

    def search_iters(b):
        L = 128 * (b + 1)
        LO = C.ST[:, 0:1]
        MID = C.ST[:, 1:2]
        CNTD = C.ST[:, 2:3]
        M = C.ST[:, 3:4]
        SGA = C.ST[:, 4:5]
        X = C.ST[:, 5:6]
        if b < 2:
            K.memset(LO, -1e29)
            return []
        L2 = 128 * int(ACT_FRAC * (b + 1))
        L1 = L - L2
        thr = float(TOPK) - 0.5 * L2
        K.memset(MID, 0.0)
        junk = View(C.T1.h[:, 0:1].broadcast_to([128, L1]), C.T1[:, 0:1].regs)
        its = []
        step = THR_B
        for it in range(N_ITER):
            nstep = step / 2.0

            def f(it=it, step=step, nstep=nstep):
                K.ts(junk, C.SS_[:, 0:L1], MID, 0.0, ALU.is_ge, ALU.add, accum=CNTD)
                if L2:
                    K.act(C.JA[:, 0:L2], C.SS_[:, L1:L], AF.Sign, bias=MID, scale=-1.0, accum=SGA)
                    K.stt(X, SGA, -0.5, CNTD, ALU.mult, ALU.add)
                    K.ts(M, X, thr, step, ALU.is_ge, ALU.mult)
                else:
                    K.ts(M, CNTD, thr, step, ALU.is_ge, ALU.mult)
                if it < N_ITER - 1:
                    K.stt(MID, M, -nstep, MID, ALU.add, ALU.add)
                else:
                    K.stt(LO, M, -step, MID, ALU.add, ALU.add)
            its.append(f)
            step = nstep
        return its

    def negm_piece(b, p):
        L = 128 * (b + 1)
        k0 = 512 * p
        w = min(512, L - k0)
        K.ts(C.NEGM[:, k0:k0 + w], C.SS_[:, k0:k0 + w], C.ST[:, 0:1], NEG, ALU.is_lt, ALU.mult)

    def npieces(b):
        return (128 * (b + 1) + 511) // 512

    import os
    dbg = os.environ.get("HYB_DBG", "psta3")

    def attn(b, nb=None):
        qq = b % 2
        QT = C.QT[(b // 2) % 2]
        OT = [C.PS[6], C.PS[7]]
        its = search_iters(nb) if nb is not None else []
        idone = 0

        def LM(j):
            psl = [C.PS[(2 * j) % 4], C.PS[(2 * j + 1) % 4]]
            for hl in range(4):
                for par in range(2):
                    base = 64 * par
                    K.mm(psl[par][:, hl * 128:(hl + 1) * 128], C.KT2[base:base + 64, 128 * j:128 * j + 128],
                         QT[base:base + 64, hl, qq * 128:(qq + 1) * 128], start=(hl == 0), stop=False)
            for par in range(2):
                K.mm(psl[par][:, :], C.NEGM[:, 128 * j:128 * j + 128], C.IDB[:, :], start=False, stop=True)

        LM(0)
        for j in range(b + 1):
            if j + 1 <= b:
                LM(j + 1)
            pk = j % 2
            pair = View(C.PSP[pk][:, :], [("ps", 2 * pk * 2048, (2 * pk + 2) * 2048)])
            ptf = C.PTF[j % 2]
            K.act(ptf[:, :], pair, AF.Exp, scale=0.125)
            for par in range(2):
                K.mm(OT[par][0:65, :], C.VA[:, j, 0:65], ptf[:, par * 512:(par + 1) * 512], start=(j == 0), stop=(j == b))
            want = (len(its) * (j + 1) + b) // (b + 1)
            while idone < min(want, len(its)):
                its[idone]()
                idone += 1
        while idone < len(its):
            its[idone]()
            idone += 1
        if nb is not None:
            for p in range(npieces(nb)):
                negm_piece(nb, p)
        if "3" not in dbg:
            return
        for par in range(2):
            rd = C.RDEN[64:65, par * 512:(par + 1) * 512]
            ot = OT[par]
            K.op("dve", (lambda rd=rd, ot=ot: nc.vector.reciprocal(rd.ap, ot[64:65, :].ap)), reads=[ot[64:65, :]], writes=[rd])
            pb = C.PS[4 + par][0:64, :]
            K.mm(pb, C.ONES[64:65, 0:64], rd)
            rb = C.RB[par]
            K.copy(rb[0:64, :], pb, eng="act")
            yv = C.YAT[0:64, 4 * par:4 * par + 4, qq * 128:(qq + 1) * 128]
            otv = View(ot[0:64, :].ap.rearrange("p (h t) -> p h t", h=4), ot[0:64, :].regs)
            rbv = View(rb.h[0:64, :].rearrange("p (h t) -> p h t", h=4), rb[0:64, :].regs)
            K.tt(yv, otv, rbv, ALU.mult)

    def outproj(i):
        t0 = i * NH
        xt = C.XH[i % 2]
        YC = C.YC[i % 2]
        for m in range(NC8):
            po = C.PS[m % 4][:, 0:NH]
            for h in range(8):
                K.mm(po, C.WOA[0:64, h, m * 128:(m + 1) * 128], C.YAT[0:64, 4 * (h % 2) + h // 2, :], start=(h == 0), stop=False)
            for c in range(4):
                K.mm(po, C.WOC[:, c, m * 128:(m + 1) * 128], YC[:, c, :], start=False, stop=(c == 3))
            K.tt(xt[:, m, :], xt[:, m, :], po, ALU.add)
        stores.append(K.dma("act", K.dview(dst[:, t0:t0 + NH].rearrange("(c p) t -> p c t", p=128), dst_name, t0, t0 + NH), xt[:, :, :]))

    for i in range(nht):
        proj(i)
        for b in (2 * i, 2 * i + 1):
            score(b)
            if b == 0:
                search_iters(0)
                for p in range(npieces(0)):
                    negm_piece(0, p)
            else:
                attn(b - 1, b)
                if (b - 1) % 2 == 1:
                    outproj((b - 1) // 2)
    attn(nblk - 1, None)
    outproj(nht - 1)
    return stores


PHASES = [
    ("ffn", "xT", "d_x", "hA", "d_h", 0, 0, VOFF["ffn_norm"] + 0),
    ("hyb", "hA", "d_h", "hA", "d_h"),
    ("ffn", "hA", "d_h", "hA", "d_h", 0, 1, VOFF["ffn_norm"] + 8),
    ("ffn", "hA", "d_h", "hA", "d_h", 1, 0, VOFF["ffn_norm"] + 16),
    ("conf", "hA", "d_h", "hA", "d_h"),
    ("ffn", "hA", "d_h", "outT", "d_out", 1, 1, VOFF["ffn_norm"] + 24, VOFF["final_norm"]),
]

_CACHE = {}


def host_inputs(inputs, S):
    f = lambda a: np.ascontiguousarray(np.asarray(a, np.float32))
    tab, cst = hyb_consts(S)
    shared = {
        "vecs": pack_vecs(inputs),
        "ffn_w_gate": f(inputs["ffn_w_gate"]), "ffn_w_up": f(inputs["ffn_w_up"]), "ffn_w_down": f(inputs["ffn_w_down"]),
        "conf_w_pw1": f(np.asarray(inputs["conf_w_pw1"])[0]), "conf_w_pw2": f(np.asarray(inputs["conf_w_pw2"])[0]),
        "hyb_w_in": f(np.asarray(inputs["hyb_w_in"])[0]), "w_hyb_a": pack_hyb_w(np.asarray(inputs["hyb_w_in"])[0]),
        "hyb_w_out": f(np.asarray(inputs["hyb_w_out"])[0]), "rope_tab": tab, "hyb_cst": cst,
    }
    return shared


def kernel(**inputs):
    x = np.asarray(inputs["x"], np.float32)
    B, S, _ = x.shape
    if S not in _CACHE:
        _CACHE[S] = build(S, PHASES)
    nc = _CACHE[S]
    shared = host_inputs(inputs, S)
    in_maps = []
    for b in range(B):
        m = dict(shared)
        m["xT"] = np.ascontiguousarray(x[b].T)
        in_maps.append(m)
    res = run_bass_kernel_spmd(nc, in_maps, core_ids=list(range(B)))
    out = np.stack([np.ascontiguousarray(r["outT"].T) for r in res.results], 0)
    return out.astype(np.float32)
```

```python
import numpy as np
import concourse.bass as bass
import concourse.mybir as mybir

F32 = mybir.dt.float32
BF16 = mybir.dt.bfloat16
AF = mybir.ActivationFunctionType
ALU = mybir.AluOpType
AX = mybir.AxisListType

DT_SIZE = {F32: 4, BF16: 2}
SEM_LIMIT = 2000
N_DMA_SEMS = 16


class View:
    __slots__ = ("ap", "regs")

    def __init__(self, ap, regs):
        self.ap = ap
        self.regs = regs


class Space:
    def __init__(self):
        self.b = [0, 1 << 60]
        self.w = [None]
        self.r = [[]]

    def _split(self, x):
        import bisect
        i = bisect.bisect_left(self.b, x)
        if self.b[i] == x:
            return i
        self.b.insert(i, x)
        self.w.insert(i, self.w[i - 1])
        self.r.insert(i, list(self.r[i - 1]))
        return i

    def rng(self, lo, hi):
        i = self._split(lo)
        j = self._split(hi)
        return range(i, j)


class Op:
    __slots__ = ("eng", "fn", "deps", "needed", "ticket", "dma", "idx")

    def __init__(self, eng, fn, dma):
        self.eng = eng
        self.fn = fn
        self.deps = []
        self.needed = False
        self.ticket = None
        self.dma = dma


class SBT:
    def __init__(self, K, name, shape, dtype, offset=None):
        self.K = K
        self.shape = list(shape)
        self.dtype = dtype
        es = DT_SIZE[dtype]
        nfree = int(np.prod(shape[1:]))
        if offset is None:
            offset = K.sb_alloc(nfree * es)
        self.off = offset
        self.es = es
        self.h = K.nc.alloc_sbuf_tensor_at(name, list(shape), dtype, offset=offset)
        st = []
        acc = 1
        for d in reversed(shape[1:]):
            st.append(acc)
            acc *= d
        self.strides = list(reversed(st))

    def __getitem__(self, idx):
        if not isinstance(idx, tuple):
            idx = (idx,)
        idx = tuple(idx) + (slice(None),) * (len(self.shape) - len(idx))
        lo = 0
        hi = 0
        for k, (ix, d) in enumerate(zip(idx[1:], self.shape[1:])):
            s = self.strides[k]
            if isinstance(ix, int):
                a, b = ix, ix + 1
            else:
                a, b, step = ix.indices(d)
                assert step == 1
            lo += a * s
            hi += (b - 1) * s
        hi += 1
        ap = self.h[idx]
        return View(ap, [("sb", self.off + lo * self.es, self.off + hi * self.es)])


class PST:
    def __init__(self, K, name, bank, handle=None, coff=0):
        self.K = K
        self.bank = bank
        self.coff = coff
        self.h = handle if handle is not None else K.nc.alloc_psum_tensor(name, [128, 512], F32)

    def __getitem__(self, idx):
        if not isinstance(idx, tuple):
            idx = (idx,)
        idx = tuple(idx) + (slice(None),) * (2 - len(idx))
        a, b, _ = idx[1].indices(512)
        return View(self.h[idx[0], self.coff + a:self.coff + b], [("ps", self.bank * 2048, self.bank * 2048 + 2048)])


class Kern:
    def __init__(self, nc):
        self.nc = nc
        self.ops = []
        self.spaces = {}
        self.sb_top = 16512
        self.eng = {"pe": nc.tensor, "act": nc.scalar, "dve": nc.vector,
                    "pool": nc.gpsimd, "sp": nc.sync}

    def sb_alloc(self, nbytes):
        off = (self.sb_top + 63) // 64 * 64
        self.sb_top = off + nbytes
        return off

    def sb(self, name, shape, dtype, offset=None):
        return SBT(self, name, shape, dtype, offset)

    def dview(self, ap, space, lo, hi):
        return View(ap, [(space, lo, hi)])

    def _sp(self, name):
        if name not in self.spaces:
            self.spaces[name] = Space()
        return self.spaces[name]

    def op(self, eng, fn, reads=(), writes=(), dma=False):
        o = Op(eng, fn, dma)
        o.idx = len(self.ops)
        deps = {}

        def add(d, raw):
            if d is None:
                return
            if not d.dma and not o.dma and d.eng == eng:
                if eng == "pe" or not raw:
                    return
            deps[d.idx] = d

        for v in reads:
            for (s, lo, hi) in v.regs:
                sp = self._sp(s)
                for i in sp.rng(lo, hi):
                    add(sp.w[i], True)
        for v in writes:
            for (s, lo, hi) in v.regs:
                sp = self._sp(s)
                for i in sp.rng(lo, hi):
                    add(sp.w[i], False)
                    for r in sp.r[i]:
                        add(r, False)
        for v in reads:
            for (s, lo, hi) in v.regs:
                sp = self._sp(s)
                for i in sp.rng(lo, hi):
                    sp.r[i].append(o)
        for v in writes:
            for (s, lo, hi) in v.regs:
                sp = self._sp(s)
                for i in sp.rng(lo, hi):
                    sp.w[i] = o
                    sp.r[i] = []
        o.deps = list(deps.values())
        for d in o.deps:
            d.needed = True
        self.ops.append(o)
        return o

    def mm(self, out, lhsT, rhs, start=True, stop=True):
        nc = self.nc
        return self.op("pe", lambda: nc.tensor.matmul(out.ap, lhsT.ap, rhs.ap, start=start, stop=stop),
                       reads=[lhsT, rhs], writes=[out])

    def act(self, out, in_, func, bias=None, scale=None, accum=None, eng="act"):
        nc = self.nc
        kw = {}
        rd = [in_]
        wr = [out]
        if bias is not None:
            if isinstance(bias, View):
                kw["bias"] = bias.ap
                rd.append(bias)
            else:
                kw["bias"] = bias
        if scale is not None:
            if isinstance(scale, View):
                kw["scale"] = scale.ap
                rd.append(scale)
            else:
                kw["scale"] = scale
        if accum is not None:
            kw["accum_out"] = accum.ap
            wr.append(accum)
        return self.op("act", lambda: nc.scalar.activation(out.ap, in_.ap, func, **kw), reads=rd, writes=wr)

    def _v(self, eng):
        return self.nc.vector if eng == "dve" else self.nc.gpsimd

    def tt(self, out, in0, in1, op, eng="dve"):
        e = self._v(eng)
        return self.op(eng, lambda: e.tensor_tensor(out.ap, in0.ap, in1.ap, op), reads=[in0, in1], writes=[out])

    def ts(self, out, in0, s1, s2, op0, op1=None, accum=None, eng="dve"):
        e = self._v(eng)
        rd = [in0]
        wr = [out]
        a1 = s1
        a2 = s2
        if isinstance(s1, View):
            rd.append(s1)
            a1 = s1.ap
        if isinstance(s2, View):
            rd.append(s2)
            a2 = s2.ap
        kw = {}
        if op1 is not None:
            kw["op1"] = op1
        if accum is not None:
            kw["accum_out"] = accum.ap
            wr.append(accum)
        return self.op(eng, lambda: e.tensor_scalar(out.ap, in0.ap, a1, a2, op0, **kw), reads=rd, writes=wr)

    def stt(self, out, in0, scalar, in1, op0, op1, eng="dve"):
        e = self._v(eng)
        rd = [in0, in1]
        a = scalar
        if isinstance(scalar, View):
            rd.append(scalar)
            a = scalar.ap
        return self.op(eng, lambda: e.scalar_tensor_tensor(out.ap, in0.ap, a, in1.ap, op0, op1),
                       reads=rd, writes=[out])

    def copy(self, out, in_, eng="dve"):
        if eng == "act":
            nc = self.nc
            return self.op("act", lambda: nc.scalar.copy(out.ap, in_.ap), reads=[in_], writes=[out])
        e = self._v(eng)
        return self.op(eng, lambda: e.tensor_copy(out.ap, in_.ap), reads=[in_], writes=[out])

    def memset(self, out, val, eng="dve"):
        e = self._v(eng)
        return self.op(eng, lambda: e.memset(out.ap, val), writes=[out])

    def dma(self, q, out, in_):
        e = self.eng[q]
        return self.op(q, lambda: e.dma_start(out=out.ap, in_=in_.ap), reads=[in_], writes=[out], dma=True)

    def emit(self, final_waits=()):
        nc = self.nc
        import contextlib
        self._stack = contextlib.ExitStack()
        sems = []

        def new_sem(nm):
            s = self._stack.enter_context(nc.semaphore(nm))
            sems.append(s)
            return s

        cur = {}
        nsem = [0]
        dsem = [[new_sem(f"dq{i}"), 0] for i in range(N_DMA_SEMS)]
        dnext = [0]
        waited = {}

        def wait(engname, sem, val):
            key = (engname, id(sem))
            if waited.get(key, 0) >= val:
                return
            waited[key] = val
            self.eng[engname].wait_ge(sem, val)

        nw = 0
        for o in self.ops:
            for d in o.deps:
                sem, val = d.ticket
                wait(o.eng, sem, val)
            if o.dma:
                k = dnext[0] % N_DMA_SEMS
                dnext[0] += 1
                sem, cnt = dsem[k]
                if cnt:
                    wait(o.eng, sem, cnt)
                ins = o.fn()
                ins.then_inc(sem, 16)
                dsem[k][1] = cnt + 16
                o.ticket = (sem, cnt + 16)
            else:
                ins = o.fn()
                if o.needed:
                    c = cur.get(o.eng)
                    if c is None or c[1] >= SEM_LIMIT:
                        nsem[0] += 1
                        c = [new_sem(f"s_{o.eng}{nsem[0]}"), 0]
                        cur[o.eng] = c
                    c[1] += 1
                    ins.then_inc(c[0], 1)
                    o.ticket = (c[0], c[1])
        for d in final_waits:
            sem, val = d.ticket
            wait("sp", sem, val)
        self.n_sems = len(sems)
        return len(self.ops)

from concourse.bass_utils import run_bass_kernel_spmd

D = 1024
DFF = 2816
NF = DFF // 128
NC8 = D // 128
NT = 512
EPS = 1e-6


class Ctx:
    pass


def build(S, phases, n_vec_cols=None):
    if n_vec_cols is None:
        n_vec_cols = NVEC
    nc = bass.Bass("TRN2", target_bir_lowering=False)
    K = Kern(nc)
    C = Ctx()
    C.nc, C.K, C.S = nc, K, S
    C.ntiles = S // NT

    def din(name, shape, dt=F32):
        return nc.dram_tensor(name, list(shape), dt, kind="ExternalInput").ap()

    C.xT = din("xT", [D, S])
    C.vecs = din("vecs", [128, n_vec_cols])
    C.wg = din("ffn_w_gate", [2, 2, D, DFF])
    C.wu = din("ffn_w_up", [2, 2, D, DFF])
    C.wd = din("ffn_w_down", [2, 2, DFF, D])
    C.w_pw1 = din("conf_w_pw1", [D, 2 * D])
    C.w_pw2 = din("conf_w_pw2", [D, D])
    C.w_in = din("hyb_w_in", [D, 2472])
    C.w_hyb_a = din("w_hyb_a", [D, HCOLS_A])
    C.w_out = din("hyb_w_out", [D, D])
    C.tab = din("rope_tab", [4, 128, S])
    C.cst = din("hyb_cst", [128, 896])
    C.outT = nc.dram_tensor("outT", [D, S], F32, kind="ExternalOutput").ap()
    C.hA = nc.dram_tensor("hA", [D, S], F32, kind="Internal").ap()

    C.VEC = K.sb("VEC", [128, n_vec_cols], F32)
    C.ONES = K.sb("ONES", [128, 128], F32)
    C.X = [K.sb(f"X{i}", [128, NC8, NT], F32) for i in range(2)]
    C.TMP = [K.sb(f"TMP{i}", [128, NT], F32) for i in range(2)]
    C.ACC = K.sb("ACC", [128, NT], F32)
    C.RSTD = K.sb("RSTD", [128, NT], F32)
    C.HN = K.sb("HN", [128, NC8, NT], BF16)
    C.arena = K.sb_top
    C.PSP = [nc.alloc_psum_tensor(f"PSP{i}", [128, 1024], F32) for i in range(4)]
    C.PS = [PST(K, f"PS{i}", i, C.PSP[i // 2], (i % 2) * 512) for i in range(8)]

    K.dma("sp", C.VEC[:, :], K.dview(C.vecs, "d_vecs", 0, 1))
    K.memset(C.ONES[:, :], 1.0)

    ffn_alloc(C)
    conf_alloc(C)
    hyb_alloc(C)
    last = None
    for ph in phases:
        if ph[0] == "ffn":
            last = ffn_phase(C, getattr(C, ph[1]), ph[2], getattr(C, ph[3]), *ph[4:])
        elif ph[0] == "hyb":
            last = hyb_phase(C, getattr(C, ph[1]), ph[2], getattr(C, ph[3]), ph[4])
        elif ph[0] == "conf":
            last = conf_phase(C, getattr(C, ph[1]), ph[2], getattr(C, ph[3]), ph[4])
    n = K.emit(final_waits=last)
    print("ops", n, "sems", K.n_sems, "sbuf top", K.sb_top)
    return nc

def ffn_alloc(C):
    K = C.K
    K.sb_top = C.arena
    C.WG = K.sb("WG", [128, NC8, DFF], BF16)
    C.WU = K.sb("WU", [128, NC8, DFF], BF16)
    C.WD = K.sb("WD", [128, NF, D], BF16)
    C.G = K.sb("G", [128, NF, NT], BF16)
    C.SILU = [K.sb(f"SILU{i}", [128, NT], BF16) for i in range(2)]
    C.OUTT = K.sb("OUTT", [128, NC8, NT], F32, offset=C.G.off)


def dram_tile(C, ap, name, i):
    t0 = i * NT
    return C.K.dview(ap[:, t0:t0 + NT].rearrange("(c p) t -> p c t", p=128), name, t0, t0 + NT)


def rmsnorm(C, xt, vcol, out):
    K = C.K
    K.tt(C.ACC[:, :], xt[:, 0, :], xt[:, 0, :], ALU.mult)
    for c in range(1, NC8):
        tmp = C.TMP[c % 2]
        K.tt(tmp[:, :], xt[:, c, :], xt[:, c, :], ALU.mult)
        K.tt(C.ACC[:, :], C.ACC[:, :], tmp[:, :], ALU.add)
    ps = C.PS[6][:, :]
    K.mm(ps, C.ONES[:, :], C.ACC[:, :])
    K.ts(C.RSTD[:, :], ps, 1.0 / D, EPS, ALU.mult, ALU.add)
    nc = C.nc
    rs = C.RSTD[:, :]
    K.op("dve", lambda: nc.vector.reciprocal(rs.ap, rs.ap), reads=[rs], writes=[rs])
    K.act(rs, rs, AF.Sqrt)
    for c in range(NC8):
        K.stt(out[:, c, :], xt[:, c, :], C.VEC[:, vcol + c:vcol + c + 1], C.RSTD[:, :], ALU.mult, ALU.mult)


def ffn_phase(C, src, src_name, dst, dst_name, l, j, vcol, final_vcol=None):
    K, nc = C.K, C.nc
    wg = C.wg[l, j].rearrange("(c p) f -> p c f", p=128)
    wu = C.wu[l, j].rearrange("(c p) f -> p c f", p=128)
    wd = C.wd[l, j].rearrange("(c p) m -> p c m", p=128)
    NCB = 4
    cbw = DFF // NCB
    for cb in range(NCB):
        c0 = cb * cbw
        for c in range(NC8):
            K.dma("pool", C.WG[:, c, c0:c0 + cbw], K.dview(wg[:, c, c0:c0 + cbw], "d_w", 0, 1))
            K.dma("pool", C.WU[:, c, c0:c0 + cbw], K.dview(wu[:, c, c0:c0 + cbw], "d_w", 0, 1))
    for c in range(NF):
        K.dma("pool", C.WD[:, c, :], K.dview(wd[:, c, :], "d_w", 0, 1))
    stores = []
    nt = C.ntiles
    K.dma("sp", C.X[0][:, :, :], dram_tile(C, src, src_name, 0))
    rmsnorm(C, C.X[0], vcol, C.HN)
    for i in range(nt):
        xt = C.X[i % 2]
        if i + 1 < nt:
            K.dma("sp", C.X[(i + 1) % 2][:, :, :], dram_tile(C, src, src_name, i + 1))
        for f in range(NF):
            pg = C.PS[f % 2][:, :]
            pu = C.PS[2 + f % 2][:, :]
            for c in range(NC8):
                K.mm(pg, C.WG[:, c, f * 128:(f + 1) * 128], C.HN[:, c, :], start=(c == 0), stop=(c == NC8 - 1))
            for c in range(NC8):
                K.mm(pu, C.WU[:, c, f * 128:(f + 1) * 128], C.HN[:, c, :], start=(c == 0), stop=(c == NC8 - 1))
            sl = C.SILU[f % 2]
            K.act(sl[:, :], pg, AF.Silu)
            K.tt(C.G[:, f, :], sl[:, :], pu, ALU.mult)
        if i + 1 < nt:
            rmsnorm(C, C.X[(i + 1) % 2], vcol, C.HN)
        for m in range(NC8):
            po = C.PS[4 + m % 2][:, :]
            for f in range(NF):
                K.mm(po, C.WD[:, f, m * 128:(m + 1) * 128], C.G[:, f, :], start=(f == 0), stop=(f == NF - 1))
            K.stt(xt[:, m, :], po, 0.5, xt[:, m, :], ALU.mult, ALU.add)
        if final_vcol is None:
            stores.append(K.dma("act", dram_tile(C, dst, dst_name, i), xt[:, :, :]))
        else:
            rmsnorm(C, xt, final_vcol, C.OUTT)
            stores.append(K.dma("act", dram_tile(C, dst, dst_name, i), C.OUTT[:, :, :]))
    return stores


VEC_SPEC = [("ffn_norm", 4 * 8), ("mix_norm", 2 * 8), ("final_norm", 8), ("conf_b_pw1", 16), ("conf_conv_b", 8),
            ("conf_ln_g", 8), ("conf_ln_b", 8), ("conf_b_pw2", 8), ("conf_conv_w", 8 * 31), ("hyb_conv_w", 4 * 3)]
VOFF = {}
_o = 0
for _n, _c in VEC_SPEC:
    VOFF[_n] = _o
    _o += _c
NVEC = _o


def pack_vecs(inp):
    v = np.zeros((128, NVEC), np.float32)

    def put(name, arr):
        a = np.asarray(arr, np.float32).reshape(-1)
        n = a.size // 128
        v[:, VOFF[name]:VOFF[name] + n] = a.reshape(n, 128).T

    put("ffn_norm", inp["ffn_norm"])
    put("mix_norm", inp["mix_norm"])
    put("final_norm", inp["final_norm"])
    put("conf_b_pw1", inp["conf_b_pw1"])
    put("conf_conv_b", inp["conf_conv_b"])
    put("conf_ln_g", inp["conf_ln_g"])
    put("conf_ln_b", inp["conf_ln_b"])
    put("conf_b_pw2", inp["conf_b_pw2"])
    cw = np.asarray(inp["conf_conv_w"], np.float32).reshape(31, 8, 128)
    v[:, VOFF["conf_conv_w"]:VOFF["conf_conv_w"] + 248] = cw.transpose(2, 1, 0).reshape(128, 248)
    hw = np.asarray(inp["hyb_conv_w"], np.float32).reshape(3, 4, 128)
    v[:, VOFF["hyb_conv_w"]:VOFF["hyb_conv_w"] + 12] = hw.transpose(2, 1, 0).reshape(128, 12)
    return v


CK = 31
HALO = CK - 1
CONV_POOL_CHUNKS = 0


def conf_alloc(C):
    K = C.K
    K.sb_top = C.arena
    C.W1 = K.sb("W1", [128, NC8, 2 * D], BF16)
    C.W2 = K.sb("W2", [128, NC8, D], BF16)
    C.U = K.sb("U", [128, NC8, HALO + NT], BF16)
    C.Y = K.sb("Y", [128, NC8, NT], F32)
    C.V = K.sb("V", [128, NC8, NT], BF16)
    C.SG = [K.sb(f"SG{i}", [128, NT], F32) for i in range(2)]
    C.A1 = K.sb("A1", [128, NT], F32)
    C.A2 = K.sb("A2", [128, NT], F32)
    C.MEAN = K.sb("MEAN", [128, NT], F32)
    C.DIAG = K.sb("DIAG", [128, NC8, CK, 128], BF16)
    C.IDF = K.sb("IDF", [128, 128], F32)
    C.IDC = K.sb("IDC", [128, 128], BF16)
    print("conf sbuf top", K.sb_top)
    assert K.sb_top <= 229344


def conf_phase(C, src, src_name, dst, dst_name):
    K, nc = C.K, C.nc
    import os
    cd = os.environ.get('CONF_DBG', 'dngclp')
    w1 = C.w_pw1.rearrange("(c p) f -> p c f", p=128)
    w2 = C.w_pw2.rearrange("(c p) f -> p c f", p=128)
    for c in range(NC8):
        K.dma("pool", C.W1[:, c, :], K.dview(w1[:, c, :], "d_w", 0, 1))
    for c in range(NC8):
        K.dma("pool", C.W2[:, c, :], K.dview(w2[:, c, :], "d_w", 0, 1))
    vb1 = VOFF["conf_b_pw1"]
    vcb = VOFF["conf_conv_b"]
    vg, vb = VOFF["conf_ln_g"], VOFF["conf_ln_b"]
    vb2 = VOFF["conf_b_pw2"]
    vcw = VOFF["conf_conv_w"]
    vnorm = VOFF["mix_norm"] + 8
    stores = []
    nt = C.ntiles
    K.dma("sp", C.IDF[:, :], K.dview(C.cst[:, 256:384], "d_cst", 0, 1))
    K.copy(C.IDC[:, :], C.IDF[:, :])
    for c in (range(NC8) if 'd' in cd else []):
        for j in range(CK):
            K.ts(C.DIAG[:, c, j, :], C.IDC[:, :], C.VEC[:, vcw + c * CK + j:vcw + c * CK + j + 1], None, ALU.mult)
    K.dma("sp", C.X[0][:, :, :], dram_tile(C, src, src_name, 0))
    U = C.U
    for i in range(nt):
        xt = C.X[i % 2]
        if i + 1 < nt:
            K.dma("sp", C.X[(i + 1) % 2][:, :, :], dram_tile(C, src, src_name, i + 1))
        if 'n' in cd:
            rmsnorm(C, xt, vnorm, C.HN)
        if i == 0:
            K.memset(U[:, :, 0:HALO], 0.0)
        else:
            for c in range(NC8):
                K.copy(U[:, c, 0:HALO], U[:, c, NT:NT + HALO])
        for c in (range(NC8) if 'g' in cd else []):
            pa = C.PS[c % 2][:, :]
            pg = C.PS[2 + c % 2][:, :]
            for k in range(NC8):
                K.mm(pa, C.W1[:, k, c * 128:(c + 1) * 128], C.HN[:, k, :], start=(k == 0), stop=(k == NC8 - 1))
            for k in range(NC8):
                K.mm(pg, C.W1[:, k, D + c * 128:D + (c + 1) * 128], C.HN[:, k, :], start=(k == 0), stop=(k == NC8 - 1))
            sg = C.SG[c % 2]
            K.act(sg[:, :], pg, AF.Sigmoid, bias=C.VEC[:, vb1 + 8 + c:vb1 + 9 + c])
            K.stt(U[:, c, HALO:HALO + NT], pa, C.VEC[:, vb1 + c:vb1 + c + 1], sg[:, :], ALU.add, ALU.mult)
        for c in (range(NC8) if 'c' in cd else []):
            pc = C.PS[4 + c % 2][:, :]
            for j in range(CK):
                K.mm(pc, C.DIAG[:, c, j, :], U[:, c, j:j + NT], start=(j == 0), stop=(j == CK - 1))
            K.ts(C.Y[:, c, :], pc, C.VEC[:, vcb + c:vcb + c + 1], None, ALU.add)
        if 'l' not in cd:
            stores.append(K.dma("act", dram_tile(C, dst, dst_name, i), xt[:, :, :]))
            continue
        K.copy(C.A1[:, :], C.Y[:, 0, :])
        K.tt(C.A2[:, :], C.Y[:, 0, :], C.Y[:, 0, :], ALU.mult)
        for c in range(1, NC8):
            tmp = C.TMP[c % 2]
            K.tt(tmp[:, :], C.Y[:, c, :], C.Y[:, c, :], ALU.mult)
            K.tt(C.A1[:, :], C.A1[:, :], C.Y[:, c, :], ALU.add)
            K.tt(C.A2[:, :], C.A2[:, :], tmp[:, :], ALU.add)
        p1 = C.PS[6][:, :]
        p2 = C.PS[7][:, :]
        K.mm(p1, C.ONES[:, :], C.A1[:, :])
        K.mm(p2, C.ONES[:, :], C.A2[:, :])
        K.ts(C.MEAN[:, :], p1, 1.0 / D, None, ALU.mult)
        K.tt(C.A1[:, :], C.MEAN[:, :], C.MEAN[:, :], ALU.mult)
        K.stt(C.A2[:, :], p2, 1.0 / D, C.A1[:, :], ALU.mult, ALU.subtract)
        K.ts(C.A2[:, :], C.A2[:, :], EPS, None, ALU.add)
        a2 = C.A2[:, :]
        K.op("dve", lambda: nc.vector.reciprocal(a2.ap, a2.ap), reads=[a2], writes=[a2])
        K.act(C.RSTD[:, :], a2, AF.Sqrt)
        for c in range(NC8):
            K.tt(C.Y[:, c, :], C.Y[:, c, :], C.MEAN[:, :], ALU.subtract)
            K.tt(C.Y[:, c, :], C.Y[:, c, :], C.RSTD[:, :], ALU.mult)
            K.act(C.V[:, c, :], C.Y[:, c, :], AF.Silu, bias=C.VEC[:, vb + c:vb + c + 1],
                  scale=C.VEC[:, vg + c:vg + c + 1])
        for m in (range(NC8) if 'p' in cd else []):
            po = C.PS[4 + m % 2][:, :]
            for k in range(NC8):
                K.mm(po, C.W2[:, k, m * 128:(m + 1) * 128], C.V[:, k, :], start=(k == 0), stop=(k == NC8 - 1))
            K.stt(xt[:, m, :], po, C.VEC[:, vb2 + m:vb2 + m + 1], xt[:, m, :], ALU.add, ALU.add)
        stores.append(K.dma("act", dram_tile(C, dst, dst_name, i), xt[:, :, :]))
    return stores


NH = 256
HCOLS_A = 1224
CQ, CK2, CVW, CQI, CKI = 0, 512, 640, 712, 1096
GU0 = 936
N_ITER = 17
THR_B = 8.0
ACT_FRAC = 0.34
DVE_RELU_HEADS = (1, 4, 6)
TOPK = 256
NEG = -30000.0


def hyb_consts(S):
    theta = np.float32(500000.0)
    t = np.arange(S, dtype=np.float32)

    def tabs(half, period, nrep):
        inv = (theta ** (-(np.arange(half, dtype=np.float32) / np.float32(half)))).astype(np.float32)
        ang = (t[:, None] * inv[None, :]).astype(np.float32)
        c, s = np.cos(ang).astype(np.float32).T, np.sin(ang).astype(np.float32).T
        CC = np.ones((128, S), np.float32)
        SS = np.zeros((128, S), np.float32)
        for base in range(0, 128, period):
            CC[base:base + half] = c
            CC[base + half:base + 2 * half] = c
            SS[base:base + half] = -s
            SS[base + half:base + 2 * half] = s
        return CC, SS

    CCa, SSa = tabs(8, 64, 2)
    CCi, SSi = tabs(4, 32, 4)
    tab = np.stack([CCa, SSa, CCi, SSi], 0)

    def perm(half, period):
        P = np.zeros((128, 128), np.float32)
        for m in range(128):
            r = m % period
            if r < half:
                P[m + half, m] = 1.0
            elif r < 2 * half:
                P[m - half, m] = 1.0
        return P

    cst = np.zeros((128, 128 + 128 + 512 + 128), np.float32)
    cst[:, 0:128] = perm(8, 64)
    cst[:, 128:256] = perm(4, 32)
    cst[:, 256:768] = np.tile(np.eye(128, dtype=np.float32), (1, 4))
    ii = np.arange(128)
    cst[:, 768:896] = np.where(ii[None, :] <= ii[:, None], 0.0, -1e30).astype(np.float32)
    return tab, cst


def pack_hyb_w(w_in):
    w = np.asarray(w_in, np.float32)
    q = w[:, 0:512]
    k = w[:, 512:576]
    v = w[:, 576:640]
    qi = w[:, 640:896]
    ki = w[:, 896:928]
    wi = w[:, 928:936]
    qig = []
    for g in range(3):
        hs = [3 * g, 3 * g + 1, 3 * g + 2]
        cols = [qi[:, 32 * h:32 * h + 32] if h < 8 else qi[:, 0:32] for h in hs] + [qi[:, 0:32]]
        qig.append(np.concatenate(cols, 1))
    out = np.concatenate([q, k, k, v, wi] + qig + [ki, ki, ki, ki], 1)
    assert out.shape[1] == HCOLS_A
    return np.ascontiguousarray(out)


def hyb_alloc(C):
    K = C.K
    S = C.S
    o = C.X[0].off
    C.XH = [K.sb(f"XH{i}", [128, NC8, NH], F32, offset=o + i * 8192) for i in range(2)]
    C.TAB = K.sb("TAB", [128, 4, NH], F32, offset=o + 16384)
    C.YAT = K.sb("YAT", [128, 8, NH], BF16, offset=o + 20480)
    C.QT = [K.sb(f"QT{i}", [128, 4, NH], BF16, offset=o + 24576 + i * 2048) for i in range(2)]
    C.YC = [K.sb(f"YC{i}", [128, 4, NH], BF16, offset=o + 28672 + i * 2048) for i in range(2)]
    o = C.TMP[0].off
    C.TMPH = [K.sb(f"TMPH{i}", [128, NH], F32, offset=o + i * 1024) for i in range(2)]
    C.ACCH = K.sb("ACCH", [128, NH], F32, offset=o + 2048)
    C.RSTDH = K.sb("RSTDH", [128, NH], F32, offset=o + 3072)
    C.HNH = K.sb("HNH", [128, NC8, NH], BF16, offset=o + 4096)
    C.RR = [K.sb(f"RR{i}", [128, 512], BF16, offset=o + 8192 + i * 1024) for i in range(4)]
    C.PTF = [K.sb("PTF0", [128, 1024], BF16, offset=o + 12288), None]
    C.T1 = K.sb("T1", [128, NH], F32, offset=o + 14336)
    assert o + 16384 <= C.arena
    K.sb_top = C.arena
    C.WINR = K.sb("WINR", [128, NC8, HCOLS_A], BF16)
    C.WOA = K.sb("WOA", [128, 8, D], BF16)
    C.WOC = K.sb("WOC", [128, 4, D], BF16)
    C.KT2 = K.sb("KT2", [128, S], BF16)
    C.KI3 = K.sb("KI3", [128, S], BF16)
    C.VA = K.sb("VA", [128, S // 128, 65], BF16)
    C.SS_ = K.sb("SS_", [128, 8192], F32)
    so = C.SS_.off
    C.WGU = K.sb("WGU", [128, NC8, 1536], BF16, offset=so)
    C.PBUF = K.sb("PBUF", [128, 4, NH + 2], F32, offset=so + 24576)
    C.GCS = K.sb("GCS", [128, NH], F32, offset=so + 24576 + 4160)
    C.CV = K.sb("CV", [128, NH], F32, offset=so + 24576 + 4160 + 1024)
    C.QF = K.sb("QF", [128, NH], F32, offset=so + 24576 + 4160 + 2048)
    assert 24576 + 4160 + 3072 <= 32768
    C.NEGM = K.sb("NEGM", [128, 8192], BF16)
    C.QIT = K.sb("QIT", [128, 3, NH], BF16)
    C.DG = K.sb("DG", [128, 8, 128], BF16)
    C.RDEN = K.sb("RDEN", [128, 1024], F32)
    C.RB = [K.sb(f"RB{i}", [128, 512], F32) for i in range(2)]
    C.HALO = K.sb("HALO", [128, 4, 2], F32)
    C.WI = K.sb("WI", [128, 2, 8], F32)
    C.WA = K.sb("WA", [128, 2, 8], F32)
    C.SGN = K.sb("SGN", [128, 2, 8], F32)
    C.ST = K.sb("ST", [128, 8], F32)
    C.JA = K.sb("JA", [128, 2688], BF16)
    C.RR += [K.sb(f"RR{i}", [128, 512], BF16) for i in (4, 5)]
    C.PTF[1] = K.sb("PTF1", [128, 1024], BF16)
    C.CST = K.sb("CST", [128, 896], F32)
    C.IDB = K.sb("IDB", [128, 512], BF16)
    print("hyb sbuf top", K.sb_top)
    assert K.sb_top <= 229344


def rmsnorm_h(C, xt, vcol):
    K = C.K
    nc = C.nc
    K.tt(C.ACCH[:, :], xt[:, 0, :], xt[:, 0, :], ALU.mult)
    for c in range(1, NC8):
        tmp = C.TMPH[c % 2]
        K.tt(tmp[:, :], xt[:, c, :], xt[:, c, :], ALU.mult)
        K.tt(C.ACCH[:, :], C.ACCH[:, :], tmp[:, :], ALU.add)
    ps = C.PS[6][:, 0:NH]
    K.mm(ps, C.ONES[:, :], C.ACCH[:, :])
    rs = C.RSTDH[:, :]
    K.ts(rs, ps, 1.0 / D, EPS, ALU.mult, ALU.add)
    K.op("dve", lambda: nc.vector.reciprocal(rs.ap, rs.ap), reads=[rs], writes=[rs])
    K.act(rs, rs, AF.Sqrt)
    for c in range(NC8):
        K.stt(C.HNH[:, c, :], xt[:, c, :], C.VEC[:, vcol + c:vcol + c + 1], rs, ALU.mult, ALU.mult)


def hyb_phase(C, src, src_name, dst, dst_name):
    K, nc = C.K, C.nc
    S = C.S
    nht = S // NH
    nblk = S // 128
    wa = C.w_hyb_a.rearrange("(c p) f -> p c f", p=128)
    for c in range(NC8):
        K.dma("pool", C.WINR[:, c, :], K.dview(wa[:, c, :], "d_w", 0, 1))
    woa = C.w_out[0:512, :].rearrange("(h p) m -> p h m", p=64)
    woc = C.w_out[512:1024, :].rearrange("(c p) m -> p c m", p=128)
    for h in range(8):
        K.dma("pool", C.WOA[0:64, h, :], K.dview(woa[:, h, :], "d_w", 0, 1))
    for c in range(4):
        K.dma("pool", C.WOC[:, c, :], K.dview(woc[:, c, :], "d_w", 0, 1))
    K.dma("sp", C.CST[:, :], K.dview(C.cst, "d_cst", 0, 1))
    K.copy(C.IDB[:, :], C.CST[:, 256:768])
    K.memset(C.VA[:, :, 64:65], 1.0)
    K.memset(C.HALO[:, :, :], 0.0)
    PMA = C.CST[:, 0:128]
    PMI = C.CST[:, 128:256]
    CAUS = C.CST[:, 768:896]
    wgu = C.w_in[:, GU0:GU0 + 1536].rearrange("(c p) f -> p c f", p=128)
    vnorm = VOFF["mix_norm"]
    vcw = VOFF["hyb_conv_w"]
    stores = []

    def rope(ps, ci, si, PM, out):
        K.copy(C.QF[:, :], ps, eng="act")
        ps2 = C.PS[4 + rope.n % 2][:, 0:NH]
        rope.n += 1
        K.mm(ps2, PM, C.QF[:, :])
        K.tt(C.T1[:, :], C.QF[:, :], C.TAB[:, ci, :], ALU.mult)
        K.tt(C.QF[:, :], ps2, C.TAB[:, si, :], ALU.mult)
        K.tt(out, C.T1[:, :], C.QF[:, :], ALU.add)
    rope.n = 0

    def proj(i):
        t0 = i * NH
        xt = C.XH[i % 2]
        K.dma("sp", xt[:, :, :], C.K.dview(src[:, t0:t0 + NH].rearrange("(c p) t -> p c t", p=128), src_name, t0, t0 + NH))
        K.dma("sp", C.TAB[:, :, :], K.dview(C.tab[:, :, t0:t0 + NH].rearrange("k p t -> p k t"), "d_tab", 0, 1))
        for c in range(NC8):
            K.dma("pool", C.WGU[:, c, :], K.dview(wgu[:, c, :], "d_w", 0, 1))
        rmsnorm_h(C, xt, vnorm)
        pi = [0]

        def nps(rows=128, cols=NH):
            p = C.PS[pi[0] % 4][0:rows, 0:cols]
            pi[0] += 1
            return p

        def fm(ps, W, col0, ncol):
            for k in range(NC8):
                K.mm(ps, W[:, k, col0:col0 + ncol], C.HNH[:, k, :], start=(k == 0), stop=(k == NC8 - 1))

        QT = C.QT[i % 2]
        for c in range(4):
            ps = nps()
            fm(ps, C.WINR, CQ + c * 128, 128)
            rope(ps, 0, 1, PMA, QT[:, c, :])
        ps = nps()
        fm(ps, C.WINR, CK2, 128)
        rope(ps, 0, 1, PMA, C.KT2[:, t0:t0 + NH])
        for g in range(3):
            ps = nps()
            fm(ps, C.WINR, CQI + g * 128, 128)
            rope(ps, 2, 3, PMI, C.QIT[:, g, :])
        ps = nps()
        fm(ps, C.WINR, CKI, 128)
        rope(ps, 2, 3, PMI, C.KI3[:, t0:t0 + NH])
        for q in range(2):
            ps = nps(128, 72)
            for k in range(NC8):
                K.mm(ps, C.HNH[:, k, q * 128:(q + 1) * 128], C.WINR[:, k, CVW:CVW + 72], start=(k == 0), stop=(k == NC8 - 1))
            ch = (t0 // 128) + q
            K.copy(C.VA[:, ch, 0:64], C.PS[(pi[0] - 1) % 4][:, 0:64])
            K.ts(C.WI[:, q, :], C.PS[(pi[0] - 1) % 4][:, 64:72], 1.0 / 16.0, None, ALU.mult)
            K.ts(C.SGN[:, q, :], C.WI[:, q, :], 0.0, 2.0, ALU.is_ge, ALU.mult)
            K.ts(C.SGN[:, q, :], C.SGN[:, q, :], -1.0, None, ALU.add)
            K.tt(C.WA[:, q, :], C.WI[:, q, :], C.SGN[:, q, :], ALU.mult)
        YC = C.YC[i % 2]
        for c in range(4):
            pgc = nps()
            fm(pgc, C.WGU, 512 + c * 128, 128)
            K.copy(C.GCS[:, :], pgc, eng="act")
            pu = nps()
            fm(pu, C.WGU, 1024 + c * 128, 128)
            K.copy(C.PBUF[:, c, 0:2], C.HALO[:, c, :])
            K.tt(C.PBUF[:, c, 2:2 + NH], pu, C.GCS[:, :], ALU.mult)
            K.copy(C.HALO[:, c, :], C.PBUF[:, c, NH:NH + 2])
            K.ts(C.CV[:, :], C.PBUF[:, c, 2:2 + NH], C.VEC[:, vcw + c * 3 + 2:vcw + c * 3 + 3], None, ALU.mult)
            K.stt(C.CV[:, :], C.PBUF[:, c, 1:1 + NH], C.VEC[:, vcw + c * 3 + 1:vcw + c * 3 + 2], C.CV[:, :], ALU.mult, ALU.add)
            K.stt(C.CV[:, :], C.PBUF[:, c, 0:NH], C.VEC[:, vcw + c * 3:vcw + c * 3 + 1], C.CV[:, :], ALU.mult, ALU.add)
            pgb = nps()
            fm(pgb, C.WGU, c * 128, 128)
            K.tt(YC[:, c, :], pgb, C.CV[:, :], ALU.mult)

    def score(b):
        L = 128 * (b + 1)
        qq = b % 2
        for h in range(8):
            K.ts(C.DG[:, h, :], C.IDB[:, 0:128], C.SGN[:, qq, h:h + 1], None, ALU.mult)
        nkt = (L + 511) // 512
        DB = [C.PS[0], C.PS[1], C.PS[2], C.PS[3], C.PS[6], C.PS[7]]
        LAG = 5
        pend = []

        def sum_mm(item):
            n, kt, hh, w = item
            pss = C.PS[4 + kt % 2][:, 0:w]
            K.mm(pss, C.DG[:, hh, :], C.RR[n % 6][:, 0:w], start=(hh == 0), stop=(hh == 7))
            if hh == 7:
                K.copy(C.SS_[:, 512 * kt:512 * kt + w], pss)

        groups = []
        n = 0
        for kt in range(nkt):
            w = min(512, L - 512 * kt)
            for hs in ((0, 1, 2), (3, 4, 5), (6, 7)):
                groups.append([(n + i, kt, h, w) for i, h in enumerate(hs)])
                n += len(hs)
        pendg = []
        for grp in groups:
            for (n_, kt, h, w) in grp:
                k0 = 512 * kt
                g, r = divmod(h, 3)
                ps = DB[n_ % 6][:, 0:w]
                K.mm(ps, C.QIT[32 * r:32 * r + 32, g, qq * 128:(qq + 1) * 128], C.KI3[32 * r:32 * r + 32, k0:k0 + w])
            for (n_, kt, h, w) in grp:
                ps = DB[n_ % 6][:, 0:w]
                if h in DVE_RELU_HEADS:
                    K.ts(C.RR[n_ % 6][:, 0:w], ps, C.WA[:, qq, h:h + 1], 0.0, ALU.mult, ALU.max)
                else:
                    K.act(C.RR[n_ % 6][:, 0:w], ps, AF.Relu, scale=C.WA[:, qq, h:h + 1])
            pendg.append(grp)
            if len(pendg) > 1:
                for item in pendg.pop(0):
                    sum_mm(item)
        for grp in pendg:
            for item in grp:
                sum_mm(item)
        K.tt(C.SS_[:, L - 128:L], C.SS_[:, L - 128:L], CAUS, ALU.add)

    def search_iters(b):
        L = 128 * (b + 1)
        LO = C.ST[:, 0:1]
        MID = C.ST[:, 1:2]
        CNTD = C.ST[:, 2:3]
        M = C.ST[:, 3:4]
        SGA = C.ST[:, 4:5]
        X = C.ST[:, 5:6]
        if b < 2:
            K.memset(LO, -1e29)
            return []
        L2 = 128 * int(ACT_FRAC * (b + 1))
        L1 = L - L2
        thr = float(TOPK) - 0.5 * L2
        K.memset(MID, 0.0)
        junk = View(C.T1.h[:, 0:1].broadcast_to([128, L1]), C.T1[:, 0:1].regs)
        its = []
        step = THR_B
        for it in range(N_ITER):
            nstep = step / 2.0

            def f(it=it, step=step, nstep=nstep):
                K.ts(junk, C.SS_[:, 0:L1], MID, 0.0, ALU.is_ge, ALU.add, accum=CNTD)
                if L2:
                    K.act(C.JA[:, 0:L2], C.SS_[:, L1:L], AF.Sign, bias=MID, scale=-1.0, accum=SGA)
                    K.stt(X, SGA, -0.5, CNTD, ALU.mult, ALU.add)
                    K.ts(M, X, thr, step, ALU.is_ge, ALU.mult)
                else:
                    K.ts(M, CNTD, thr, step, ALU.is_ge, ALU.mult)
                if it < N_ITER - 1:
                    K.stt(MID, M, -nstep, MID, ALU.add, ALU.add)
                else:
                    K.stt(LO, M, -step, MID, ALU.add, ALU.add)
            its.append(f)
            step = nstep
        return its

    def negm_piece(b, p):
        L = 128 * (b + 1)
        k0 = 512 * p
        w = min(512, L - k0)
        K.ts(C.NEGM[:, k0:k0 + w], C.SS_[:, k0:k0 + w], C.ST[:, 0:1], NEG, ALU.is_lt, ALU.mult)

    def npieces(b):
        return (128 * (b + 1) + 511) // 512

    import os
    dbg = os.environ.get("HYB_DBG", "psta3")

    def attn(b, nb=None):
        qq = b % 2
        QT = C.QT[(b // 2) % 2]
        OT = [C.PS[6], C.PS[7]]
        its = search_iters(nb) if nb is not None else []
        idone = 0

        def LM(j):
            psl = [C.PS[(2 * j) % 4], C.PS[(2 * j + 1) % 4]]
            for hl in range(4):
                for par in range(2):
                    base = 64 * par
                    K.mm(psl[par][:, hl * 128:(hl + 1) * 128], C.KT2[base:base + 64, 128 * j:128 * j + 128],
                         QT[base:base + 64, hl, qq * 128:(qq + 1) * 128], start=(hl == 0), stop=False)
            for par in range(2):
                K.mm(psl[par][:, :], C.NEGM[:, 128 * j:128 * j + 128], C.IDB[:, :], start=False, stop=True)

        LM(0)
        for j in range(b + 1):
            if j + 1 <= b:
                LM(j + 1)
            pk = j % 2
            pair = View(C.PSP[pk][:, :], [("ps", 2 * pk * 2048, (2 * pk + 2) * 2048)])
            ptf = C.PTF[j % 2]
            K.act(ptf[:, :], pair, AF.Exp, scale=0.125)
            for par in range(2):
                K.mm(OT[par][0:65, :], C.VA[:, j, 0:65], ptf[:, par * 512:(par + 1) * 512], start=(j == 0), stop=(j == b))
            want = (len(its) * (j + 1) + b) // (b + 1)
            while idone < min(want, len(its)):
                its[idone]()
                idone += 1
        while idone < len(its):
            its[idone]()
            idone += 1
        if nb is not None:
            for p in range(npieces(nb)):
                negm_piece(nb, p)
        if "3" not in dbg:
            return
        for par in range(2):
            rd = C.RDEN[64:65, par * 512:(par + 1) * 512]
            ot = OT[par]
            K.op("dve", (lambda rd=rd, ot=ot: nc.vector.reciprocal(rd.ap, ot[64:65, :].ap)), reads=[ot[64:65, :]], writes=[rd])
            pb = C.PS[4 + par][0:64, :]
            K.mm(pb, C.ONES[64:65, 0:64], rd)
            rb = C.RB[par]
            K.copy(rb[0:64, :], pb, eng="act")
            yv = C.YAT[0:64, 4 * par:4 * par + 4, qq * 128:(qq + 1) * 128]
            otv = View(ot[0:64, :].ap.rearrange("p (h t) -> p h t", h=4), ot[0:64, :].regs)
            rbv = View(rb.h[0:64, :].rearrange("p (h t) -> p h t", h=4), rb[0:64, :].regs)
            K.tt(yv, otv, rbv, ALU.mult)

    def outproj(i):
        t0 = i * NH
        xt = C.XH[i % 2]
        YC = C.YC[i % 2]
        for m in range(NC8):
            po = C.PS[m % 4][:, 0:NH]
            for h in range(8):
                K.mm(po, C.WOA[0:64, h, m * 128:(m + 1) * 128], C.YAT[0:64, 4 * (h % 2) + h // 2, :], start=(h == 0), stop=False)
            for c in range(4):
                K.mm(po, C.WOC[:, c, m * 128:(m + 1) * 128], YC[:, c, :], start=False, stop=(c == 3))
            K.tt(xt[:, m, :], xt[:, m, :], po, ALU.add)
        stores.append(K.dma("act", K.dview(dst[:, t0:t0 + NH].rearrange("(c p) t -> p c t", p=128), dst_name, t0, t0 + NH), xt[:, :, :]))

    for i in range(nht):
        proj(i)
        for b in (2 * i, 2 * i + 1):
            score(b)
            if b == 0:
                search_iters(0)
                for p in range(npieces(0)):
                    negm_piece(0, p)
            else:
                attn(b - 1, b)
                if (b - 1) % 2 == 1:
                    outproj((b - 1) // 2)
    attn(nblk - 1, None)
    outproj(nht - 1)
    return stores


PHASES = [
    ("ffn", "xT", "d_x", "hA", "d_h", 0, 0, VOFF["ffn_norm"] + 0),
    ("hyb", "hA", "d_h", "hA", "d_h"),
    ("ffn", "hA", "d_h", "hA", "d_h", 0, 1, VOFF["ffn_norm"] + 8),
    ("ffn", "hA", "d_h", "hA", "d_h", 1, 0, VOFF["ffn_norm"] + 16),
    ("conf", "hA", "d_h", "hA", "d_h"),
    ("ffn", "hA", "d_h", "outT", "d_out", 1, 1, VOFF["ffn_norm"] + 24, VOFF["final_norm"]),
]

_CACHE = {}


def host_inputs(inputs, S):
    f = lambda a: np.ascontiguousarray(np.asarray(a, np.float32))
    tab, cst = hyb_consts(S)
    shared = {
        "vecs": pack_vecs(inputs),
        "ffn_w_gate": f(inputs["ffn_w_gate"]), "ffn_w_up": f(inputs["ffn_w_up"]), "ffn_w_down": f(inputs["ffn_w_down"]),
        "conf_w_pw1": f(np.asarray(inputs["conf_w_pw1"])[0]), "conf_w_pw2": f(np.asarray(inputs["conf_w_pw2"])[0]),
        "hyb_w_in": f(np.asarray(inputs["hyb_w_in"])[0]), "w_hyb_a": pack_hyb_w(np.asarray(inputs["hyb_w_in"])[0]),
        "hyb_w_out": f(np.asarray(inputs["hyb_w_out"])[0]), "rope_tab": tab, "hyb_cst": cst,
    }
    return shared


def kernel(**inputs):
    x = np.asarray(inputs["x"], np.float32)
    B, S, _ = x.shape
    if S not in _CACHE:
        _CACHE[S] = build(S, PHASES)
    nc = _CACHE[S]
    shared = host_inputs(inputs, S)
    in_maps = []
    for b in range(B):
        m = dict(shared)
        m["xT"] = np.ascontiguousarray(x[b].T)
        in_maps.append(m)
    res = run_bass_kernel_spmd(nc, in_maps, core_ids=list(range(B)))
    out = np.stack([np.ascontiguousarray(r["outT"].T) for r in res.results], 0)
    return out.astype(np.float32)
```

```python
import numpy as np
import concourse.bass as bass
import concourse.mybir as mybir

F32 = mybir.dt.float32
BF16 = mybir.dt.bfloat16
AF = mybir.ActivationFunctionType
ALU = mybir.AluOpType
AX = mybir.AxisListType

DT_SIZE = {F32: 4, BF16: 2}
SEM_LIMIT = 2000
N_DMA_SEMS = 16


class View:
    __slots__ = ("ap", "regs")

    def __init__(self, ap, regs):
        self.ap = ap
        self.regs = regs


class Space:
    def __init__(self):
        self.b = [0, 1 << 60]
        self.w = [None]
        self.r = [[]]

    def _split(self, x):
        import bisect
        i = bisect.bisect_left(self.b, x)
        if self.b[i] == x:
            return i
        self.b.insert(i, x)
        self.w.insert(i, self.w[i - 1])
        self.r.insert(i, list(self.r[i - 1]))
        return i

    def rng(self, lo, hi):
        i = self._split(lo)
        j = self._split(hi)
        return range(i, j)


class Op:
    __slots__ = ("eng", "fn", "deps", "needed", "ticket", "dma", "idx")

    def __init__(self, eng, fn, dma):
        self.eng = eng
        self.fn = fn
        self.deps = []
        self.needed = False
        self.ticket = None
        self.dma = dma


class SBT:
    def __init__(self, K, name, shape, dtype, offset=None):
        self.K = K
        self.shape = list(shape)
        self.dtype = dtype
        es = DT_SIZE[dtype]
        nfree = int(np.prod(shape[1:]))
        if offset is None:
            offset = K.sb_alloc(nfree * es)
        self.off = offset
        self.es = es
        self.h = K.nc.alloc_sbuf_tensor_at(name, list(shape), dtype, offset=offset)
        st = []
        acc = 1
        for d in reversed(shape[1:]):
            st.append(acc)
            acc *= d
        self.strides = list(reversed(st))

    def __getitem__(self, idx):
        if not isinstance(idx, tuple):
            idx = (idx,)
        idx = tuple(idx) + (slice(None),) * (len(self.shape) - len(idx))
        lo = 0
        hi = 0
        for k, (ix, d) in enumerate(zip(idx[1:], self.shape[1:])):
            s = self.strides[k]
            if isinstance(ix, int):
                a, b = ix, ix + 1
            else:
                a, b, step = ix.indices(d)
                assert step == 1
            lo += a * s
            hi += (b - 1) * s
        hi += 1
        ap = self.h[idx]
        return View(ap, [("sb", self.off + lo * self.es, self.off + hi * self.es)])


class PST:
    def __init__(self, K, name, bank, handle=None, coff=0):
        self.K = K
        self.bank = bank
        self.coff = coff
        self.h = handle if handle is not None else K.nc.alloc_psum_tensor(name, [128, 512], F32)

    def __getitem__(self, idx):
        if not isinstance(idx, tuple):
            idx = (idx,)
        idx = tuple(idx) + (slice(None),) * (2 - len(idx))
        a, b, _ = idx[1].indices(512)
        return View(self.h[idx[0], self.coff + a:self.coff + b], [("ps", self.bank * 2048, self.bank * 2048 + 2048)])


class Kern:
    def __init__(self, nc):
        self.nc = nc
        self.ops = []
        self.spaces = {}
        self.sb_top = 16512
        self.eng = {"pe": nc.tensor, "act": nc.scalar, "dve": nc.vector,
                    "pool": nc.gpsimd, "sp": nc.sync}

    def sb_alloc(self, nbytes):
        off = (self.sb_top + 63) // 64 * 64
        self.sb_top = off + nbytes
        return off

    def sb(self, name, shape, dtype, offset=None):
        return SBT(self, name, shape, dtype, offset)

    def dview(self, ap, space, lo, hi):
        return View(ap, [(space, lo, hi)])

    def _sp(self, name):
        if name not in self.spaces:
            self.spaces[name] = Space()
        return self.spaces[name]

    def op(self, eng, fn, reads=(), writes=(), dma=False):
        o = Op(eng, fn, dma)
        o.idx = len(self.ops)
        deps = {}

        def add(d, raw):
            if d is None:
                return
            if not d.dma and not o.dma and d.eng == eng:
                if eng == "pe" or not raw:
                    return
            deps[d.idx] = d

        for v in reads:
            for (s, lo, hi) in v.regs:
                sp = self._sp(s)
                for i in sp.rng(lo, hi):
                    add(sp.w[i], True)
        for v in writes:
            for (s, lo, hi) in v.regs:
                sp = self._sp(s)
                for i in sp.rng(lo, hi):
                    add(sp.w[i], False)
                    for r in sp.r[i]:
                        add(r, False)
        for v in reads:
            for (s, lo, hi) in v.regs:
                sp = self._sp(s)
                for i in sp.rng(lo, hi):
                    sp.r[i].append(o)
        for v in writes:
            for (s, lo, hi) in v.regs:
                sp = self._sp(s)
                for i in sp.rng(lo, hi):
                    sp.w[i] = o
                    sp.r[i] = []
        o.deps = list(deps.values())
        for d in o.deps:
            d.needed = True
        self.ops.append(o)
        return o

    def mm(self, out, lhsT, rhs, start=True, stop=True):
        nc = self.nc
        return self.op("pe", lambda: nc.tensor.matmul(out.ap, lhsT.ap, rhs.ap, start=start, stop=stop),
                       reads=[lhsT, rhs], writes=[out])

    def act(self, out, in_, func, bias=None, scale=None, accum=None, eng="act"):
        nc = self.nc
        kw = {}
        rd = [in_]
        wr = [out]
        if bias is not None:
            if isinstance(bias, View):
                kw["bias"] = bias.ap
                rd.append(bias)
            else:
                kw["bias"] = bias
        if scale is not None:
            if isinstance(scale, View):
                kw["scale"] = scale.ap
                rd.append(scale)
            else:
                kw["scale"] = scale
        if accum is not None:
            kw["accum_out"] = accum.ap
            wr.append(accum)
        return self.op("act", lambda: nc.scalar.activation(out.ap, in_.ap, func, **kw), reads=rd, writes=wr)

    def _v(self, eng):
        return self.nc.vector if eng == "dve" else self.nc.gpsimd

    def tt(self, out, in0, in1, op, eng="dve"):
        e = self._v(eng)
        return self.op(eng, lambda: e.tensor_tensor(out.ap, in0.ap, in1.ap, op), reads=[in0, in1], writes=[out])

    def ts(self, out, in0, s1, s2, op0, op1=None, accum=None, eng="dve"):
        e = self._v(eng)
        rd = [in0]
        wr = [out]
        a1 = s1
        a2 = s2
        if isinstance(s1, View):
            rd.append(s1)
            a1 = s1.ap
        if isinstance(s2, View):
            rd.append(s2)
            a2 = s2.ap
        kw = {}
        if op1 is not None:
            kw["op1"] = op1
        if accum is not None:
            kw["accum_out"] = accum.ap
            wr.append(accum)
        return self.op(eng, lambda: e.tensor_scalar(out.ap, in0.ap, a1, a2, op0, **kw), reads=rd, writes=wr)

    def stt(self, out, in0, scalar, in1, op0, op1, eng="dve"):
        e = self._v(eng)
        rd = [in0, in1]
        a = scalar
        if isinstance(scalar, View):
            rd.append(scalar)
            a = scalar.ap
        return self.op(eng, lambda: e.scalar_tensor_tensor(out.ap, in0.ap, a, in1.ap, op0, op1),
                       reads=rd, writes=[out])

    def copy(self, out, in_, eng="dve"):
        if eng == "act":
            nc = self.nc
            return self.op("act", lambda: nc.scalar.copy(out.ap, in_.ap), reads=[in_], writes=[out])
        e = self._v(eng)
        return self.op(eng, lambda: e.tensor_copy(out.ap, in_.ap), reads=[in_], writes=[out])

    def memset(self, out, val, eng="dve"):
        e = self._v(eng)
        return self.op(eng, lambda: e.memset(out.ap, val), writes=[out])

    def dma(self, q, out, in_):
        e = self.eng[q]
        return self.op(q, lambda: e.dma_start(out=out.ap, in_=in_.ap), reads=[in_], writes=[out], dma=True)

    def emit(self, final_waits=()):
        nc = self.nc
        import contextlib
        self._stack = contextlib.ExitStack()
        sems = []

        def new_sem(nm):
            s = self._stack.enter_context(nc.semaphore(nm))
            sems.append(s)
            return s

        cur = {}
        nsem = [0]
        dsem = [[new_sem(f"dq{i}"), 0] for i in range(N_DMA_SEMS)]
        dnext = [0]
        waited = {}

        def wait(engname, sem, val):
            key = (engname, id(sem))
            if waited.get(key, 0) >= val:
                return
            waited[key] = val
            self.eng[engname].wait_ge(sem, val)

        nw = 0
        for o in self.ops:
            for d in o.deps:
                sem, val = d.ticket
                wait(o.eng, sem, val)
            if o.dma:
                k = dnext[0] % N_DMA_SEMS
                dnext[0] += 1
                sem, cnt = dsem[k]
                if cnt:
                    wait(o.eng, sem, cnt)
                ins = o.fn()
                ins.then_inc(sem, 16)
                dsem[k][1] = cnt + 16
                o.ticket = (sem, cnt + 16)
            else:
                ins = o.fn()
                if o.needed:
                    c = cur.get(o.eng)
                    if c is None or c[1] >= SEM_LIMIT:
                        nsem[0] += 1
                        c = [new_sem(f"s_{o.eng}{nsem[0]}"), 0]
                        cur[o.eng] = c
                    c[1] += 1
                    ins.then_inc(c[0], 1)
                    o.ticket = (c[0], c[1])
        for d in final_waits:
            sem, val = d.ticket
            wait("sp", sem, val)
        self.n_sems = len(sems)
        return len(self.ops)

from concourse.bass_utils import run_bass_kernel_spmd

D = 1024
DFF = 2816
NF = DFF // 128
NC8 = D // 128
NT = 512
EPS = 1e-6


class Ctx:
    pass


def build(S, phases, n_vec_cols=None):
    if n_vec_cols is None:
        n_vec_cols = NVEC
    nc = bass.Bass("TRN2", target_bir_lowering=False)
    K = Kern(nc)
    C = Ctx()
    C.nc, C.K, C.S = nc, K, S
    C.ntiles = S // NT

    def din(name, shape, dt=F32):
        return nc.dram_tensor(name, list(shape), dt, kind="ExternalInput").ap()

    C.xT = din("xT", [D, S])
    C.vecs = din("vecs", [128, n_vec_cols])
    C.wg = din("ffn_w_gate", [2, 2, D, DFF])
    C.wu = din("ffn_w_up", [2, 2, D, DFF])
    C.wd = din("ffn_w_down", [2, 2, DFF, D])
    C.w_pw1 = din("conf_w_pw1", [D, 2 * D])
    C.w_pw2 = din("conf_w_pw2", [D, D])
    C.w_in = din("hyb_w_in", [D, 2472])
    C.w_hyb_a = din("w_hyb_a", [D, HCOLS_A])
    C.w_out = din("hyb_w_out", [D, D])
    C.tab = din("rope_tab", [4, 128, S])
    C.cst = din("hyb_cst", [128, 896])
    C.outT = nc.dram_tensor("outT", [D, S], F32, kind="ExternalOutput").ap()
    C.hA = nc.dram_tensor("hA", [D, S], F32, kind="Internal").ap()

    C.VEC = K.sb("VEC", [128, n_vec_cols], F32)
    C.ONES = K.sb("ONES", [128, 128], F32)
    C.X = [K.sb(f"X{i}", [128, NC8, NT], F32) for i in range(2)]
    C.TMP = [K.sb(f"TMP{i}", [128, NT], F32) for i in range(2)]
    C.ACC = K.sb("ACC", [128, NT], F32)
    C.RSTD = K.sb("RSTD", [128, NT], F32)
    C.HN = K.sb("HN", [128, NC8, NT], BF16)
    C.arena = K.sb_top
    C.PSP = [nc.alloc_psum_tensor(f"PSP{i}", [128, 1024], F32) for i in range(4)]
    C.PS = [PST(K, f"PS{i}", i, C.PSP[i // 2], (i % 2) * 512) for i in range(8)]

    K.dma("sp", C.VEC[:, :], K.dview(C.vecs, "d_vecs", 0, 1))
    K.memset(C.ONES[:, :], 1.0)

    ffn_alloc(C)
    conf_alloc(C)
    hyb_alloc(C)
    last = None
    for ph in phases:
        if ph[0] == "ffn":
            last = ffn_phase(C, getattr(C, ph[1]), ph[2], getattr(C, ph[3]), *ph[4:])
        elif ph[0] == "hyb":
            last = hyb_phase(C, getattr(C, ph[1]), ph[2], getattr(C, ph[3]), ph[4])
        elif ph[0] == "conf":
            last = conf_phase(C, getattr(C, ph[1]), ph[2], getattr(C, ph[3]), ph[4])
    n = K.emit(final_waits=last)
    print("ops", n, "sems", K.n_sems, "sbuf top", K.sb_top)
    return nc

def ffn_alloc(C):
    K = C.K
    K.sb_top = C.arena
    C.WG = K.sb("WG", [128, NC8, DFF], BF16)
    C.WU = K.sb("WU", [128, NC8, DFF], BF16)
    C.WD = K.sb("WD", [128, NF, D], BF16)
    C.G = K.sb("G", [128, NF, NT], BF16)
    C.SILU = [K.sb(f"SILU{i}", [128, NT], BF16) for i in range(2)]
    C.OUTT = K.sb("OUTT", [128, NC8, NT], F32, offset=C.G.off)


def dram_tile(C, ap, name, i):
    t0 = i * NT
    return C.K.dview(ap[:, t0:t0 + NT].rearrange("(c p) t -> p c t", p=128), name, t0, t0 + NT)


def rmsnorm(C, xt, vcol, out, part="ab"):
    K = C.K
    if "a" in part:
        K.tt(C.ACC[:, :], xt[:, 0, :], xt[:, 0, :], ALU.mult)
        for c in range(1, NC8):
            tmp = C.TMP[c % 2]
            K.tt(tmp[:, :], xt[:, c, :], xt[:, c, :], ALU.mult)
            K.tt(C.ACC[:, :], C.ACC[:, :], tmp[:, :], ALU.add)
    if "b" not in part:
        return
    ps = C.PS[6][:, :]
    K.mm(ps, C.ONES[:, :], C.ACC[:, :])
    K.ts(C.RSTD[:, :], ps, 1.0 / D, EPS, ALU.mult, ALU.add)
    nc = C.nc
    rs = C.RSTD[:, :]
    K.op("dve", lambda: nc.vector.reciprocal(rs.ap, rs.ap), reads=[rs], writes=[rs])
    K.act(rs, rs, AF.Sqrt)
    for c in range(NC8):
        K.stt(out[:, c, :], xt[:, c, :], C.VEC[:, vcol + c:vcol + c + 1], C.RSTD[:, :], ALU.mult, ALU.mult)


def ffn_phase(C, src, src_name, dst, dst_name, l, j, vcol, final_vcol=None):
    K, nc = C.K, C.nc
    wg = C.wg[l, j].rearrange("(c p) f -> p c f", p=128)
    wu = C.wu[l, j].rearrange("(c p) f -> p c f", p=128)
    wd = C.wd[l, j].rearrange("(c p) m -> p c m", p=128)
    NCB = 4
    cbw = DFF // NCB
    for cb in range(NCB):
        c0 = cb * cbw
        for c in range(NC8):
            K.dma("pool", C.WG[:, c, c0:c0 + cbw], K.dview(wg[:, c, c0:c0 + cbw], "d_w", 0, 1))
            K.dma("pool", C.WU[:, c, c0:c0 + cbw], K.dview(wu[:, c, c0:c0 + cbw], "d_w", 0, 1))
    for c in range(NF):
        K.dma("pool", C.WD[:, c, :], K.dview(wd[:, c, :], "d_w", 0, 1))
    stores = []
    nt = C.ntiles
    K.dma("sp", C.X[0][:, :, :], dram_tile(C, src, src_name, 0))
    rmsnorm(C, C.X[0], vcol, C.HN)
    for i in range(nt):
        xt = C.X[i % 2]
        if i + 1 < nt:
            K.dma("sp", C.X[(i + 1) % 2][:, :, :], dram_tile(C, src, src_name, i + 1))
        for f in range(NF):
            pg = C.PS[f % 2][:, :]
            pu = C.PS[2 + f % 2][:, :]
            for c in range(NC8):
                K.mm(pg, C.WG[:, c, f * 128:(f + 1) * 128], C.HN[:, c, :], start=(c == 0), stop=(c == NC8 - 1))
            for c in range(NC8):
                K.mm(pu, C.WU[:, c, f * 128:(f + 1) * 128], C.HN[:, c, :], start=(c == 0), stop=(c == NC8 - 1))
            sl = C.SILU[f % 2]
            K.act(sl[:, :], pg, AF.Silu)
            K.tt(C.G[:, f, :], sl[:, :], pu, ALU.mult)
        if i + 1 < nt:
            rmsnorm(C, C.X[(i + 1) % 2], vcol, C.HN, part="a")
        for m in range(NC8):
            po = C.PS[4 + m % 2][:, :]
            for f in range(NF):
                K.mm(po, C.WD[:, f, m * 128:(m + 1) * 128], C.G[:, f, :], start=(f == 0), stop=(f == NF - 1))
            K.stt(xt[:, m, :], po, 0.5, xt[:, m, :], ALU.mult, ALU.add)
            if m == 3 and i + 1 < nt:
                rmsnorm(C, C.X[(i + 1) % 2], vcol, C.HN, part="b")
        if final_vcol is None:
            stores.append(K.dma("act", dram_tile(C, dst, dst_name, i), xt[:, :, :]))
        else:
            rmsnorm(C, xt, final_vcol, C.OUTT)
            stores.append(K.dma("act", dram_tile(C, dst, dst_name, i), C.OUTT[:, :, :]))
    return stores


VEC_SPEC = [("ffn_norm", 4 * 8), ("mix_norm", 2 * 8), ("final_norm", 8), ("conf_b_pw1", 16), ("conf_conv_b", 8),
            ("conf_ln_g", 8), ("conf_ln_b", 8), ("conf_b_pw2", 8), ("conf_conv_w", 8 * 31), ("hyb_conv_w", 4 * 3)]
VOFF = {}
_o = 0
for _n, _c in VEC_SPEC:
    VOFF[_n] = _o
    _o += _c
NVEC = _o


def pack_vecs(inp):
    v = np.zeros((128, NVEC), np.float32)

    def put(name, arr):
        a = np.asarray(arr, np.float32).reshape(-1)
        n = a.size // 128
        v[:, VOFF[name]:VOFF[name] + n] = a.reshape(n, 128).T

    put("ffn_norm", inp["ffn_norm"])
    put("mix_norm", inp["mix_norm"])
    put("final_norm", inp["final_norm"])
    put("conf_b_pw1", inp["conf_b_pw1"])
    put("conf_conv_b", inp["conf_conv_b"])
    put("conf_ln_g", inp["conf_ln_g"])
    put("conf_ln_b", inp["conf_ln_b"])
    put("conf_b_pw2", inp["conf_b_pw2"])
    cw = np.asarray(inp["conf_conv_w"], np.float32).reshape(31, 8, 128)
    v[:, VOFF["conf_conv_w"]:VOFF["conf_conv_w"] + 248] = cw.transpose(2, 1, 0).reshape(128, 248)
    hw = np.asarray(inp["hyb_conv_w"], np.float32).reshape(3, 4, 128)
    v[:, VOFF["hyb_conv_w"]:VOFF["hyb_conv_w"] + 12] = hw.transpose(2, 1, 0).reshape(128, 12)
    return v


CK = 31
HALO = CK - 1
CONV_POOL_CHUNKS = 0


def conf_alloc(C):
    K = C.K
    K.sb_top = C.arena
    C.W1 = K.sb("W1", [128, NC8, 2 * D], BF16)
    C.W2 = K.sb("W2", [128, NC8, D], BF16)
    C.U = K.sb("U", [128, NC8, HALO + NT], BF16)
    C.Y = K.sb("Y", [128, NC8, NT], F32)
    C.V = K.sb("V", [128, NC8, NT], BF16)
    C.SG = [K.sb(f"SG{i}", [128, NT], F32) for i in range(2)]
    C.A1 = K.sb("A1", [128, NT], F32)
    C.A2 = K.sb("A2", [128, NT], F32)
    C.MEAN = K.sb("MEAN", [128, NT], F32)
    C.DIAG = K.sb("DIAG", [128, NC8, CK, 128], BF16)
    C.IDF = K.sb("IDF", [128, 128], F32)
    C.IDC = K.sb("IDC", [128, 128], BF16)
    print("conf sbuf top", K.sb_top)
    assert K.sb_top <= 229344


def conf_phase(C, src, src_name, dst, dst_name):
    K, nc = C.K, C.nc
    import os
    cd = os.environ.get('CONF_DBG', 'dngclp')
    w1 = C.w_pw1.rearrange("(c p) f -> p c f", p=128)
    w2 = C.w_pw2.rearrange("(c p) f -> p c f", p=128)
    for c in range(NC8):
        K.dma("pool", C.W1[:, c, :], K.dview(w1[:, c, :], "d_w", 0, 1))
    for c in range(NC8):
        K.dma("pool", C.W2[:, c, :], K.dview(w2[:, c, :], "d_w", 0, 1))
    vb1 = VOFF["conf_b_pw1"]
    vcb = VOFF["conf_conv_b"]
    vg, vb = VOFF["conf_ln_g"], VOFF["conf_ln_b"]
    vb2 = VOFF["conf_b_pw2"]
    vcw = VOFF["conf_conv_w"]
    vnorm = VOFF["mix_norm"] + 8
    stores = []
    nt = C.ntiles
    K.dma("sp", C.IDF[:, :], K.dview(C.cst[:, 256:384], "d_cst", 0, 1))
    K.copy(C.IDC[:, :], C.IDF[:, :])
    for c in (range(NC8) if 'd' in cd else []):
        for j in range(CK):
            K.ts(C.DIAG[:, c, j, :], C.IDC[:, :], C.VEC[:, vcw + c * CK + j:vcw + c * CK + j + 1], None, ALU.mult)
    K.dma("sp", C.X[0][:, :, :], dram_tile(C, src, src_name, 0))
    U = C.U
    for i in range(nt):
        xt = C.X[i % 2]
        if i + 1 < nt:
            K.dma("sp", C.X[(i + 1) % 2][:, :, :], dram_tile(C, src, src_name, i + 1))
        if 'n' in cd:
            rmsnorm(C, xt, vnorm, C.HN)
        if i == 0:
            K.memset(U[:, :, 0:HALO], 0.0)
        else:
            for c in range(NC8):
                K.copy(U[:, c, 0:HALO], U[:, c, NT:NT + HALO])
        for c in (range(NC8) if 'g' in cd else []):
            pa = C.PS[c % 2][:, :]
            pg = C.PS[2 + c % 2][:, :]
            for k in range(NC8):
                K.mm(pa, C.W1[:, k, c * 128:(c + 1) * 128], C.HN[:, k, :], start=(k == 0), stop=(k == NC8 - 1))
            for k in range(NC8):
                K.mm(pg, C.W1[:, k, D + c * 128:D + (c + 1) * 128], C.HN[:, k, :], start=(k == 0), stop=(k == NC8 - 1))
            sg = C.SG[c % 2]
            K.act(sg[:, :], pg, AF.Sigmoid, bias=C.VEC[:, vb1 + 8 + c:vb1 + 9 + c])
            K.stt(U[:, c, HALO:HALO + NT], pa, C.VEC[:, vb1 + c:vb1 + c + 1], sg[:, :], ALU.add, ALU.mult)
        for c in (range(NC8) if 'c' in cd else []):
            pc = C.PS[4 + c % 2][:, :]
            for j in range(CK):
                K.mm(pc, C.DIAG[:, c, j, :], U[:, c, j:j + NT], start=(j == 0), stop=(j == CK - 1))
            K.ts(C.Y[:, c, :], pc, C.VEC[:, vcb + c:vcb + c + 1], None, ALU.add)
        if 'l' not in cd:
            stores.append(K.dma("act", dram_tile(C, dst, dst_name, i), xt[:, :, :]))
            continue
        K.copy(C.A1[:, :], C.Y[:, 0, :])
        K.tt(C.A2[:, :], C.Y[:, 0, :], C.Y[:, 0, :], ALU.mult)
        for c in range(1, NC8):
            tmp = C.TMP[c % 2]
            K.tt(tmp[:, :], C.Y[:, c, :], C.Y[:, c, :], ALU.mult)
            K.tt(C.A1[:, :], C.A1[:, :], C.Y[:, c, :], ALU.add)
            K.tt(C.A2[:, :], C.A2[:, :], tmp[:, :], ALU.add)
        p1 = C.PS[6][:, :]
        p2 = C.PS[7][:, :]
        K.mm(p1, C.ONES[:, :], C.A1[:, :])
        K.mm(p2, C.ONES[:, :], C.A2[:, :])
        K.ts(C.MEAN[:, :], p1, 1.0 / D, None, ALU.mult)
        K.tt(C.A1[:, :], C.MEAN[:, :], C.MEAN[:, :], ALU.mult)
        K.stt(C.A2[:, :], p2, 1.0 / D, C.A1[:, :], ALU.mult, ALU.subtract)
        K.ts(C.A2[:, :], C.A2[:, :], EPS, None, ALU.add)
        a2 = C.A2[:, :]
        K.op("dve", lambda: nc.vector.reciprocal(a2.ap, a2.ap), reads=[a2], writes=[a2])
        K.act(C.RSTD[:, :], a2, AF.Sqrt)
        for c in range(NC8):
            K.tt(C.Y[:, c, :], C.Y[:, c, :], C.MEAN[:, :], ALU.subtract)
            K.tt(C.Y[:, c, :], C.Y[:, c, :], C.RSTD[:, :], ALU.mult)
            K.act(C.V[:, c, :], C.Y[:, c, :], AF.Silu, bias=C.VEC[:, vb + c:vb + c + 1],
                  scale=C.VEC[:, vg + c:vg + c + 1])
        for m in (range(NC8) if 'p' in cd else []):
            po = C.PS[4 + m % 2][:, :]
            for k in range(NC8):
                K.mm(po, C.W2[:, k, m * 128:(m + 1) * 128], C.V[:, k, :], start=(k == 0), stop=(k == NC8 - 1))
            K.stt(xt[:, m, :], po, C.VEC[:, vb2 + m:vb2 + m + 1], xt[:, m, :], ALU.add, ALU.add)
        stores.append(K.dma("act", dram_tile(C, dst, dst_name, i), xt[:, :, :]))
    return stores


NH = 256
HCOLS_A = 1224
CQ, CK2, CVW, CQI, CKI = 0, 512, 640, 712, 1096
GU0 = 936
N_ITER = 17
THR_B = 8.0
ACT_FRAC = 0.34
DVE_RELU_HEADS = (1, 4, 6)
TOPK = 256
NEG = -30000.0


def hyb_consts(S):
    theta = np.float32(500000.0)
    t = np.arange(S, dtype=np.float32)

    def tabs(half, period, nrep):
        inv = (theta ** (-(np.arange(half, dtype=np.float32) / np.float32(half)))).astype(np.float32)
        ang = (t[:, None] * inv[None, :]).astype(np.float32)
        c, s = np.cos(ang).astype(np.float32).T, np.sin(ang).astype(np.float32).T
        CC = np.ones((128, S), np.float32)
        SS = np.zeros((128, S), np.float32)
        for base in range(0, 128, period):
            CC[base:base + half] = c
            CC[base + half:base + 2 * half] = c
            SS[base:base + half] = -s
            SS[base + half:base + 2 * half] = s
        return CC, SS

    CCa, SSa = tabs(8, 64, 2)
    CCi, SSi = tabs(4, 32, 4)
    tab = np.stack([CCa, SSa, CCi, SSi], 0)

    def perm(half, period):
        P = np.zeros((128, 128), np.float32)
        for m in range(128):
            r = m % period
            if r < half:
                P[m + half, m] = 1.0
            elif r < 2 * half:
                P[m - half, m] = 1.0
        return P

    cst = np.zeros((128, 128 + 128 + 512 + 128), np.float32)
    cst[:, 0:128] = perm(8, 64)
    cst[:, 128:256] = perm(4, 32)
    cst[:, 256:768] = np.tile(np.eye(128, dtype=np.float32), (1, 4))
    ii = np.arange(128)
    cst[:, 768:896] = np.where(ii[None, :] <= ii[:, None], 0.0, -1e30).astype(np.float32)
    return tab, cst


def pack_hyb_w(w_in):
    w = np.asarray(w_in, np.float32)
    q = w[:, 0:512]
    k = w[:, 512:576]
    v = w[:, 576:640]
    qi = w[:, 640:896]
    ki = w[:, 896:928]
    wi = w[:, 928:936]
    qig = []
    for g in range(3):
        hs = [3 * g, 3 * g + 1, 3 * g + 2]
        cols = [qi[:, 32 * h:32 * h + 32] if h < 8 else qi[:, 0:32] for h in hs] + [qi[:, 0:32]]
        qig.append(np.concatenate(cols, 1))
    out = np.concatenate([q, k, k, v, wi] + qig + [ki, ki, ki, ki], 1)
    assert out.shape[1] == HCOLS_A
    return np.ascontiguousarray(out)


def hyb_alloc(C):
    K = C.K
    S = C.S
    o = C.X[0].off
    C.XH = [K.sb(f"XH{i}", [128, NC8, NH], F32, offset=o + i * 8192) for i in range(2)]
    C.TAB = K.sb("TAB", [128, 4, NH], F32, offset=o + 16384)
    C.YAT = K.sb("YAT", [128, 8, NH], BF16, offset=o + 20480)
    C.QT = [K.sb(f"QT{i}", [128, 4, NH], BF16, offset=o + 24576 + i * 2048) for i in range(2)]
    C.YC = [K.sb(f"YC{i}", [128, 4, NH], BF16, offset=o + 28672 + i * 2048) for i in range(2)]
    o = C.TMP[0].off
    C.TMPH = [K.sb(f"TMPH{i}", [128, NH], F32, offset=o + i * 1024) for i in range(2)]
    C.ACCH = K.sb("ACCH", [128, NH], F32, offset=o + 2048)
    C.RSTDH = K.sb("RSTDH", [128, NH], F32, offset=o + 3072)
    C.HNH = K.sb("HNH", [128, NC8, NH], BF16, offset=o + 4096)
    C.RR = [K.sb(f"RR{i}", [128, 512], BF16, offset=o + 8192 + i * 1024) for i in range(4)]
    C.PTF = [K.sb("PTF0", [128, 1024], BF16, offset=o + 12288), None]
    C.T1 = K.sb("T1", [128, NH], F32, offset=o + 14336)
    assert o + 16384 <= C.arena
    K.sb_top = C.arena
    C.WINR = K.sb("WINR", [128, NC8, HCOLS_A], BF16)
    C.WOA = K.sb("WOA", [128, 8, D], BF16)
    C.WOC = K.sb("WOC", [128, 4, D], BF16)
    C.KT2 = K.sb("KT2", [128, S], BF16)
    C.KI3 = K.sb("KI3", [128, S], BF16)
    C.VA = K.sb("VA", [128, S // 128, 65], BF16)
    C.SS_ = K.sb("SS_", [128, 8192], F32)
    so = C.SS_.off
    C.WGU = K.sb("WGU", [128, NC8, 1536], BF16, offset=so)
    C.PBUF = K.sb("PBUF", [128, 4, NH + 2], F32, offset=so + 24576)
    C.GCS = K.sb("GCS", [128, NH], F32, offset=so + 24576 + 4160)
    C.CV = K.sb("CV", [128, NH], F32, offset=so + 24576 + 4160 + 1024)
    C.QF = K.sb("QF", [128, NH], F32, offset=so + 24576 + 4160 + 2048)
    assert 24576 + 4160 + 3072 <= 32768
    C.NEGM = K.sb("NEGM", [128, 8192], BF16)
    C.QIT = K.sb("QIT", [128, 3, NH], BF16)
    C.DG = K.sb("DG", [128, 8, 128], BF16)
    C.RDEN = K.sb("RDEN", [128, 1024], F32)
    C.RB = [K.sb(f"RB{i}", [128, 512], F32) for i in range(2)]
    C.HALO = K.sb("HALO", [128, 4, 2], F32)
    C.WI = K.sb("WI", [128, 2, 8], F32)
    C.WA = K.sb("WA", [128, 2, 8], F32)
    C.SGN = K.sb("SGN", [128, 2, 8], F32)
    C.ST = K.sb("ST", [128, 8], F32)
    C.JA = K.sb("JA", [128, 2688], BF16)
    C.RR += [K.sb(f"RR{i}", [128, 512], BF16) for i in (4, 5)]
    C.PTF[1] = K.sb("PTF1", [128, 1024], BF16)
    C.CST = K.sb("CST", [128, 896], F32)
    C.IDB = K.sb("IDB", [128, 512], BF16)
    print("hyb sbuf top", K.sb_top)
    assert K.sb_top <= 229344


def rmsnorm_h(C, xt, vcol):
    K = C.K
    nc = C.nc
    K.tt(C.ACCH[:, :], xt[:, 0, :], xt[:, 0, :], ALU.mult)
    for c in range(1, NC8):
        tmp = C.TMPH[c % 2]
        K.tt(tmp[:, :], xt[:, c, :], xt[:, c, :], ALU.mult)
        K.tt(C.ACCH[:, :], C.ACCH[:, :], tmp[:, :], ALU.add)
    ps = C.PS[6][:, 0:NH]
    K.mm(ps, C.ONES[:, :], C.ACCH[:, :])
    rs = C.RSTDH[:, :]
    K.ts(rs, ps, 1.0 / D, EPS, ALU.mult, ALU.add)
    K.op("dve", lambda: nc.vector.reciprocal(rs.ap, rs.ap), reads=[rs], writes=[rs])
    K.act(rs, rs, AF.Sqrt)
    for c in range(NC8):
        K.stt(C.HNH[:, c, :], xt[:, c, :], C.VEC[:, vcol + c:vcol + c + 1], rs, ALU.mult, ALU.mult)


def hyb_phase(C, src, src_name, dst, dst_name):
    K, nc = C.K, C.nc
    S = C.S
    nht = S // NH
    nblk = S // 128
    wa = C.w_hyb_a.rearrange("(c p) f -> p c f", p=128)
    for c in range(NC8):
        K.dma("pool", C.WINR[:, c, :], K.dview(wa[:, c, :], "d_w", 0, 1))
    woa = C.w_out[0:512, :].rearrange("(h p) m -> p h m", p=64)
    woc = C.w_out[512:1024, :].rearrange("(c p) m -> p c m", p=128)
    for h in range(8):
        K.dma("pool", C.WOA[0:64, h, :], K.dview(woa[:, h, :], "d_w", 0, 1))
    for c in range(4):
        K.dma("pool", C.WOC[:, c, :], K.dview(woc[:, c, :], "d_w", 0, 1))
    K.dma("sp", C.CST[:, :], K.dview(C.cst, "d_cst", 0, 1))
    K.copy(C.IDB[:, :], C.CST[:, 256:768])
    K.memset(C.VA[:, :, 64:65], 1.0)
    K.memset(C.HALO[:, :, :], 0.0)
    PMA = C.CST[:, 0:128]
    PMI = C.CST[:, 128:256]
    CAUS = C.CST[:, 768:896]
    wgu = C.w_in[:, GU0:GU0 + 1536].rearrange("(c p) f -> p c f", p=128)
    vnorm = VOFF["mix_norm"]
    vcw = VOFF["hyb_conv_w"]
    stores = []

    def rope(ps, ci, si, PM, out):
        K.copy(C.QF[:, :], ps, eng="act")
        ps2 = C.PS[4 + rope.n % 2][:, 0:NH]
        rope.n += 1
        K.mm(ps2, PM, C.QF[:, :])
        K.tt(C.T1[:, :], C.QF[:, :], C.TAB[:, ci, :], ALU.mult)
        K.tt(C.QF[:, :], ps2, C.TAB[:, si, :], ALU.mult)
        K.tt(out, C.T1[:, :], C.QF[:, :], ALU.add)
    rope.n = 0

    def proj(i):
        t0 = i * NH
        xt = C.XH[i % 2]
        K.dma("sp", xt[:, :, :], C.K.dview(src[:, t0:t0 + NH].rearrange("(c p) t -> p c t", p=128), src_name, t0, t0 + NH))
        K.dma("sp", C.TAB[:, :, :], K.dview(C.tab[:, :, t0:t0 + NH].rearrange("k p t -> p k t"), "d_tab", 0, 1))
        for c in range(NC8):
            K.dma("pool", C.WGU[:, c, :], K.dview(wgu[:, c, :], "d_w", 0, 1))
        rmsnorm_h(C, xt, vnorm)
        pi = [0]

        def nps(rows=128, cols=NH):
            p = C.PS[pi[0] % 4][0:rows, 0:cols]
            pi[0] += 1
            return p

        def fm(ps, W, col0, ncol):
            for k in range(NC8):
                K.mm(ps, W[:, k, col0:col0 + ncol], C.HNH[:, k, :], start=(k == 0), stop=(k == NC8 - 1))

        QT = C.QT[i % 2]
        for c in range(4):
            ps = nps()
            fm(ps, C.WINR, CQ + c * 128, 128)
            rope(ps, 0, 1, PMA, QT[:, c, :])
        ps = nps()
        fm(ps, C.WINR, CK2, 128)
        rope(ps, 0, 1, PMA, C.KT2[:, t0:t0 + NH])
        for g in range(3):
            ps = nps()
            fm(ps, C.WINR, CQI + g * 128, 128)
            rope(ps, 2, 3, PMI, C.QIT[:, g, :])
        ps = nps()
        fm(ps, C.WINR, CKI, 128)
        rope(ps, 2, 3, PMI, C.KI3[:, t0:t0 + NH])
        for q in range(2):
            ps = nps(128, 72)
            for k in range(NC8):
                K.mm(ps, C.HNH[:, k, q * 128:(q + 1) * 128], C.WINR[:, k, CVW:CVW + 72], start=(k == 0), stop=(k == NC8 - 1))
            ch = (t0 // 128) + q
            K.copy(C.VA[:, ch, 0:64], C.PS[(pi[0] - 1) % 4][:, 0:64])
            K.ts(C.WI[:, q, :], C.PS[(pi[0] - 1) % 4][:, 64:72], 1.0 / 16.0, None, ALU.mult)
            K.ts(C.SGN[:, q, :], C.WI[:, q, :], 0.0, 2.0, ALU.is_ge, ALU.mult)
            K.ts(C.SGN[:, q, :], C.SGN[:, q, :], -1.0, None, ALU.add)
            K.tt(C.WA[:, q, :], C.WI[:, q, :], C.SGN[:, q, :], ALU.mult)
        YC = C.YC[i % 2]
        for c in range(4):
            pgc = nps()
            fm(pgc, C.WGU, 512 + c * 128, 128)
            K.copy(C.GCS[:, :], pgc, eng="act")
            pu = nps()
            fm(pu, C.WGU, 1024 + c * 128, 128)
            K.copy(C.PBUF[:, c, 0:2], C.HALO[:, c, :])
            K.tt(C.PBUF[:, c, 2:2 + NH], pu, C.GCS[:, :], ALU.mult)
            K.copy(C.HALO[:, c, :], C.PBUF[:, c, NH:NH + 2])
            K.ts(C.CV[:, :], C.PBUF[:, c, 2:2 + NH], C.VEC[:, vcw + c * 3 + 2:vcw + c * 3 + 3], None, ALU.mult)
            K.stt(C.CV[:, :], C.PBUF[:, c, 1:1 + NH], C.VEC[:, vcw + c * 3 + 1:vcw + c * 3 + 2], C.CV[:, :], ALU.mult, ALU.add)
            K.stt(C.CV[:, :], C.PBUF[:, c, 0:NH], C.VEC[:, vcw + c * 3:vcw + c * 3 + 1], C.CV[:, :], ALU.mult, ALU.add)
            pgb = nps()
            fm(pgb, C.WGU, c * 128, 128)
            K.tt(YC[:, c, :], pgb, C.CV[:, :], ALU.mult)

    def score(b):
        L = 128 * (b + 1)
        qq = b % 2
        for h in range(8):
            K.ts(C.DG[:, h, :], C.IDB[:, 0:128], C.SGN[:, qq, h:h + 1], None, ALU.mult)
        nkt = (L + 511) // 512
        DB = [C.PS[0], C.PS[1], C.PS[2], C.PS[3], C.PS[6], C.PS[7]]
        LAG = 5
        pend = []

        def sum_mm(item):
            n, kt, hh, w = item
            pss = C.PS[4 + kt % 2][:, 0:w]
            K.mm(pss, C.DG[:, hh, :], C.RR[n % 6][:, 0:w], start=(hh == 0), stop=(hh == 7))
            if hh == 7:
                K.copy(C.SS_[:, 512 * kt:512 * kt + w], pss)

        groups = []
        n = 0
        for kt in range(nkt):
            w = min(512, L - 512 * kt)
            for hs in ((0, 1, 2), (3, 4, 5), (6, 7)):
                groups.append([(n + i, kt, h, w) for i, h in enumerate(hs)])
                n += len(hs)
        pendg = []
        for grp in groups:
            for (n_, kt, h, w) in grp:
                k0 = 512 * kt
                g, r = divmod(h, 3)
                ps = DB[n_ % 6][:, 0:w]
                K.mm(ps, C.QIT[32 * r:32 * r + 32, g, qq * 128:(qq + 1) * 128], C.KI3[32 * r:32 * r + 32, k0:k0 + w])
            for (n_, kt, h, w) in grp:
                ps = DB[n_ % 6][:, 0:w]
                if h in DVE_RELU_HEADS:
                    K.ts(C.RR[n_ % 6][:, 0:w], ps, C.WA[:, qq, h:h + 1], 0.0, ALU.mult, ALU.max)
                else:
                    K.act(C.RR[n_ % 6][:, 0:w], ps, AF.Relu, scale=C.WA[:, qq, h:h + 1])
            pendg.append(grp)
            if len(pendg) > 1:
                for item in pendg.pop(0):
                    sum_mm(item)
        for grp in pendg:
            for item in grp:
                sum_mm(item)
        K.tt(C.SS_[:, L - 128:L], C.SS_[:, L - 128:L], CAUS, ALU.add)

    def search_iters(b):
        L = 128 * (b + 1)
        LO = C.ST[:, 0:1]
        MID = C.ST[:, 1:2]
        CNTD = C.ST[:, 2:3]
        M = C.ST[:, 3:4]
        SGA = C.ST[:, 4:5]
        X = C.ST[:, 5:6]
        if b < 2:
            K.memset(LO, -1e29)
            return []
        L2 = 128 * int(ACT_FRAC * (b + 1))
        L1 = L - L2
        thr = float(TOPK) - 0.5 * L2
        K.memset(MID, 0.0)
        junk = View(C.T1.h[:, 0:1].broadcast_to([128, L1]), C.T1[:, 0:1].regs)
        its = []
        step = THR_B
        for it in range(N_ITER):
            nstep = step / 2.0

            def f(it=it, step=step, nstep=nstep):
                K.ts(junk, C.SS_[:, 0:L1], MID, 0.0, ALU.is_ge, ALU.add, accum=CNTD)
                if L2:
                    K.act(C.JA[:, 0:L2], C.SS_[:, L1:L], AF.Sign, bias=MID, scale=-1.0, accum=SGA)
                    K.stt(X, SGA, -0.5, CNTD, ALU.mult, ALU.add)
                    K.ts(M, X, thr, step, ALU.is_ge, ALU.mult)
                else:
                    K.ts(M, CNTD, thr, step, ALU.is_ge, ALU.mult)
                if it < N_ITER - 1:
                    K.stt(MID, M, -nstep, MID, ALU.add, ALU.add)
                else:
                    K.stt(LO, M, -step, MID, ALU.add, ALU.add)
            its.append(f)
            step = nstep
        return its

    def negm_piece(b, p):
        L = 128 * (b + 1)
        k0 = 512 * p
        w = min(512, L - k0)
        K.ts(C.NEGM[:, k0:k0 + w], C.SS_[:, k0:k0 + w], C.ST[:, 0:1], NEG, ALU.is_lt, ALU.mult)

    def npieces(b):
        return (128 * (b + 1) + 511) // 512

    import os
    dbg = os.environ.get("HYB_DBG", "psta3")

    def attn(b, nb=None):
        qq = b % 2
        QT = C.QT[(b // 2) % 2]
        OT = [C.PS[6], C.PS[7]]
        its = search_iters(nb) if nb is not None else []
        idone = 0

        def LM(j):
            psl = [C.PS[(2 * j) % 4], C.PS[(2 * j + 1) % 4]]
            for hl in range(4):
                for par in range(2):
                    base = 64 * par
                    K.mm(psl[par][:, hl * 128:(hl + 1) * 128], C.KT2[base:base + 64, 128 * j:128 * j + 128],
                         QT[base:base + 64, hl, qq * 128:(qq + 1) * 128], start=(hl == 0), stop=False)
            for par in range(2):
                K.mm(psl[par][:, :], C.NEGM[:, 128 * j:128 * j + 128], C.IDB[:, :], start=False, stop=True)

        LM(0)
        for j in range(b + 1):
            if j + 1 <= b:
                LM(j + 1)
            pk = j % 2
            pair = View(C.PSP[pk][:, :], [("ps", 2 * pk * 2048, (2 * pk + 2) * 2048)])
            ptf = C.PTF[j % 2]
            K.act(ptf[:, :], pair, AF.Exp, scale=0.125)
            for par in range(2):
                K.mm(OT[par][0:65, :], C.VA[:, j, 0:65], ptf[:, par * 512:(par + 1) * 512], start=(j == 0), stop=(j == b))
            want = (len(its) * (j + 1) + b) // (b + 1)
            while idone < min(want, len(its)):
                its[idone]()
                idone += 1
        while idone < len(its):
            its[idone]()
            idone += 1
        if nb is not None:
            for p in range(npieces(nb)):
                negm_piece(nb, p)
        if "3" not in dbg:
            return
        for par in range(2):
            rd = C.RDEN[64:65, par * 512:(par + 1) * 512]
            ot = OT[par]
            K.op("dve", (lambda rd=rd, ot=ot: nc.vector.reciprocal(rd.ap, ot[64:65, :].ap)), reads=[ot[64:65, :]], writes=[rd])
            pb = C.PS[4 + par][0:64, :]
            K.mm(pb, C.ONES[64:65, 0:64], rd)
            rb = C.RB[par]
            K.copy(rb[0:64, :], pb, eng="act")
            yv = C.YAT[0:64, 4 * par:4 * par + 4, qq * 128:(qq + 1) * 128]
            otv = View(ot[0:64, :].ap.rearrange("p (h t) -> p h t", h=4), ot[0:64, :].regs)
            rbv = View(rb.h[0:64, :].rearrange("p (h t) -> p h t", h=4), rb[0:64, :].regs)
            K.tt(yv, otv, rbv, ALU.mult)

    def outproj(i):
        t0 = i * NH
        xt = C.XH[i % 2]
        YC = C.YC[i % 2]
        for m in range(NC8):
            po = C.PS[m % 4][:, 0:NH]
            for h in range(8):
                K.mm(po, C.WOA[0:64, h, m * 128:(m + 1) * 128], C.YAT[0:64, 4 * (h % 2) + h // 2, :], start=(h == 0), stop=False)
            for c in range(4):
                K.mm(po, C.WOC[:, c, m * 128:(m + 1) * 128], YC[:, c, :], start=False, stop=(c == 3))
            K.tt(xt[:, m, :], xt[:, m, :], po, ALU.add)
        stores.append(K.dma("act", K.dview(dst[:, t0:t0 + NH].rearrange("(c p) t -> p c t", p=128), dst_name, t0, t0 + NH), xt[:, :, :]))

    for i in range(nht):
        proj(i)
        for b in (2 * i, 2 * i + 1):
            score(b)
            if b == 0:
                search_iters(0)
                for p in range(npieces(0)):
                    negm_piece(0, p)
            else:
                attn(b - 1, b)
                if (b - 1) % 2 == 1:
                    outproj((b - 1) // 2)
    attn(nblk - 1, None)
    outproj(nht - 1)
    return stores


PHASES = [
    ("ffn", "xT", "d_x", "hA", "d_h", 0, 0, VOFF["ffn_norm"] + 0),
    ("hyb", "hA", "d_h", "hA", "d_h"),
    ("ffn", "hA", "d_h", "hA", "d_h", 0, 1, VOFF["ffn_norm"] + 8),
    ("ffn", "hA", "d_h", "hA", "d_h", 1, 0, VOFF["ffn_norm"] + 16),
    ("conf", "hA", "d_h", "hA", "d_h"),
    ("ffn", "hA", "d_h", "outT", "d_out", 1, 1, VOFF["ffn_norm"] + 24, VOFF["final_norm"]),
]

_CACHE = {}


def host_inputs(inputs, S):
    f = lambda a: np.ascontiguousarray(np.asarray(a, np.float32))
    tab, cst = hyb_consts(S)
    shared = {
        "vecs": pack_vecs(inputs),
        "ffn_w_gate": f(inputs["ffn_w_gate"]), "ffn_w_up": f(inputs["ffn_w_up"]), "ffn_w_down": f(inputs["ffn_w_down"]),
        "conf_w_pw1": f(np.asarray(inputs["conf_w_pw1"])[0]), "conf_w_pw2": f(np.asarray(inputs["conf_w_pw2"])[0]),
        "hyb_w_in": f(np.asarray(inputs["hyb_w_in"])[0]), "w_hyb_a": pack_hyb_w(np.asarray(inputs["hyb_w_in"])[0]),
        "hyb_w_out": f(np.asarray(inputs["hyb_w_out"])[0]), "rope_tab": tab, "hyb_cst": cst,
    }
    return shared


def kernel(**inputs):
    x = np.asarray(inputs["x"], np.float32)
    B, S, _ = x.shape
    if S not in _CACHE:
        _CACHE[S] = build(S, PHASES)
    nc = _CACHE[S]
    shared = host_inputs(inputs, S)
    in_maps = []
    for b in range(B):
        m = dict(shared)
        m["xT"] = np.ascontiguousarray(x[b].T)
        in_maps.append(m)
    res = run_bass_kernel_spmd(nc, in_maps, core_ids=list(range(B)))
    out = np.stack([np.ascontiguousarray(r["outT"].T) for r in res.results], 0)
    return out.astype(np.float32)
```

```python
import numpy as np
import concourse.bass as bass
import concourse.mybir as mybir

F32 = mybir.dt.float32
BF16 = mybir.dt.bfloat16
AF = mybir.ActivationFunctionType
ALU = mybir.AluOpType
AX = mybir.AxisListType

DT_SIZE = {F32: 4, BF16: 2}
SEM_LIMIT = 2000
N_DMA_SEMS = 16


class View:
    __slots__ = ("ap", "regs")

    def __init__(self, ap, regs):
        self.ap = ap
        self.regs = regs


class Space:
    def __init__(self):
        self.b = [0, 1 << 60]
        self.w = [None]
        self.r = [[]]

    def _split(self, x):
        import bisect
        i = bisect.bisect_left(self.b, x)
        if self.b[i] == x:
            return i
        self.b.insert(i, x)
        self.w.insert(i, self.w[i - 1])
        self.r.insert(i, list(self.r[i - 1]))
        return i

    def rng(self, lo, hi):
        i = self._split(lo)
        j = self._split(hi)
        return range(i, j)


class Op:
    __slots__ = ("eng", "fn", "deps", "needed", "ticket", "dma", "idx")

    def __init__(self, eng, fn, dma):
        self.eng = eng
        self.fn = fn
        self.deps = []
        self.needed = False
        self.ticket = None
        self.dma = dma


class SBT:
    def __init__(self, K, name, shape, dtype, offset=None):
        self.K = K
        self.shape = list(shape)
        self.dtype = dtype
        es = DT_SIZE[dtype]
        nfree = int(np.prod(shape[1:]))
        if offset is None:
            offset = K.sb_alloc(nfree * es)
        self.off = offset
        self.es = es
        self.h = K.nc.alloc_sbuf_tensor_at(name, list(shape), dtype, offset=offset)
        st = []
        acc = 1
        for d in reversed(shape[1:]):
            st.append(acc)
            acc *= d
        self.strides = list(reversed(st))

    def __getitem__(self, idx):
        if not isinstance(idx, tuple):
            idx = (idx,)
        idx = tuple(idx) + (slice(None),) * (len(self.shape) - len(idx))
        lo = 0
        hi = 0
        for k, (ix, d) in enumerate(zip(idx[1:], self.shape[1:])):
            s = self.strides[k]
            if isinstance(ix, int):
                a, b = ix, ix + 1
            else:
                a, b, step = ix.indices(d)
                assert step == 1
            lo += a * s
            hi += (b - 1) * s
        hi += 1
        ap = self.h[idx]
        return View(ap, [("sb", self.off + lo * self.es, self.off + hi * self.es)])


class PST:
    def __init__(self, K, name, bank, handle=None, coff=0):
        self.K = K
        self.bank = bank
        self.coff = coff
        self.h = handle if handle is not None else K.nc.alloc_psum_tensor(name, [128, 512], F32)

    def __getitem__(self, idx):
        if not isinstance(idx, tuple):
            idx = (idx,)
        idx = tuple(idx) + (slice(None),) * (2 - len(idx))
        a, b, _ = idx[1].indices(512)
        return View(self.h[idx[0], self.coff + a:self.coff + b], [("ps", self.bank * 2048, self.bank * 2048 + 2048)])


class Kern:
    def __init__(self, nc):
        self.nc = nc
        self.ops = []
        self.spaces = {}
        self.sb_top = 16512
        self.eng = {"pe": nc.tensor, "act": nc.scalar, "dve": nc.vector,
                    "pool": nc.gpsimd, "sp": nc.sync}

    def sb_alloc(self, nbytes):
        off = (self.sb_top + 63) // 64 * 64
        self.sb_top = off + nbytes
        return off

    def sb(self, name, shape, dtype, offset=None):
        return SBT(self, name, shape, dtype, offset)

    def dview(self, ap, space, lo, hi):
        return View(ap, [(space, lo, hi)])

    def _sp(self, name):
        if name not in self.spaces:
            self.spaces[name] = Space()
        return self.spaces[name]

    def op(self, eng, fn, reads=(), writes=(), dma=False):
        o = Op(eng, fn, dma)
        o.idx = len(self.ops)
        deps = {}

        def add(d, raw):
            if d is None:
                return
            if not d.dma and not o.dma and d.eng == eng:
                if eng == "pe" or not raw:
                    return
            deps[d.idx] = d

        for v in reads:
            for (s, lo, hi) in v.regs:
                sp = self._sp(s)
                for i in sp.rng(lo, hi):
                    add(sp.w[i], True)
        for v in writes:
            for (s, lo, hi) in v.regs:
                sp = self._sp(s)
                for i in sp.rng(lo, hi):
                    add(sp.w[i], False)
                    for r in sp.r[i]:
                        add(r, False)
        for v in reads:
            for (s, lo, hi) in v.regs:
                sp = self._sp(s)
                for i in sp.rng(lo, hi):
                    sp.r[i].append(o)
        for v in writes:
            for (s, lo, hi) in v.regs:
                sp = self._sp(s)
                for i in sp.rng(lo, hi):
                    sp.w[i] = o
                    sp.r[i] = []
        o.deps = list(deps.values())
        for d in o.deps:
            d.needed = True
        self.ops.append(o)
        return o

    def mm(self, out, lhsT, rhs, start=True, stop=True):
        nc = self.nc
        return self.op("pe", lambda: nc.tensor.matmul(out.ap, lhsT.ap, rhs.ap, start=start, stop=stop),
                       reads=[lhsT, rhs], writes=[out])

    def act(self, out, in_, func, bias=None, scale=None, accum=None, eng="act"):
        nc = self.nc
        kw = {}
        rd = [in_]
        wr = [out]
        if bias is not None:
            if isinstance(bias, View):
                kw["bias"] = bias.ap
                rd.append(bias)
            else:
                kw["bias"] = bias
        if scale is not None:
            if isinstance(scale, View):
                kw["scale"] = scale.ap
                rd.append(scale)
            else:
                kw["scale"] = scale
        if accum is not None:
            kw["accum_out"] = accum.ap
            wr.append(accum)
        return self.op("act", lambda: nc.scalar.activation(out.ap, in_.ap, func, **kw), reads=rd, writes=wr)

    def _v(self, eng):
        return self.nc.vector if eng == "dve" else self.nc.gpsimd

    def tt(self, out, in0, in1, op, eng="dve"):
        e = self._v(eng)
        return self.op(eng, lambda: e.tensor_tensor(out.ap, in0.ap, in1.ap, op), reads=[in0, in1], writes=[out])

    def ts(self, out, in0, s1, s2, op0, op1=None, accum=None, eng="dve"):
        e = self._v(eng)
        rd = [in0]
        wr = [out]
        a1 = s1
        a2 = s2
        if isinstance(s1, View):
            rd.append(s1)
            a1 = s1.ap
        if isinstance(s2, View):
            rd.append(s2)
            a2 = s2.ap
        kw = {}
        if op1 is not None:
            kw["op1"] = op1
        if accum is not None:
            kw["accum_out"] = accum.ap
            wr.append(accum)
        return self.op(eng, lambda: e.tensor_scalar(out.ap, in0.ap, a1, a2, op0, **kw), reads=rd, writes=wr)

    def stt(self, out, in0, scalar, in1, op0, op1, eng="dve"):
        e = self._v(eng)
        rd = [in0, in1]
        a = scalar
        if isinstance(scalar, View):
            rd.append(scalar)
            a = scalar.ap
        return self.op(eng, lambda: e.scalar_tensor_tensor(out.ap, in0.ap, a, in1.ap, op0, op1),
                       reads=rd, writes=[out])

    def copy(self, out, in_, eng="dve"):
        if eng == "act":
            nc = self.nc
            return self.op("act", lambda: nc.scalar.copy(out.ap, in_.ap), reads=[in_], writes=[out])
        e = self._v(eng)
        return self.op(eng, lambda: e.tensor_copy(out.ap, in_.ap), reads=[in_], writes=[out])

    def memset(self, out, val, eng="dve"):
        e = self._v(eng)
        return self.op(eng, lambda: e.memset(out.ap, val), writes=[out])

    def dma(self, q, out, in_):
        e = self.eng[q]
        return self.op(q, lambda: e.dma_start(out=out.ap, in_=in_.ap), reads=[in_], writes=[out], dma=True)

    def emit(self, final_waits=()):
        nc = self.nc
        import contextlib
        self._stack = contextlib.ExitStack()
        sems = []

        def new_sem(nm):
            s = self._stack.enter_context(nc.semaphore(nm))
            sems.append(s)
            return s

        cur = {}
        nsem = [0]
        dsem = [[new_sem(f"dq{i}"), 0] for i in range(N_DMA_SEMS)]
        dnext = [0]
        waited = {}

        def wait(engname, sem, val):
            key = (engname, id(sem))
            if waited.get(key, 0) >= val:
                return
            waited[key] = val
            self.eng[engname].wait_ge(sem, val)

        nw = 0
        for o in self.ops:
            for d in o.deps:
                sem, val = d.ticket
                wait(o.eng, sem, val)
            if o.dma:
                k = dnext[0] % N_DMA_SEMS
                dnext[0] += 1
                sem, cnt = dsem[k]
                if cnt:
                    wait(o.eng, sem, cnt)
                ins = o.fn()
                ins.then_inc(sem, 16)
                dsem[k][1] = cnt + 16
                o.ticket = (sem, cnt + 16)
            else:
                ins = o.fn()
                if o.needed:
                    c = cur.get(o.eng)
                    if c is None or c[1] >= SEM_LIMIT:
                        nsem[0] += 1
                        c = [new_sem(f"s_{o.eng}{nsem[0]}"), 0]
                        cur[o.eng] = c
                    c[1] += 1
                    ins.then_inc(c[0], 1)
                    o.ticket = (c[0], c[1])
        for d in final_waits:
            sem, val = d.ticket
            wait("sp", sem, val)
        self.n_sems = len(sems)
        return len(self.ops)

from concourse.bass_utils import run_bass_kernel_spmd

D = 1024
DFF = 2816
NF = DFF // 128
NC8 = D // 128
NT = 512
EPS = 1e-6


class Ctx:
    pass


def build(S, phases, n_vec_cols=None):
    if n_vec_cols is None:
        n_vec_cols = NVEC
    nc = bass.Bass("TRN2", target_bir_lowering=False)
    K = Kern(nc)
    C = Ctx()
    C.nc, C.K, C.S = nc, K, S
    C.ntiles = S // NT

    def din(name, shape, dt=F32):
        return nc.dram_tensor(name, list(shape), dt, kind="ExternalInput").ap()

    C.xT = din("xT", [D, S])
    C.vecs = din("vecs", [128, n_vec_cols])
    C.wg = din("ffn_w_gate", [2, 2, D, DFF])
    C.wu = din("ffn_w_up", [2, 2, D, DFF])
    C.wd = din("ffn_w_down", [2, 2, DFF, D])
    C.w_pw1 = din("conf_w_pw1", [D, 2 * D])
    C.w_pw2 = din("conf_w_pw2", [D, D])
    C.w_in = din("hyb_w_in", [D, 2472])
    C.w_hyb_a = din("w_hyb_a", [D, HCOLS_A])
    C.w_out = din("hyb_w_out", [D, D])
    C.tab = din("rope_tab", [4, 128, S])
    C.cst = din("hyb_cst", [128, 896])
    C.outT = nc.dram_tensor("outT", [D, S], F32, kind="ExternalOutput").ap()
    C.hA = nc.dram_tensor("hA", [D, S], F32, kind="Internal").ap()

    C.VEC = K.sb("VEC", [128, n_vec_cols], F32)
    C.ONES = K.sb("ONES", [128, 128], F32)
    C.X = [K.sb(f"X{i}", [128, NC8, NT], F32) for i in range(2)]
    C.TMP = [K.sb(f"TMP{i}", [128, NT], F32) for i in range(2)]
    C.ACC = K.sb("ACC", [128, NT], F32)
    C.RSTD = K.sb("RSTD", [128, NT], F32)
    C.HN = K.sb("HN", [128, NC8, NT], BF16)
    C.arena = K.sb_top
    C.PSP = [nc.alloc_psum_tensor(f"PSP{i}", [128, 1024], F32) for i in range(4)]
    C.PS = [PST(K, f"PS{i}", i, C.PSP[i // 2], (i % 2) * 512) for i in range(8)]

    K.dma("sp", C.VEC[:, :], K.dview(C.vecs, "d_vecs", 0, 1))
    K.memset(C.ONES[:, :], 1.0)

    ffn_alloc(C)
    conf_alloc(C)
    hyb_alloc(C)
    last = None
    for ph in phases:
        if ph[0] == "ffn":
            last = ffn_phase(C, getattr(C, ph[1]), ph[2], getattr(C, ph[3]), *ph[4:])
        elif ph[0] == "hyb":
            last = hyb_phase(C, getattr(C, ph[1]), ph[2], getattr(C, ph[3]), ph[4])
        elif ph[0] == "conf":
            last = conf_phase(C, getattr(C, ph[1]), ph[2], getattr(C, ph[3]), ph[4])
    n = K.emit(final_waits=last)
    print("ops", n, "sems", K.n_sems, "sbuf top", K.sb_top)
    return nc

def ffn_alloc(C):
    K = C.K
    K.sb_top = C.arena
    C.WG = K.sb("WG", [128, NC8, DFF], BF16)
    C.WU = K.sb("WU", [128, NC8, DFF], BF16)
    C.WD = K.sb("WD", [128, NF, D], BF16)
    C.G = K.sb("G", [128, NF, NT], BF16)
    C.SILU = [K.sb(f"SILU{i}", [128, NT], BF16) for i in range(2)]
    C.OUTT = K.sb("OUTT", [128, NC8, NT], F32, offset=C.G.off)


def dram_tile(C, ap, name, i):
    t0 = i * NT
    return C.K.dview(ap[:, t0:t0 + NT].rearrange("(c p) t -> p c t", p=128), name, t0, t0 + NT)


def rmsnorm(C, xt, vcol, out, part="ab"):
    K = C.K
    if "a" in part:
        K.tt(C.ACC[:, :], xt[:, 0, :], xt[:, 0, :], ALU.mult)
        for c in range(1, NC8):
            tmp = C.TMP[c % 2]
            K.tt(tmp[:, :], xt[:, c, :], xt[:, c, :], ALU.mult)
            K.tt(C.ACC[:, :], C.ACC[:, :], tmp[:, :], ALU.add)
    if "b" not in part:
        return
    ps = C.PS[6][:, :]
    K.mm(ps, C.ONES[:, :], C.ACC[:, :])
    K.ts(C.RSTD[:, :], ps, 1.0 / D, EPS, ALU.mult, ALU.add)
    nc = C.nc
    rs = C.RSTD[:, :]
    K.op("dve", lambda: nc.vector.reciprocal(rs.ap, rs.ap), reads=[rs], writes=[rs])
    K.act(rs, rs, AF.Sqrt)
    for c in range(NC8):
        K.stt(out[:, c, :], xt[:, c, :], C.VEC[:, vcol + c:vcol + c + 1], C.RSTD[:, :], ALU.mult, ALU.mult)


def ffn_phase(C, src, src_name, dst, dst_name, l, j, vcol, final_vcol=None):
    K, nc = C.K, C.nc
    wg = C.wg[l, j].rearrange("(c p) f -> p c f", p=128)
    wu = C.wu[l, j].rearrange("(c p) f -> p c f", p=128)
    wd = C.wd[l, j].rearrange("(c p) m -> p c m", p=128)
    NCB = 4
    cbw = DFF // NCB
    for cb in range(NCB):
        c0 = cb * cbw
        for c in range(NC8):
            K.dma("pool", C.WG[:, c, c0:c0 + cbw], K.dview(wg[:, c, c0:c0 + cbw], "d_w", 0, 1))
            K.dma("pool", C.WU[:, c, c0:c0 + cbw], K.dview(wu[:, c, c0:c0 + cbw], "d_w", 0, 1))
    for c in range(NF):
        K.dma("pool", C.WD[:, c, :], K.dview(wd[:, c, :], "d_w", 0, 1))
    stores = []
    nt = C.ntiles
    K.dma("sp", C.X[0][:, :, :], dram_tile(C, src, src_name, 0))
    rmsnorm(C, C.X[0], vcol, C.HN)
    for i in range(nt):
        xt = C.X[i % 2]
        if i + 1 < nt:
            K.dma("sp", C.X[(i + 1) % 2][:, :, :], dram_tile(C, src, src_name, i + 1))
        for f in range(NF):
            pg = C.PS[f % 2][:, :]
            pu = C.PS[2 + f % 2][:, :]
            for c in range(NC8):
                K.mm(pg, C.WG[:, c, f * 128:(f + 1) * 128], C.HN[:, c, :], start=(c == 0), stop=(c == NC8 - 1))
            for c in range(NC8):
                K.mm(pu, C.WU[:, c, f * 128:(f + 1) * 128], C.HN[:, c, :], start=(c == 0), stop=(c == NC8 - 1))
            sl = C.SILU[f % 2]
            K.act(sl[:, :], pg, AF.Silu)
            K.tt(C.G[:, f, :], sl[:, :], pu, ALU.mult)
        if i + 1 < nt:
            rmsnorm(C, C.X[(i + 1) % 2], vcol, C.HN, part="a")
        for m in range(NC8):
            po = C.PS[4 + m % 2][:, :]
            for f in range(NF):
                K.mm(po, C.WD[:, f, m * 128:(m + 1) * 128], C.G[:, f, :], start=(f == 0), stop=(f == NF - 1))
            K.stt(xt[:, m, :], po, 0.5, xt[:, m, :], ALU.mult, ALU.add)
            if m == 3 and i + 1 < nt:
                rmsnorm(C, C.X[(i + 1) % 2], vcol, C.HN, part="b")
        if final_vcol is None:
            stores.append(K.dma("act", dram_tile(C, dst, dst_name, i), xt[:, :, :]))
        else:
            rmsnorm(C, xt, final_vcol, C.OUTT)
            stores.append(K.dma("act", dram_tile(C, dst, dst_name, i), C.OUTT[:, :, :]))
    return stores


VEC_SPEC = [("ffn_norm", 4 * 8), ("mix_norm", 2 * 8), ("final_norm", 8), ("conf_b_pw1", 16), ("conf_conv_b", 8),
            ("conf_ln_g", 8), ("conf_ln_b", 8), ("conf_b_pw2", 8), ("conf_conv_w", 8 * 31), ("hyb_conv_w", 4 * 3)]
VOFF = {}
_o = 0
for _n, _c in VEC_SPEC:
    VOFF[_n] = _o
    _o += _c
NVEC = _o


def pack_vecs(inp):
    v = np.zeros((128, NVEC), np.float32)

    def put(name, arr):
        a = np.asarray(arr, np.float32).reshape(-1)
        n = a.size // 128
        v[:, VOFF[name]:VOFF[name] + n] = a.reshape(n, 128).T

    put("ffn_norm", inp["ffn_norm"])
    put("mix_norm", inp["mix_norm"])
    put("final_norm", inp["final_norm"])
    put("conf_b_pw1", inp["conf_b_pw1"])
    put("conf_conv_b", inp["conf_conv_b"])
    put("conf_ln_g", inp["conf_ln_g"])
    put("conf_ln_b", inp["conf_ln_b"])
    put("conf_b_pw2", inp["conf_b_pw2"])
    cw = np.asarray(inp["conf_conv_w"], np.float32).reshape(31, 8, 128)
    v[:, VOFF["conf_conv_w"]:VOFF["conf_conv_w"] + 248] = cw.transpose(2, 1, 0).reshape(128, 248)
    hw = np.asarray(inp["hyb_conv_w"], np.float32).reshape(3, 4, 128)
    v[:, VOFF["hyb_conv_w"]:VOFF["hyb_conv_w"] + 12] = hw.transpose(2, 1, 0).reshape(128, 12)
    return v


CK = 31
HALO = CK - 1
CONV_POOL_CHUNKS = 0


def conf_alloc(C):
    K = C.K
    K.sb_top = C.arena
    C.W1 = K.sb("W1", [128, NC8, 2 * D], BF16)
    C.W2 = K.sb("W2", [128, NC8, D], BF16)
    C.U = K.sb("U", [128, NC8, HALO + NT], BF16)
    C.Y = K.sb("Y", [128, NC8, NT], F32)
    C.V = K.sb("V", [128, NC8, NT], BF16)
    C.SG = [K.sb(f"SG{i}", [128, NT], F32) for i in range(2)]
    C.A1 = K.sb("A1", [128, NT], F32)
    C.A2 = K.sb("A2", [128, NT], F32)
    C.MEAN = K.sb("MEAN", [128, NT], F32)
    C.DIAG = K.sb("DIAG", [128, NC8, CK, 128], BF16)
    C.IDF = K.sb("IDF", [128, 128], F32)
    C.IDC = K.sb("IDC", [128, 128], BF16)
    print("conf sbuf top", K.sb_top)
    assert K.sb_top <= 229344


def conf_phase(C, src, src_name, dst, dst_name):
    K, nc = C.K, C.nc
    import os
    cd = os.environ.get('CONF_DBG', 'dngclp')
    w1 = C.w_pw1.rearrange("(c p) f -> p c f", p=128)
    w2 = C.w_pw2.rearrange("(c p) f -> p c f", p=128)
    for c in range(NC8):
        K.dma("pool", C.W1[:, c, :], K.dview(w1[:, c, :], "d_w", 0, 1))
    for c in range(NC8):
        K.dma("pool", C.W2[:, c, :], K.dview(w2[:, c, :], "d_w", 0, 1))
    vb1 = VOFF["conf_b_pw1"]
    vcb = VOFF["conf_conv_b"]
    vg, vb = VOFF["conf_ln_g"], VOFF["conf_ln_b"]
    vb2 = VOFF["conf_b_pw2"]
    vcw = VOFF["conf_conv_w"]
    vnorm = VOFF["mix_norm"] + 8
    stores = []
    nt = C.ntiles
    K.dma("sp", C.IDF[:, :], K.dview(C.cst[:, 256:384], "d_cst", 0, 1))
    K.copy(C.IDC[:, :], C.IDF[:, :])
    for c in (range(NC8) if 'd' in cd else []):
        for j in range(CK):
            K.ts(C.DIAG[:, c, j, :], C.IDC[:, :], C.VEC[:, vcw + c * CK + j:vcw + c * CK + j + 1], None, ALU.mult)
    K.dma("sp", C.X[0][:, :, :], dram_tile(C, src, src_name, 0))
    U = C.U
    rmsnorm(C, C.X[0], vnorm, C.HN)
    for i in range(nt):
        xt = C.X[i % 2]
        if i + 1 < nt:
            K.dma("sp", C.X[(i + 1) % 2][:, :, :], dram_tile(C, src, src_name, i + 1))
        if i == 0:
            K.memset(U[:, :, 0:HALO], 0.0)
        else:
            for c in range(NC8):
                K.copy(U[:, c, 0:HALO], U[:, c, NT:NT + HALO])
        for c in (range(NC8) if 'g' in cd else []):
            pa = C.PS[c % 2][:, :]
            pg = C.PS[2 + c % 2][:, :]
            for k in range(NC8):
                K.mm(pa, C.W1[:, k, c * 128:(c + 1) * 128], C.HN[:, k, :], start=(k == 0), stop=(k == NC8 - 1))
            for k in range(NC8):
                K.mm(pg, C.W1[:, k, D + c * 128:D + (c + 1) * 128], C.HN[:, k, :], start=(k == 0), stop=(k == NC8 - 1))
            sg = C.SG[c % 2]
            K.act(sg[:, :], pg, AF.Sigmoid, bias=C.VEC[:, vb1 + 8 + c:vb1 + 9 + c])
            K.stt(U[:, c, HALO:HALO + NT], pa, C.VEC[:, vb1 + c:vb1 + c + 1], sg[:, :], ALU.add, ALU.mult)
        if i + 1 < nt:
            rmsnorm(C, C.X[(i + 1) % 2], vnorm, C.HN, part="a")
        for c in (range(NC8) if 'c' in cd else []):
            pc = C.PS[4 + c % 2][:, :]
            for j in range(CK):
                K.mm(pc, C.DIAG[:, c, j, :], U[:, c, j:j + NT], start=(j == 0), stop=(j == CK - 1))
            K.ts(C.Y[:, c, :], pc, C.VEC[:, vcb + c:vcb + c + 1], None, ALU.add)
            if c == 3 and i + 1 < nt:
                rmsnorm(C, C.X[(i + 1) % 2], vnorm, C.HN, part="b")
        if 'l' not in cd:
            stores.append(K.dma("act", dram_tile(C, dst, dst_name, i), xt[:, :, :]))
            continue
        K.copy(C.A1[:, :], C.Y[:, 0, :])
        K.tt(C.A2[:, :], C.Y[:, 0, :], C.Y[:, 0, :], ALU.mult)
        for c in range(1, NC8):
            tmp = C.TMP[c % 2]
            K.tt(tmp[:, :], C.Y[:, c, :], C.Y[:, c, :], ALU.mult)
            K.tt(C.A1[:, :], C.A1[:, :], C.Y[:, c, :], ALU.add)
            K.tt(C.A2[:, :], C.A2[:, :], tmp[:, :], ALU.add)
        p1 = C.PS[6][:, :]
        p2 = C.PS[7][:, :]
        K.mm(p1, C.ONES[:, :], C.A1[:, :])
        K.mm(p2, C.ONES[:, :], C.A2[:, :])
        K.ts(C.MEAN[:, :], p1, 1.0 / D, None, ALU.mult)
        K.tt(C.A1[:, :], C.MEAN[:, :], C.MEAN[:, :], ALU.mult)
        K.stt(C.A2[:, :], p2, 1.0 / D, C.A1[:, :], ALU.mult, ALU.subtract)
        K.ts(C.A2[:, :], C.A2[:, :], EPS, None, ALU.add)
        a2 = C.A2[:, :]
        K.op("dve", lambda: nc.vector.reciprocal(a2.ap, a2.ap), reads=[a2], writes=[a2])
        K.act(C.RSTD[:, :], a2, AF.Sqrt)
        for c in range(NC8):
            K.tt(C.Y[:, c, :], C.Y[:, c, :], C.MEAN[:, :], ALU.subtract)
            K.tt(C.Y[:, c, :], C.Y[:, c, :], C.RSTD[:, :], ALU.mult)
            K.act(C.V[:, c, :], C.Y[:, c, :], AF.Silu, bias=C.VEC[:, vb + c:vb + c + 1],
                  scale=C.VEC[:, vg + c:vg + c + 1])
        for m in (range(NC8) if 'p' in cd else []):
            po = C.PS[4 + m % 2][:, :]
            for k in range(NC8):
                K.mm(po, C.W2[:, k, m * 128:(m + 1) * 128], C.V[:, k, :], start=(k == 0), stop=(k == NC8 - 1))
            K.stt(xt[:, m, :], po, C.VEC[:, vb2 + m:vb2 + m + 1], xt[:, m, :], ALU.add, ALU.add)
        stores.append(K.dma("act", dram_tile(C, dst, dst_name, i), xt[:, :, :]))
    return stores


NH = 256
HCOLS_A = 1224
CQ, CK2, CVW, CQI, CKI = 0, 512, 640, 712, 1096
GU0 = 936
N_ITER = 17
THR_B = 8.0
ACT_FRAC = 0.34
DVE_RELU_HEADS = (1, 4, 6)
TOPK = 256
NEG = -30000.0


def hyb_consts(S):
    theta = np.float32(500000.0)
    t = np.arange(S, dtype=np.float32)

    def tabs(half, period, nrep):
        inv = (theta ** (-(np.arange(half, dtype=np.float32) / np.float32(half)))).astype(np.float32)
        ang = (t[:, None] * inv[None, :]).astype(np.float32)
        c, s = np.cos(ang).astype(np.float32).T, np.sin(ang).astype(np.float32).T
        CC = np.ones((128, S), np.float32)
        SS = np.zeros((128, S), np.float32)
        for base in range(0, 128, period):
            CC[base:base + half] = c
            CC[base + half:base + 2 * half] = c
            SS[base:base + half] = -s
            SS[base + half:base + 2 * half] = s
        return CC, SS

    CCa, SSa = tabs(8, 64, 2)
    CCi, SSi = tabs(4, 32, 4)
    tab = np.stack([CCa, SSa, CCi, SSi], 0)

    def perm(half, period):
        P = np.zeros((128, 128), np.float32)
        for m in range(128):
            r = m % period
            if r < half:
                P[m + half, m] = 1.0
            elif r < 2 * half:
                P[m - half, m] = 1.0
        return P

    cst = np.zeros((128, 128 + 128 + 512 + 128), np.float32)
    cst[:, 0:128] = perm(8, 64)
    cst[:, 128:256] = perm(4, 32)
    cst[:, 256:768] = np.tile(np.eye(128, dtype=np.float32), (1, 4))
    ii = np.arange(128)
    cst[:, 768:896] = np.where(ii[None, :] <= ii[:, None], 0.0, -1e30).astype(np.float32)
    return tab, cst


def pack_hyb_w(w_in):
    w = np.asarray(w_in, np.float32)
    q = w[:, 0:512]
    k = w[:, 512:576]
    v = w[:, 576:640]
    qi = w[:, 640:896]
    ki = w[:, 896:928]
    wi = w[:, 928:936]
    qig = []
    for g in range(3):
        hs = [3 * g, 3 * g + 1, 3 * g + 2]
        cols = [qi[:, 32 * h:32 * h + 32] if h < 8 else qi[:, 0:32] for h in hs] + [qi[:, 0:32]]
        qig.append(np.concatenate(cols, 1))
    out = np.concatenate([q, k, k, v, wi] + qig + [ki, ki, ki, ki], 1)
    assert out.shape[1] == HCOLS_A
    return np.ascontiguousarray(out)


def hyb_alloc(C):
    K = C.K
    S = C.S
    o = C.X[0].off
    C.XH = [K.sb(f"XH{i}", [128, NC8, NH], F32, offset=o + i * 8192) for i in range(2)]
    C.TAB = K.sb("TAB", [128, 4, NH], F32, offset=o + 16384)
    C.YAT = K.sb("YAT", [128, 8, NH], BF16, offset=o + 20480)
    C.QT = [K.sb(f"QT{i}", [128, 4, NH], BF16, offset=o + 24576 + i * 2048) for i in range(2)]
    C.YC = [K.sb(f"YC{i}", [128, 4, NH], BF16, offset=o + 28672 + i * 2048) for i in range(2)]
    o = C.TMP[0].off
    C.TMPH = [K.sb(f"TMPH{i}", [128, NH], F32, offset=o + i * 1024) for i in range(2)]
    C.ACCH = K.sb("ACCH", [128, NH], F32, offset=o + 2048)
    C.RSTDH = K.sb("RSTDH", [128, NH], F32, offset=o + 3072)
    C.HNH = K.sb("HNH", [128, NC8, NH], BF16, offset=o + 4096)
    C.RR = [K.sb(f"RR{i}", [128, 512], BF16, offset=o + 8192 + i * 1024) for i in range(4)]
    C.PTF = [K.sb("PTF0", [128, 1024], BF16, offset=o + 12288), None]
    C.T1 = K.sb("T1", [128, NH], F32, offset=o + 14336)
    assert o + 16384 <= C.arena
    K.sb_top = C.arena
    C.WINR = K.sb("WINR", [128, NC8, HCOLS_A], BF16)
    C.WOA = K.sb("WOA", [128, 8, D], BF16)
    C.WOC = K.sb("WOC", [128, 4, D], BF16)
    C.KT2 = K.sb("KT2", [128, S], BF16)
    C.KI3 = K.sb("KI3", [128, S], BF16)
    C.VA = K.sb("VA", [128, S // 128, 65], BF16)
    C.SS_ = K.sb("SS_", [128, 8192], F32)
    so = C.SS_.off
    C.WGU = K.sb("WGU", [128, NC8, 1536], BF16, offset=so)
    C.PBUF = K.sb("PBUF", [128, 4, NH + 2], F32, offset=so + 24576)
    C.GCS = K.sb("GCS", [128, NH], F32, offset=so + 24576 + 4160)
    C.CV = K.sb("CV", [128, NH], F32, offset=so + 24576 + 4160 + 1024)
    C.QF = K.sb("QF", [128, NH], F32, offset=so + 24576 + 4160 + 2048)
    assert 24576 + 4160 + 3072 <= 32768
    C.NEGM = K.sb("NEGM", [128, 8192], BF16)
    C.QIT = K.sb("QIT", [128, 3, NH], BF16)
    C.DG = K.sb("DG", [128, 8, 128], BF16)
    C.RDEN = K.sb("RDEN", [128, 1024], F32)
    C.RB = [K.sb(f"RB{i}", [128, 512], F32) for i in range(2)]
    C.HALO = K.sb("HALO", [128, 4, 2], F32)
    C.WI = K.sb("WI", [128, 2, 8], F32)
    C.WA = K.sb("WA", [128, 2, 8], F32)
    C.SGN = K.sb("SGN", [128, 2, 8], F32)
    C.ST = K.sb("ST", [128, 8], F32)
    C.JA = K.sb("JA", [128, 2688], BF16)
    C.RR += [K.sb(f"RR{i}", [128, 512], BF16) for i in (4, 5)]
    C.PTF[1] = K.sb("PTF1", [128, 1024], BF16)
    C.CST = K.sb("CST", [128, 896], F32)
    C.IDB = K.sb("IDB", [128, 512], BF16)
    print("hyb sbuf top", K.sb_top)
    assert K.sb_top <= 229344


def rmsnorm_h(C, xt, vcol):
    K = C.K
    nc = C.nc
    K.tt(C.ACCH[:, :], xt[:, 0, :], xt[:, 0, :], ALU.mult)
    for c in range(1, NC8):
        tmp = C.TMPH[c % 2]
        K.tt(tmp[:, :], xt[:, c, :], xt[:, c, :], ALU.mult)
        K.tt(C.ACCH[:, :], C.ACCH[:, :], tmp[:, :], ALU.add)
    ps = C.PS[6][:, 0:NH]
    K.mm(ps, C.ONES[:, :], C.ACCH[:, :])
    rs = C.RSTDH[:, :]
    K.ts(rs, ps, 1.0 / D, EPS, ALU.mult, ALU.add)
    K.op("dve", lambda: nc.vector.reciprocal(rs.ap, rs.ap), reads=[rs], writes=[rs])
    K.act(rs, rs, AF.Sqrt)
    for c in range(NC8):
        K.stt(C.HNH[:, c, :], xt[:, c, :], C.VEC[:, vcol + c:vcol + c + 1], rs, ALU.mult, ALU.mult)


def hyb_phase(C, src, src_name, dst, dst_name):
    K, nc = C.K, C.nc
    S = C.S
    nht = S // NH
    nblk = S // 128
    wa = C.w_hyb_a.rearrange("(c p) f -> p c f", p=128)
    for c in range(NC8):
        K.dma("pool", C.WINR[:, c, :], K.dview(wa[:, c, :], "d_w", 0, 1))
    woa = C.w_out[0:512, :].rearrange("(h p) m -> p h m", p=64)
    woc = C.w_out[512:1024, :].rearrange("(c p) m -> p c m", p=128)
    for h in range(8):
        K.dma("pool", C.WOA[0:64, h, :], K.dview(woa[:, h, :], "d_w", 0, 1))
    for c in range(4):
        K.dma("pool", C.WOC[:, c, :], K.dview(woc[:, c, :], "d_w", 0, 1))
    K.dma("sp", C.CST[:, :], K.dview(C.cst, "d_cst", 0, 1))
    K.copy(C.IDB[:, :], C.CST[:, 256:768])
    K.memset(C.VA[:, :, 64:65], 1.0)
    K.memset(C.HALO[:, :, :], 0.0)
    PMA = C.CST[:, 0:128]
    PMI = C.CST[:, 128:256]
    CAUS = C.CST[:, 768:896]
    wgu = C.w_in[:, GU0:GU0 + 1536].rearrange("(c p) f -> p c f", p=128)
    vnorm = VOFF["mix_norm"]
    vcw = VOFF["hyb_conv_w"]
    stores = []

    def rope(ps, ci, si, PM, out):
        K.copy(C.QF[:, :], ps, eng="act")
        ps2 = C.PS[4 + rope.n % 2][:, 0:NH]
        rope.n += 1
        K.mm(ps2, PM, C.QF[:, :])
        K.tt(C.T1[:, :], C.QF[:, :], C.TAB[:, ci, :], ALU.mult)
        K.tt(C.QF[:, :], ps2, C.TAB[:, si, :], ALU.mult)
        K.tt(out, C.T1[:, :], C.QF[:, :], ALU.add)
    rope.n = 0

    def proj(i):
        t0 = i * NH
        xt = C.XH[i % 2]
        K.dma("sp", xt[:, :, :], C.K.dview(src[:, t0:t0 + NH].rearrange("(c p) t -> p c t", p=128), src_name, t0, t0 + NH))
        K.dma("sp", C.TAB[:, :, :], K.dview(C.tab[:, :, t0:t0 + NH].rearrange("k p t -> p k t"), "d_tab", 0, 1))
        for c in range(NC8):
            K.dma("pool", C.WGU[:, c, :], K.dview(wgu[:, c, :], "d_w", 0, 1))
        rmsnorm_h(C, xt, vnorm)
        pi = [0]

        def nps(rows=128, cols=NH):
            p = C.PS[pi[0] % 4][0:rows, 0:cols]
            pi[0] += 1
            return p

        def fm(ps, W, col0, ncol):
            for k in range(NC8):
                K.mm(ps, W[:, k, col0:col0 + ncol], C.HNH[:, k, :], start=(k == 0), stop=(k == NC8 - 1))

        QT = C.QT[i % 2]
        for c in range(4):
            ps = nps()
            fm(ps, C.WINR, CQ + c * 128, 128)
            rope(ps, 0, 1, PMA, QT[:, c, :])
        ps = nps()
        fm(ps, C.WINR, CK2, 128)
        rope(ps, 0, 1, PMA, C.KT2[:, t0:t0 + NH])
        for g in range(3):
            ps = nps()
            fm(ps, C.WINR, CQI + g * 128, 128)
            rope(ps, 2, 3, PMI, C.QIT[:, g, :])
        ps = nps()
        fm(ps, C.WINR, CKI, 128)
        rope(ps, 2, 3, PMI, C.KI3[:, t0:t0 + NH])
        for q in range(2):
            ps = nps(128, 72)
            for k in range(NC8):
                K.mm(ps, C.HNH[:, k, q * 128:(q + 1) * 128], C.WINR[:, k, CVW:CVW + 72], start=(k == 0), stop=(k == NC8 - 1))
            ch = (t0 // 128) + q
            K.copy(C.VA[:, ch, 0:64], C.PS[(pi[0] - 1) % 4][:, 0:64])
            K.ts(C.WI[:, q, :], C.PS[(pi[0] - 1) % 4][:, 64:72], 1.0 / 16.0, None, ALU.mult)
            K.ts(C.SGN[:, q, :], C.WI[:, q, :], 0.0, 2.0, ALU.is_ge, ALU.mult)
            K.ts(C.SGN[:, q, :], C.SGN[:, q, :], -1.0, None, ALU.add)
            K.tt(C.WA[:, q, :], C.WI[:, q, :], C.SGN[:, q, :], ALU.mult)
        YC = C.YC[i % 2]
        for c in range(4):
            pgc = nps()
            fm(pgc, C.WGU, 512 + c * 128, 128)
            K.copy(C.GCS[:, :], pgc, eng="act")
            pu = nps()
            fm(pu, C.WGU, 1024 + c * 128, 128)
            K.copy(C.PBUF[:, c, 0:2], C.HALO[:, c, :])
            K.tt(C.PBUF[:, c, 2:2 + NH], pu, C.GCS[:, :], ALU.mult)
            K.copy(C.HALO[:, c, :], C.PBUF[:, c, NH:NH + 2])
            K.ts(C.CV[:, :], C.PBUF[:, c, 2:2 + NH], C.VEC[:, vcw + c * 3 + 2:vcw + c * 3 + 3], None, ALU.mult)
            K.stt(C.CV[:, :], C.PBUF[:, c, 1:1 + NH], C.VEC[:, vcw + c * 3 + 1:vcw + c * 3 + 2], C.CV[:, :], ALU.mult, ALU.add)
            K.stt(C.CV[:, :], C.PBUF[:, c, 0:NH], C.VEC[:, vcw + c * 3:vcw + c * 3 + 1], C.CV[:, :], ALU.mult, ALU.add)
            pgb = nps()
            fm(pgb, C.WGU, c * 128, 128)
            K.tt(YC[:, c, :], pgb, C.CV[:, :], ALU.mult)

    def score(b):
        L = 128 * (b + 1)
        qq = b % 2
        for h in range(8):
            K.ts(C.DG[:, h, :], C.IDB[:, 0:128], C.SGN[:, qq, h:h + 1], None, ALU.mult)
        nkt = (L + 511) // 512
        DB = [C.PS[0], C.PS[1], C.PS[2], C.PS[3], C.PS[6], C.PS[7]]
        LAG = 5
        pend = []

        def sum_mm(item):
            n, kt, hh, w = item
            pss = C.PS[4 + kt % 2][:, 0:w]
            K.mm(pss, C.DG[:, hh, :], C.RR[n % 6][:, 0:w], start=(hh == 0), stop=(hh == 7))
            if hh == 7:
                K.copy(C.SS_[:, 512 * kt:512 * kt + w], pss)

        groups = []
        n = 0
        for kt in range(nkt):
            w = min(512, L - 512 * kt)
            for hs in ((0, 1, 2), (3, 4, 5), (6, 7)):
                groups.append([(n + i, kt, h, w) for i, h in enumerate(hs)])
                n += len(hs)
        pendg = []
        for grp in groups:
            for (n_, kt, h, w) in grp:
                k0 = 512 * kt
                g, r = divmod(h, 3)
                ps = DB[n_ % 6][:, 0:w]
                K.mm(ps, C.QIT[32 * r:32 * r + 32, g, qq * 128:(qq + 1) * 128], C.KI3[32 * r:32 * r + 32, k0:k0 + w])
            for (n_, kt, h, w) in grp:
                ps = DB[n_ % 6][:, 0:w]
                if h in DVE_RELU_HEADS:
                    K.ts(C.RR[n_ % 6][:, 0:w], ps, C.WA[:, qq, h:h + 1], 0.0, ALU.mult, ALU.max)
                else:
                    K.act(C.RR[n_ % 6][:, 0:w], ps, AF.Relu, scale=C.WA[:, qq, h:h + 1])
            pendg.append(grp)
            if len(pendg) > 1:
                for item in pendg.pop(0):
                    sum_mm(item)
        for grp in pendg:
            for item in grp:
                sum_mm(item)
        K.tt(C.SS_[:, L - 128:L], C.SS_[:, L - 128:L], CAUS, ALU.add)

    def search_iters(b):
        L = 128 * (b + 1)
        LO = C.ST[:, 0:1]
        MID = C.ST[:, 1:2]
        CNTD = C.ST[:, 2:3]
        M = C.ST[:, 3:4]
        SGA = C.ST[:, 4:5]
        X = C.ST[:, 5:6]
        if b < 2:
            K.memset(LO, -1e29)
            return []
        L2 = 128 * int(ACT_FRAC * (b + 1))
        L1 = L - L2
        thr = float(TOPK) - 0.5 * L2
        K.memset(MID, 0.0)
        junk = View(C.T1.h[:, 0:1].broadcast_to([128, L1]), C.T1[:, 0:1].regs)
        its = []
        step = THR_B
        for it in range(N_ITER):
            nstep = step / 2.0

            def f(it=it, step=step, nstep=nstep):
                K.ts(junk, C.SS_[:, 0:L1], MID, 0.0, ALU.is_ge, ALU.add, accum=CNTD)
                if L2:
                    K.act(C.JA[:, 0:L2], C.SS_[:, L1:L], AF.Sign, bias=MID, scale=-1.0, accum=SGA)
                    K.stt(X, SGA, -0.5, CNTD, ALU.mult, ALU.add)
                    K.ts(M, X, thr, step, ALU.is_ge, ALU.mult)
                else:
                    K.ts(M, CNTD, thr, step, ALU.is_ge, ALU.mult)
                if it < N_ITER - 1:
                    K.stt(MID, M, -nstep, MID, ALU.add, ALU.add)
                else:
                    K.stt(LO, M, -step, MID, ALU.add, ALU.add)
            its.append(f)
            step = nstep
        return its

    def negm_piece(b, p):
        L = 128 * (b + 1)
        k0 = 512 * p
        w = min(512, L - k0)
        K.ts(C.NEGM[:, k0:k0 + w], C.SS_[:, k0:k0 + w], C.ST[:, 0:1], NEG, ALU.is_lt, ALU.mult)

    def npieces(b):
        return (128 * (b + 1) + 511) // 512

    import os
    dbg = os.environ.get("HYB_DBG", "psta3")

    def attn(b, nb=None):
        qq = b % 2
        QT = C.QT[(b // 2) % 2]
        OT = [C.PS[6], C.PS[7]]
        its = search_iters(nb) if nb is not None else []
        idone = 0

        def LM(j):
            psl = [C.PS[(2 * j) % 4], C.PS[(2 * j + 1) % 4]]
            for hl in range(4):
                for par in range(2):
                    base = 64 * par
                    K.mm(psl[par][:, hl * 128:(hl + 1) * 128], C.KT2[base:base + 64, 128 * j:128 * j + 128],
                         QT[base:base + 64, hl, qq * 128:(qq + 1) * 128], start=(hl == 0), stop=False)
            for par in range(2):
                K.mm(psl[par][:, :], C.NEGM[:, 128 * j:128 * j + 128], C.IDB[:, :], start=False, stop=True)

        LM(0)
        for j in range(b + 1):
            if j + 1 <= b:
                LM(j + 1)
            pk = j % 2
            pair = View(C.PSP[pk][:, :], [("ps", 2 * pk * 2048, (2 * pk + 2) * 2048)])
            ptf = C.PTF[j % 2]
            K.act(ptf[:, :], pair, AF.Exp, scale=0.125)
            for par in range(2):
                K.mm(OT[par][0:65, :], C.VA[:, j, 0:65], ptf[:, par * 512:(par + 1) * 512], start=(j == 0), stop=(j == b))
            want = (len(its) * (j + 1) + b) // (b + 1)
            while idone < min(want, len(its)):
                its[idone]()
                idone += 1
        while idone < len(its):
            its[idone]()
            idone += 1
        if nb is not None:
            for p in range(npieces(nb)):
                negm_piece(nb, p)
        if "3" not in dbg:
            return
        for par in range(2):
            rd = C.RDEN[64:65, par * 512:(par + 1) * 512]
            ot = OT[par]
            K.op("dve", (lambda rd=rd, ot=ot: nc.vector.reciprocal(rd.ap, ot[64:65, :].ap)), reads=[ot[64:65, :]], writes=[rd])
            pb = C.PS[4 + par][0:64, :]
            K.mm(pb, C.ONES[64:65, 0:64], rd)
            rb = C.RB[par]
            K.copy(rb[0:64, :], pb, eng="act")
            yv = C.YAT[0:64, 4 * par:4 * par + 4, qq * 128:(qq + 1) * 128]
            otv = View(ot[0:64, :].ap.rearrange("p (h t) -> p h t", h=4), ot[0:64, :].regs)
            rbv = View(rb.h[0:64, :].rearrange("p (h t) -> p h t", h=4), rb[0:64, :].regs)
            K.tt(yv, otv, rbv, ALU.mult)

    def outproj(i):
        t0 = i * NH
        xt = C.XH[i % 2]
        YC = C.YC[i % 2]
        for m in range(NC8):
            po = C.PS[m % 4][:, 0:NH]
            for h in range(8):
                K.mm(po, C.WOA[0:64, h, m * 128:(m + 1) * 128], C.YAT[0:64, 4 * (h % 2) + h // 2, :], start=(h == 0), stop=False)
            for c in range(4):
                K.mm(po, C.WOC[:, c, m * 128:(m + 1) * 128], YC[:, c, :], start=False, stop=(c == 3))
            K.tt(xt[:, m, :], xt[:, m, :], po, ALU.add)
        stores.append(K.dma("act", K.dview(dst[:, t0:t0 + NH].rearrange("(c p) t -> p c t", p=128), dst_name, t0, t0 + NH), xt[:, :, :]))

    for i in range(nht):
        proj(i)
        for b in (2 * i, 2 * i + 1):
            score(b)
            if b == 0:
                search_iters(0)
                for p in range(npieces(0)):
                    negm_piece(0, p)
            else:
                attn(b - 1, b)
                if (b - 1) % 2 == 1:
                    outproj((b - 1) // 2)
    attn(nblk - 1, None)
    outproj(nht - 1)
    return stores


PHASES = [
    ("ffn", "xT", "d_x", "hA", "d_h", 0, 0, VOFF["ffn_norm"] + 0),
    ("hyb", "hA", "d_h", "hA", "d_h"),
    ("ffn", "hA", "d_h", "hA", "d_h", 0, 1, VOFF["ffn_norm"] + 8),
    ("ffn", "hA", "d_h", "hA", "d_h", 1, 0, VOFF["ffn_norm"] + 16),
    ("conf", "hA", "d_h", "hA", "d_h"),
    ("ffn", "hA", "d_h", "outT", "d_out", 1, 1, VOFF["ffn_norm"] + 24, VOFF["final_norm"]),
]

_CACHE = {}


def host_inputs(inputs, S):
    f = lambda a: np.ascontiguousarray(np.asarray(a, np.float32))
    tab, cst = hyb_consts(S)
    shared = {
        "vecs": pack_vecs(inputs),
        "ffn_w_gate": f(inputs["ffn_w_gate"]), "ffn_w_up": f(inputs["ffn_w_up"]), "ffn_w_down": f(inputs["ffn_w_down"]),
        "conf_w_pw1": f(np.asarray(inputs["conf_w_pw1"])[0]), "conf_w_pw2": f(np.asarray(inputs["conf_w_pw2"])[0]),
        "hyb_w_in": f(np.asarray(inputs["hyb_w_in"])[0]), "w_hyb_a": pack_hyb_w(np.asarray(inputs["hyb_w_in"])[0]),
        "hyb_w_out": f(np.asarray(inputs["hyb_w_out"])[0]), "rope_tab": tab, "hyb_cst": cst,
    }
    return shared


def kernel(**inputs):
    x = np.asarray(inputs["x"], np.float32)
    B, S, _ = x.shape
    if S not in _CACHE:
        _CACHE[S] = build(S, PHASES)
    nc = _CACHE[S]
    shared = host_inputs(inputs, S)
    in_maps = []
    for b in range(B):
        m = dict(shared)
        m["xT"] = np.ascontiguousarray(x[b].T)
        in_maps.append(m)
    res = run_bass_kernel_spmd(nc, in_maps, core_ids=list(range(B)))
    out = np.stack([np.ascontiguousarray(r["outT"].T) for r in res.results], 0)
    return out.astype(np.float32)
```
